# Optimizing a Trainium2 kernel written in Bass

```python
import math
import jax, jax.numpy as jnp
from jax import lax
import numpy as np

D_MODEL = 1024
BATCH = 8
SEQ = 4096
DEPTH = 4

GRID_W = 64
CTX_LEN = 256
HEAD_DIM = 64
N_HEADS_A = 8
N_KV_A = 2
REP_A = N_HEADS_A // N_KV_A
WINDOW = 128
ATTN_BLOCK = 128
N_HEADS_B = 4
RET_DK = 64
RET_DV = 128
RET_CHUNK = 128
N_HEADS_C = 4
DIFF_DK = 64
DIFF_DV = 128
N_BRANCH = 3
BRANCH_W = 512
D_FF = 2816
N_EXPERTS = 8
TOP_K = 2
EXPERT_BLOCK = 512
ROPE_BASE = 10000.0
ROPE_FREQS = HEAD_DIM // 4
NORM_EPS = 1e-6
NEG_INF = -1e30
IN_SIZES = (N_HEADS_A * HEAD_DIM, N_KV_A * HEAD_DIM, N_KV_A * HEAD_DIM,
            N_HEADS_B * RET_DK, N_HEADS_B * RET_DK, N_HEADS_B * RET_DV, N_HEADS_B * RET_DV,
            N_HEADS_C * 2 * DIFF_DK, N_HEADS_C * 2 * DIFF_DK, N_HEADS_C * DIFF_DV,
            N_BRANCH * D_MODEL)
D_IN = 6912

kernel_name = "hybrid_diffusion_gqa_retention_diffattn_moe"


def _rms(x):
    x32 = x.astype(jnp.float32)
    return x32 * lax.rsqrt(jnp.mean(jnp.square(x32), axis=-1, keepdims=True) + NORM_EPS)


def _rmsnorm(x, g):
    return (_rms(x) * g.astype(jnp.float32)).astype(x.dtype)


def _axial_rope_tables(n_tokens):
    rows = n_tokens // GRID_W
    row = jnp.repeat(jnp.arange(rows, dtype=jnp.float32), GRID_W)
    col = jnp.tile(jnp.arange(GRID_W, dtype=jnp.float32), rows)
    inv = 1.0 / (ROPE_BASE ** (jnp.arange(ROPE_FREQS, dtype=jnp.float32) / ROPE_FREQS))
    ang = jnp.stack([row[:, None] * inv, col[:, None] * inv], axis=1)
    return jnp.cos(ang), jnp.sin(ang)


def _rope(x, cos, sin):
    shp = x.shape
    xr = x.reshape(shp[:-1] + (2, 2, ROPE_FREQS))
    bshape = (shp[1],) + (1,) * (x.ndim - 3) + (2, ROPE_FREQS)
    c = cos.reshape(bshape).astype(x.dtype)
    s = sin.reshape(bshape).astype(x.dtype)
    x1, x2 = xr[..., 0, :], xr[..., 1, :]
    return jnp.stack([x1 * c - x2 * s, x2 * c + x1 * s], axis=-2).reshape(shp)


def _sink_softmax(s, sink):
    col = jnp.broadcast_to(sink.astype(jnp.float32)[None, :, :, None, None], s.shape[:-1] + (1,))
    return jax.nn.softmax(jnp.concatenate([s, col], axis=-1), axis=-1)[..., :-1]


def _window_gqa(q, k, v, k_ctx, v_ctx, sink):
    b, n, g, r, d = q.shape
    scale = d ** -0.5
    span = ATTN_BLOCK + 2 * WINDOW
    pad = ((0, 0), (WINDOW, WINDOW), (0, 0), (0, 0))
    kp, vp = jnp.pad(k, pad), jnp.pad(v, pad)
    qi = jnp.arange(ATTN_BLOCK)
    kj = jnp.arange(span)
    rel = kj[None, :] - qi[:, None]
    band = (rel >= 0) & (rel <= 2 * WINDOW)

    def block(i):
        start = i * ATTN_BLOCK
        qs = lax.dynamic_slice_in_dim(q, start, ATTN_BLOCK, axis=1)
        ks = lax.dynamic_slice_in_dim(kp, start, span, axis=1)
        vs = lax.dynamic_slice_in_dim(vp, start, span, axis=1)
        kpos = start - WINDOW + kj
        mask = band & ((kpos >= 0) & (kpos < n))[None, :]
        s_loc = jnp.einsum('bqgrd,bkgd->bgrqk', qs, ks).astype(jnp.float32) * scale
        s_loc = jnp.where(mask, s_loc, NEG_INF)
        s_ctx = jnp.einsum('bqgrd,bkgd->bgrqk', qs, k_ctx).astype(jnp.float32) * scale
        p = _sink_softmax(jnp.concatenate([s_loc, s_ctx], axis=-1), sink).astype(v.dtype)
        return (jnp.einsum('bgrqk,bkgd->bqgrd', p[..., :span], vs)
                + jnp.einsum('bgrqk,bkgd->bqgrd', p[..., span:], v_ctx))

    o = lax.map(block, jnp.arange(n // ATTN_BLOCK))
    return jnp.moveaxis(o, 0, 1).reshape(b, n, g * r * d)


def _ctx_gqa(q, k, v, sink):
    b, n, g, r, d = q.shape
    s = jnp.einsum('bqgrd,bkgd->bgrqk', q, k).astype(jnp.float32) * d ** -0.5
    p = _sink_softmax(s, sink).astype(v.dtype)
    return jnp.einsum('bgrqk,bkgd->bqgrd', p, v).reshape(b, n, g * r * d)


def _retention_scan(q, k, v, log_gamma, s0):
    b, t, h, dk = q.shape
    dv = v.shape[-1]
    c = RET_CHUNK
    n = t // c
    qc = q.reshape(b, n, c, h, dk)
    kc = k.reshape(b, n, c, h, dk)
    vc = v.reshape(b, n, c, h, dv)
    pos = jnp.arange(c, dtype=jnp.float32)
    diff = pos[:, None] - pos[None, :]
    dmat = jnp.where(diff >= 0, jnp.exp(jnp.maximum(diff, 0.0) * log_gamma[:, None, None]), 0.0)
    scores = jnp.einsum('bnchd,bnkhd->bnhck', qc, kc) * dmat
    intra = jnp.einsum('bnhck,bnkhe->bnche', scores, vc)
    k_dec = jnp.exp((c - 1 - pos)[None, :] * log_gamma[:, None])
    kv = jnp.einsum('bnkhd,hk,bnkhe->nbhde', kc, k_dec, vc)
    chunk_dec = jnp.exp(c * log_gamma)[None, :, None, None]

    def step(state, kv_n):
        return state * chunk_dec + kv_n, state

    s_last, s_prev = lax.scan(step, s0, kv)
    q_dec = jnp.exp((pos + 1.0)[None, :] * log_gamma[:, None])
    inter = jnp.einsum('bnchd,hc,nbhde->bnche', qc, q_dec, s_prev)
    return (intra + inter).reshape(b, t, h, dv), s_last


def _retention_bidir(q, k, v, lg_f, lg_b, s0_f, s0_b):
    o_f, s_f = _retention_scan(q, k, v, lg_f, s0_f)
    flip = lambda a: jnp.flip(a, axis=1)
    o_b, s_b = _retention_scan(flip(q), flip(k), flip(v), lg_b, s0_b)
    return o_f + flip(o_b), s_f, s_b


def _ret_out(o, g):
    b, t = o.shape[:2]
    y = _rms(o) * jax.nn.silu(g.astype(jnp.float32)).reshape(o.shape)
    return y.astype(g.dtype).reshape(b, t, -1)


def _diff_attend(q, k, v, lam):
    s = jnp.einsum('bqhcd,bkhcd->bhcqk', q, k).astype(jnp.float32) * q.shape[-1] ** -0.5
    p = jax.nn.softmax(s, axis=-1)
    a = p[:, :, 0] - lam * p[:, :, 1]
    return jnp.einsum('bhqk,bkhe->bqhe', a.astype(v.dtype), v)


def _diff_latent(q, k_all, v_all, lam):
    b, n = q.shape[:2]

    def block(i):
        qs = lax.dynamic_slice_in_dim(q, i * ATTN_BLOCK, ATTN_BLOCK, axis=1)
        return _diff_attend(qs, k_all, v_all, lam)

    o = lax.map(block, jnp.arange(n // ATTN_BLOCK))
    return jnp.moveaxis(o, 0, 1).reshape((b, n) + o.shape[-2:])


def _diff_norm(o, gain, lam_init):
    b, t = o.shape[:2]
    y = _rms(o) * gain.astype(jnp.float32) * (1.0 - lam_init)
    return y.astype(o.dtype).reshape(b, t, -1)


def _split_in(p):
    out, off = [], 0
    for sz in IN_SIZES:
        out.append(p[..., off:off + sz])
        off += sz
    return out


def _merge(outs, gate_logits, w_branch, w_out):
    g = gate_logits.reshape(gate_logits.shape[:2] + (N_BRANCH, D_MODEL))
    y = sum(jax.nn.sigmoid(g[:, :, i]) * (o @ w_branch[i]) for i, o in enumerate(outs))
    return y @ w_out


def _rs(t, *shape):
    return t.reshape(t.shape[:2] + shape)


def _mixer(h, hc, cos, sin, w_in, sink, dec_f, dec_b, lam_p, lam_init, d_gain, w_branch, w_out, ctx_out):
    f32 = jnp.float32
    b = h.shape[0]
    qa, ka, va, qb, kb, vb, gb, qd, kd, vd, gl = _split_in(h @ w_in)
    qa_c, ka_c, va_c, qb_c, kb_c, vb_c, gb_c, qd_c, kd_c, vd_c, gl_c = _split_in(hc @ w_in)
    sink = sink.reshape(N_KV_A, REP_A)
    qa = _rope(_rs(qa, N_KV_A, REP_A, HEAD_DIM), cos, sin)
    ka = _rope(_rs(ka, N_KV_A, HEAD_DIM), cos, sin)
    va = _rs(va, N_KV_A, HEAD_DIM)
    ka_c, va_c = _rs(ka_c, N_KV_A, HEAD_DIM), _rs(va_c, N_KV_A, HEAD_DIM)
    o_a = _window_gqa(qa, ka, va, ka_c, va_c, sink)
    lg_f = jax.nn.log_sigmoid(dec_f.astype(f32))
    lg_b = jax.nn.log_sigmoid(dec_b.astype(f32))
    qb = _rope(_rs(qb, N_HEADS_B, RET_DK), cos, sin).astype(f32)
    kb = _rope(_rs(kb, N_HEADS_B, RET_DK), cos, sin).astype(f32) * RET_DK ** -0.5
    vb = _rs(vb, N_HEADS_B, RET_DV).astype(f32)
    qb_c = _rs(qb_c, N_HEADS_B, RET_DK).astype(f32)
    kb_c = _rs(kb_c, N_HEADS_B, RET_DK).astype(f32) * RET_DK ** -0.5
    vb_c = _rs(vb_c, N_HEADS_B, RET_DV).astype(f32)
    zero = jnp.zeros((b, N_HEADS_B, RET_DK, RET_DV), f32)
    o_bc, s_f, s_b = _retention_bidir(qb_c, kb_c, vb_c, lg_f, lg_b, zero, zero)
    o_b, _, _ = _retention_bidir(qb, kb, vb, lg_f, lg_b, s_f, s_b)
    o_b = _ret_out(o_b, gb)
    lp = lam_p.astype(f32)
    lam = jnp.exp(jnp.sum(lp[0] * lp[1])) - jnp.exp(jnp.sum(lp[2] * lp[3])) + lam_init
    qd = _rope(_rs(qd, N_HEADS_C, 2, DIFF_DK), cos, sin)
    kd = _rope(_rs(kd, N_HEADS_C, 2, DIFF_DK), cos, sin)
    vd = _rs(vd, N_HEADS_C, DIFF_DV)
    kd_c, vd_c = _rs(kd_c, N_HEADS_C, 2, DIFF_DK), _rs(vd_c, N_HEADS_C, DIFF_DV)
    k_all = jnp.concatenate([kd, kd_c], axis=1)
    v_all = jnp.concatenate([vd, vd_c], axis=1)
    o_c = _diff_norm(_diff_latent(qd, k_all, v_all, lam), d_gain, lam_init)
    y = _merge([o_a, o_b, o_c], gl, w_branch, w_out)
    if not ctx_out:
        return y, None
    o_ac = _ctx_gqa(_rs(qa_c, N_KV_A, REP_A, HEAD_DIM), ka_c, va_c, sink)
    o_bc = _ret_out(o_bc, gb_c)
    o_cc = _diff_norm(_diff_attend(_rs(qd_c, N_HEADS_C, 2, DIFF_DK), kd_c, vd_c, lam), d_gain, lam_init)
    yc = _merge([o_ac, o_bc, o_cc], gl_c, w_branch, w_out)
    return y, yc


def _swiglu(h, w1, w3, w2):
    return (jax.nn.silu(h @ w1) * (h @ w3)) @ w2


def _moe(h, w_router, w1, w3, w2):
    t, d = h.shape
    logits = (h @ w_router).astype(jnp.float32)
    top_val, top_idx = lax.top_k(logits, TOP_K)
    gates = jax.nn.softmax(top_val, axis=-1).astype(h.dtype)
    flat_e = top_idx.reshape(-1)
    flat_t = jnp.repeat(jnp.arange(t), TOP_K)
    order = jnp.argsort(flat_e)
    e_s, t_s, g_s = flat_e[order], flat_t[order], gates.reshape(-1)[order]
    counts = jnp.bincount(flat_e, length=N_EXPERTS)
    starts = jnp.cumsum(counts) - counts
    padded = (counts + EXPERT_BLOCK - 1) // EXPERT_BLOCK * EXPERT_BLOCK
    pad_ends = jnp.cumsum(padded)
    dest = (pad_ends - padded)[e_s] + jnp.arange(t * TOP_K) - starts[e_s]
    n_blocks = (t * TOP_K + EXPERT_BLOCK - 1) // EXPERT_BLOCK + N_EXPERTS
    buf = jnp.zeros((n_blocks * EXPERT_BLOCK, d), h.dtype).at[dest].set(h[t_s])
    block_e = jnp.minimum(jnp.searchsorted(pad_ends, jnp.arange(n_blocks) * EXPERT_BLOCK, side='right'),
                          N_EXPERTS - 1)

    def expert_block(args):
        xb, e = args
        return _swiglu(xb, w1[e], w3[e], w2[e])

    out_buf = lax.map(expert_block, (buf.reshape(n_blocks, EXPERT_BLOCK, d), block_e)).reshape(-1, d)
    return jnp.zeros_like(h).at[t_s].add(out_buf[dest] * g_s[:, None])


def setup_inputs(seed: int = 0) -> dict:
    key = jax.random.key(seed)
    ks = jax.random.split(key, 26)
    nrm = lambda k, shape, s: jax.random.normal(k, shape, jnp.float32) * s
    n_dense = (DEPTH + 1) // 2
    n_moe = DEPTH // 2
    D = D_MODEL
    hb = jnp.arange(N_HEADS_B, dtype=jnp.float32)
    ret_logit = jnp.log(1.0 - 2.0 ** (-5.0 - hb)) + (5.0 + hb) * math.log(2.0)
    return {
        "x": nrm(ks[0], (BATCH, SEQ, D), 1.0),
        "c": nrm(ks[1], (BATCH, D), 1.0),
        "ctx": nrm(ks[2], (BATCH, CTX_LEN, D), 1.0),
        "c_ctx": nrm(ks[3], (D,), 1.0),
        "w_mod": nrm(ks[4], (DEPTH, D, 6 * D), 0.5 * D ** -0.5),
        "b_mod": nrm(ks[5], (DEPTH, 6 * D), 0.02),
        "norm_mix": 1.0 + nrm(ks[6], (DEPTH, D), 0.02),
        "norm_ffn": 1.0 + nrm(ks[7], (DEPTH, D), 0.02),
        "norm_final": 1.0 + nrm(ks[8], (D,), 0.02),
        "w_in": nrm(ks[9], (DEPTH, D, D_IN), D ** -0.5),
        "attn_sink": nrm(ks[10], (DEPTH, N_HEADS_A), 0.5),
        "ret_decay_fwd": ret_logit[None, :] + nrm(ks[11], (DEPTH, N_HEADS_B), 0.1),
        "ret_decay_bwd": ret_logit[None, :] + nrm(ks[12], (DEPTH, N_HEADS_B), 0.1),
        "diff_lambda": nrm(ks[13], (DEPTH, 4, DIFF_DK), 0.1),
        "diff_norm": 1.0 + nrm(ks[14], (DEPTH, DIFF_DV), 0.02),
        "w_branch": nrm(ks[15], (DEPTH, N_BRANCH, BRANCH_W, D), BRANCH_W ** -0.5),
        "w_out": nrm(ks[16], (DEPTH, D, D), D ** -0.5),
        "ffn_w1": nrm(ks[17], (n_dense, D, D_FF), D ** -0.5),
        "ffn_w3": nrm(ks[18], (n_dense, D, D_FF), D ** -0.5),
        "ffn_w2": nrm(ks[19], (n_dense, D_FF, D), D_FF ** -0.5),
        "moe_router": nrm(ks[20], (n_moe, D, N_EXPERTS), D ** -0.5),
        "moe_w1": nrm(ks[21], (n_moe, N_EXPERTS, D, D_FF), D ** -0.5),
        "moe_w3": nrm(ks[22], (n_moe, N_EXPERTS, D, D_FF), D ** -0.5),
        "moe_w2": nrm(ks[23], (n_moe, N_EXPERTS, D_FF, D), D_FF ** -0.5),
    }


def reference(x, c, ctx, c_ctx, w_mod, b_mod, norm_mix, norm_ffn, norm_final, w_in, attn_sink,
              ret_decay_fwd, ret_decay_bwd, diff_lambda, diff_norm, w_branch, w_out,
              ffn_w1, ffn_w3, ffn_w2, moe_router, moe_w1, moe_w3, moe_w2):
    b, n, d = x.shape
    cos, sin = _axial_rope_tables(n)
    cs = jax.nn.silu(c)
    ccs = jax.nn.silu(c_ctx)
    xc = ctx
    for l in range(DEPTH):
        ctx_out = l < DEPTH - 1
        lam_init = 0.8 - 0.6 * math.exp(-0.3 * l)
        mod = cs @ w_mod[l] + b_mod[l]
        mod_c = ccs @ w_mod[l] + b_mod[l]
        sh1, sc1, g1, sh2, sc2, g2 = [m[:, None, :] for m in jnp.split(mod, 6, axis=-1)]
        sh1c, sc1c, g1c, sh2c, sc2c, g2c = jnp.split(mod_c, 6, axis=-1)
        h = _rmsnorm(x, norm_mix[l]) * (1 + sc1) + sh1
        hc = _rmsnorm(xc, norm_mix[l]) * (1 + sc1c) + sh1c
        y, yc = _mixer(h, hc, cos, sin, w_in[l], attn_sink[l], ret_decay_fwd[l], ret_decay_bwd[l],
                       diff_lambda[l], lam_init, diff_norm[l], w_branch[l], w_out[l], ctx_out)
        x = x + g1 * y
        tok = (_rmsnorm(x, norm_ffn[l]) * (1 + sc2) + sh2).reshape(-1, d)
        if ctx_out:
            xc = xc + g1c * yc
            hc2 = _rmsnorm(xc, norm_ffn[l]) * (1 + sc2c) + sh2c
            tok = jnp.concatenate([tok, hc2.reshape(-1, d)], axis=0)
        if l % 2 == 0:
            f = _swiglu(tok, ffn_w1[l // 2], ffn_w3[l // 2], ffn_w2[l // 2])
        else:
            f = _moe(tok, moe_router[l // 2], moe_w1[l // 2], moe_w3[l // 2], moe_w2[l // 2])
        x = x + g2 * f[:b * n].reshape(b, n, d)
        if ctx_out:
            xc = xc + g2c * f[b * n:].reshape(xc.shape)
    return _rmsnorm(x, norm_final)
```

```python
import math
import os
from contextlib import ExitStack
import numpy as np
import ml_dtypes
import concourse.bass as bass
import concourse.mybir as mybir
from concourse.bass_utils import run_bass_kernel_spmd

F32 = mybir.dt.float32
BF16 = mybir.dt.bfloat16
AF = mybir.ActivationFunctionType
ALU = mybir.AluOpType

T = 4352
NLAT = 4096
NCTX = 256
D = 1024
DEPTH = 4
DIN = 6912
DFF = 2816
NE = 8
EPS = 1e-6
TT = [(i * 512, 512) for i in range(8)] + [(4096, 256)]
NFM = 41
NTM = 1664


class Buf:
    __slots__ = ("w", "r", "rp")

    def __init__(self):
        self.w = {}
        self.r = {}
        self.rp = {}


class EngState:
    def __init__(self, name, handle, sem, inc, stream):
        self.name, self.h, self.sem, self.inc, self.stream = name, handle, sem, inc, stream
        self.count = 0


class Prog:
    def __init__(self, nc, stack, nq=4):
        self.nc = nc
        self.engs = {}
        for name in ["tensor", "vector", "scalar", "gpsimd"]:
            sem = stack.enter_context(nc.semaphore("s_" + name))
            self.engs[name] = EngState(name, getattr(nc, name), sem, 1, name)
        self.dmaq = {}
        for qname, issuer, stream in [("sync", nc.sync, "sync"), ("pool", nc.gpsimd, "gpsimd"), ("actq", nc.scalar, "scalar")]:
            lst = []
            for i in range(nq):
                sem = stack.enter_context(nc.semaphore(f"d_{qname}{i}"))
                st = EngState(f"{qname}{i}", issuer, sem, 16, stream)
                lst.append(st)
                self.engs[st.name] = st
            self.dmaq[qname] = [lst, 0]
        self.known = {s: {} for s in ["tensor", "vector", "scalar", "gpsimd", "sync"]}
        self.n_ops = 0
        self.n_waits = 0

    def op(self, eng, fn, reads=(), writes=(), pwrites=()):
        if eng in self.dmaq:
            lst, k = self.dmaq[eng]
            st = lst[k % len(lst)]
            self.dmaq[eng][1] = k + 1
            is_dma = True
        else:
            st = self.engs[eng]
            is_dma = False
        deps = {}

        def add(d):
            for e, i in d.items():
                if deps.get(e, -1) < i:
                    deps[e] = i
        for b in reads:
            add(b.w)
        for b in writes:
            add(b.w)
            add(b.r)
            add(b.rp)
        for b in pwrites:
            if b.r:
                b.rp = dict(b.r)
                b.r = {}
                b.w = {}
            add(b.rp)
        if is_dma and st.count > 0:
            deps[st.name] = max(deps.get(st.name, -1), st.count - 1)
        known = self.known[st.stream]
        for e, i in deps.items():
            if (not is_dma) and e == st.name and e == "tensor":
                continue
            if known.get(e, -1) >= i:
                continue
            es = self.engs[e]
            st.h.wait_ge(es.sem, (i + 1) * es.inc)
            known[e] = i
            self.n_waits += 1
        ins = fn(st.h)
        ins.then_inc(st.sem, st.inc)
        idx = st.count
        st.count += 1
        self.n_ops += 1
        for b in reads:
            if b.r.get(st.name, -1) < idx:
                b.r[st.name] = idx
        for b in writes:
            b.w = {st.name: idx}
            b.r = {}
            b.rp = {}
        for b in pwrites:
            b.w[st.name] = idx
        return idx

    def barrier(self):
        hs = {"tensor": self.nc.tensor, "vector": self.nc.vector, "scalar": self.nc.scalar, "gpsimd": self.nc.gpsimd, "sync": self.nc.sync}
        for sname, h in hs.items():
            known = self.known[sname]
            for e, es in self.engs.items():
                if es.count == 0 or known.get(e, -1) >= es.count - 1:
                    continue
                if e == sname and e == "tensor":
                    continue
                h.wait_ge(es.sem, es.count * es.inc)
                known[e] = es.count - 1
                self.n_waits += 1

    def finish(self, bufs):
        for b in bufs:
            for e, i in b.w.items():
                es = self.engs[e]
                self.nc.sync.wait_ge(es.sem, (i + 1) * es.inc)


class Rot:
    def __init__(self, tiles):
        self.tiles = tiles
        self.bufs = [Buf() for _ in tiles]
        self.i = 0

    def next(self):
        k = self.i % len(self.tiles)
        self.i += 1
        return self.tiles[k], self.bufs[k]


def _consts():
    c = {}
    rows = NLAT // 64
    row = np.repeat(np.arange(rows, dtype=np.float32), 64)
    col = np.tile(np.arange(64, dtype=np.float32), rows)
    inv = (1.0 / (np.float32(10000.0) ** (np.arange(16, dtype=np.float32) / np.float32(16)))).astype(np.float32)
    ang = np.stack([row[:, None] * inv, col[:, None] * inv], axis=1).astype(np.float32)
    cos, sin = np.cos(ang).astype(np.float32), np.sin(ang).astype(np.float32)
    C = np.zeros((64, NLAT), np.float32)
    S = np.zeros((64, NLAT), np.float32)
    for d in range(64):
        ax, f = d // 32, d % 16
        C[d] = cos[:, ax, f]
        S[d] = sin[:, ax, f]
    c["ropeC"] = np.concatenate([C, C], 0)
    c["ropeS"] = np.concatenate([S, S], 0)
    R = np.zeros((64, 64), np.float32)
    for m in range(64):
        if (m % 32) < 16:
            R[m, m + 16] = -1.0
        else:
            R[m, m - 16] = 1.0
    R2 = np.zeros((128, 128), np.float32)
    R2[:64, :64] = R
    R2[64:, 64:] = R
    c["rmatT"] = R2.T.copy().astype(ml_dtypes.bfloat16)
    c["identb"] = np.eye(128, dtype=np.float32).astype(ml_dtypes.bfloat16)
    c["identf"] = np.eye(128, dtype=np.float32)
    c["onesb"] = np.ones((128, 128), np.float32).astype(ml_dtypes.bfloat16)
    k = np.arange(128)[:, None].astype(np.float32)
    q = np.arange(128)[None, :].astype(np.float32)
    ret = np.zeros((128, 8, 128), np.float32)
    ret[:, 0] = np.maximum(q - k, 0)
    ret[:, 1] = (q >= k)
    ret[:, 2] = np.maximum(k - q, 0)
    ret[:, 3] = (k >= q)
    ret[:, 4] = q + 1.0
    ret[:, 5] = 128.0 - q
    ret[:, 6, 0] = 127.0 - k[:, 0]
    ret[:, 6, 1] = k[:, 0]
    ret[:, 6, 2] = 128.0
    c["rettab"] = ret
    mprev = (k >= q).astype(np.float32)
    mnext = (k <= q).astype(np.float32)
    c["wmask"] = np.stack([np.tile(mprev, (1, 4)), np.tile(mnext, (1, 4))], 1).astype(ml_dtypes.bfloat16)
    return c


def build(dbg=False, upto=99, layers=DEPTH, sub=99):
    nc = bass.Bass("TRN2", target_bir_lowering=False)

    def din(name, shape, dt=F32):
        return nc.dram_tensor(name, list(shape), dt, kind="ExternalInput").ap()

    def dscr(name, shape, dt):
        return nc.dram_tensor(name, list(shape), dt, kind=("ExternalOutput" if dbg else "Internal")).ap()

    xT_in = din("xT_in", [D, T])
    c2 = din("c2", [128, 8, 2])
    w_mod = din("w_mod", [DEPTH, D, 6 * D])
    b_mod = din("b_mod", [128, DEPTH, 48])
    nmix = din("nmix", [128, DEPTH, 8])
    nffn = din("nffn", [128, DEPTH, 8])
    nfin = din("nfin", [128, 8])
    w_in = din("w_in", [DEPTH, D, DIN])
    sink = din("sink", [128, DEPTH * 8])
    decs = din("decs", [128, DEPTH * 8])
    dlam = din("dlam", [128, DEPTH, 4, 64])
    dgain = din("dgain", [128, DEPTH, 128])
    w_branch = din("w_branch", [DEPTH, 3, 512, D])
    w_out = din("w_out", [DEPTH, D, D])
    ffn_w1 = din("ffn_w1", [2, D, DFF])
    ffn_w3 = din("ffn_w3", [2, D, DFF])
    ffn_w2 = din("ffn_w2", [2, DFF, D])
    if layers >= 2:
        moe_router = din("moe_router", [2, D, NE])
        moe_w1 = din("moe_w1", [2, NE, D, DFF])
        moe_w3 = din("moe_w3", [2, NE, D, DFF])
        moe_w2 = din("moe_w2", [2, NE, DFF, D])
    ropeC_d = din("ropeC", [128, NLAT])
    ropeS_d = din("ropeS", [128, NLAT])
    rmatT_d = din("rmatT", [128, 128], BF16)
    identb_d = din("identb", [128, 128], BF16)
    identf_d = din("identf", [128, 128])
    onesb_d = din("onesb", [128, 128], BF16)
    rettab_d = din("rettab", [128, 8, 128])
    wmask_d = din("wmask", [128, 2, 512], BF16)

    outT = nc.dram_tensor("outT", [D, NLAT], F32, kind="ExternalOutput").ap()
    xT = dscr("xT", [D, T], F32)
    FM = dscr("FM", [NFM * 128, T], BF16)
    TM = dscr("TM", [T, NTM], BF16)
    BR = dscr("BR", [1536, T], BF16)
    H2 = dscr("H2", [D, T], BF16)
    GT = dscr("GT", [NE, T], F32)

    with ExitStack() as top:
        P = Prog(nc, top)
        ps01 = top.enter_context(nc.psum_tensor("ps01", [128, 1024], F32))
        ps23 = top.enter_context(nc.psum_tensor("ps23", [128, 1024], F32))
        psum = [ps01[:, 0:512], ps01[:, 512:1024], ps23[:, 0:512], ps23[:, 512:1024]]
        for i in range(4, 8):
            psum.append(top.enter_context(nc.psum_tensor(f"ps{i}", [128, 1024], BF16) if i == 6 else nc.psum_tensor(f"ps{i}", [128, 512], F32)))
        psb = [Buf() for _ in range(8)]

        uid = [0]

        def sb(stack, name, shape, dt):
            uid[0] += 1
            return stack.enter_context(nc.sbuf_tensor(f"{name}_s{uid[0]}", list(shape), dt))

        identb = sb(top, "identb", [128, 128], BF16)
        identf = sb(top, "identf", [128, 128], F32)
        onesb = sb(top, "onesb", [128, 128], BF16)
        rmatT = sb(top, "rmatT", [128, 128], BF16)
        modv = sb(top, "modv", [128, DEPTH, 2, 6, 8], F32)
        lgt = sb(top, "lgt", [128, DEPTH * 8], F32)
        esink = sb(top, "esink", [128, DEPTH * 8], F32)
        neglam = sb(top, "neglam", [128, DEPTH], F32)
        gainb = sb(top, "gainb", [128, DEPTH, 128], F32)
        nfin_sb = sb(top, "nfin_sb", [128, 8], F32)
        epsc = sb(top, "epsc", [128, 1], F32)
        cB = Buf()
        xb = [Buf() for _ in TT]
        FMb, TMb, BRb, H2b, GTb, outb = Buf(), Buf(), Buf(), Buf(), Buf(), Buf()

        for dst, src in [(identb, identb_d), (identf, identf_d), (onesb, onesb_d), (rmatT, rmatT_d), (nfin_sb, nfin)]:
            P.op("sync", lambda e, dst=dst, src=src: e.dma_start(out=dst[:], in_=src), pwrites=[cB])
        P.op("vector", lambda e: e.memset(epsc[:], EPS), pwrites=[cB])

        with ExitStack() as ph:
            P.barrier()
            xcp = Rot([sb(ph, f"xcp{i}", [128, 8, 512], F32) for i in range(2)])
            for ti, (t0, w) in enumerate(TT):
                tl, tb = xcp.next()
                P.op("sync", lambda e, tl=tl, t0=t0, w=w: e.dma_start(
                    out=tl[:, :, 0:w], in_=xT_in.rearrange("(kc k) t -> k kc t", k=128)[:, :, t0:t0 + w]), writes=[tb])
                P.op("pool", lambda e, tl=tl, t0=t0, w=w: e.dma_start(
                    out=xT.rearrange("(kc k) t -> k kc t", k=128)[:, :, t0:t0 + w], in_=tl[:, :, 0:w]), reads=[tb], writes=[xb[ti]])

            c_sb = sb(ph, "c_sb", [128, 8, 2], F32)
            cs_sb = sb(ph, "cs_sb", [128, 8, 2], F32)
            bmod_sb = sb(ph, "bmod_sb", [128, DEPTH, 48], F32)
            nmix_sb = sb(ph, "nmix_sb", [128, DEPTH, 8], F32)
            nffn_sb = sb(ph, "nffn_sb", [128, DEPTH, 8], F32)
            modT = sb(ph, "modT", [128, 48, 2], F32)
            sB, mB = Buf(), Buf()
            for dst, src in [(c_sb, c2), (bmod_sb, b_mod), (nmix_sb, nmix), (nffn_sb, nffn)]:
                P.op("sync", lambda e, dst=dst, src=src: e.dma_start(out=dst[:], in_=src), pwrites=[sB])
            P.op("scalar", lambda e: e.activation(out=cs_sb[:], in_=c_sb[:], func=AF.Silu), reads=[sB], writes=[mB])
            wm = Rot([sb(ph, f"wm{i}", [128, 8, 512], F32) for i in range(2)])
            for l in range(layers):
                for n in range(12):
                    wt, wb = wm.next()
                    P.op("sync", lambda e, wt=wt, l=l, n=n: e.dma_start(
                        out=wt[:], in_=w_mod[l].rearrange("(kc k) n -> k kc n", k=128)[:, :, n * 512:(n + 1) * 512]), writes=[wb])
                    for jj in range(4):
                        j = n * 4 + jj
                        for kc in range(8):
                            P.op("tensor", lambda e, wt=wt, jj=jj, kc=kc, j=j: e.matmul(
                                psum[0][:, 2 * j:2 * j + 2], lhsT=wt[:, kc, jj * 128:(jj + 1) * 128], rhs=cs_sb[:, kc, :],
                                start=(kc == 0), stop=(kc == 7)), reads=[wb, mB], writes=[psb[0]])
                mtB = Buf()
                for col in range(2):
                    P.op("vector", lambda e, col=col, l=l: e.tensor_tensor(
                        out=modT[:, :, col], in0=psum[0][:, 0:96].rearrange("p (j c) -> p j c", c=2)[:, :, col],
                        in1=bmod_sb[:, l, :], op=ALU.add), reads=[psb[0], sB], pwrites=[mtB])
                for col in range(2):
                    for kind, (src0, nrm) in enumerate([(8, nmix_sb), (0, None), (16, None), (32, nffn_sb), (24, None), (40, None)]):
                        if nrm is not None:
                            P.op("vector", lambda e, col=col, l=l, kind=kind, src0=src0, nrm=nrm: e.scalar_tensor_tensor(
                                out=modv[:, l, col, kind, :], in0=modT[:, src0:src0 + 8, col], scalar=1.0, in1=nrm[:, l, :],
                                op0=ALU.add, op1=ALU.mult), reads=[mtB, sB], pwrites=[cB])
                        else:
                            P.op("vector", lambda e, col=col, l=l, kind=kind, src0=src0: e.tensor_copy(
                                out=modv[:, l, col, kind, :], in_=modT[:, src0:src0 + 8, col]), reads=[mtB], pwrites=[cB])
            dtmp = sb(ph, "dtmp", [128, DEPTH * 8], F32)
            dtmp2 = sb(ph, "dtmp2", [128, DEPTH * 8], F32)
            dB, dB2 = Buf(), Buf()
            P.op("sync", lambda e: e.dma_start(out=dtmp[:], in_=decs), writes=[dB])
            P.op("scalar", lambda e: e.activation(out=dtmp2[:], in_=dtmp[:], func=AF.Exp, scale=-1.0), reads=[dB], writes=[dB2])
            P.op("scalar", lambda e: e.activation(out=dtmp2[:], in_=dtmp2[:], func=AF.Ln, bias=1.0), reads=[dB2], writes=[dB2])
            P.op("vector", lambda e: e.tensor_scalar(out=lgt[:], in0=dtmp2[:], scalar1=-1.0, scalar2=None, op0=ALU.mult),
                 reads=[dB2], pwrites=[cB])
            stmp = sb(ph, "stmp", [128, DEPTH * 8], F32)
            sB2 = Buf()
            P.op("sync", lambda e: e.dma_start(out=stmp[:], in_=sink), writes=[sB2])
            P.op("scalar", lambda e: e.activation(out=esink[:], in_=stmp[:], func=AF.Exp), reads=[sB2], pwrites=[cB])
            lam_sb = sb(ph, "lam_sb", [128, DEPTH, 4, 64], F32)
            lprod = sb(ph, "lprod", [128, DEPTH, 2, 64], F32)
            lsum = sb(ph, "lsum", [128, DEPTH, 2], F32)
            lB, lB2, lB3 = Buf(), Buf(), Buf()
            P.op("sync", lambda e: e.dma_start(out=lam_sb[:], in_=dlam), writes=[lB])
            P.op("vector", lambda e: e.tensor_tensor(
                out=lprod[:], in0=lam_sb[:].rearrange("p l (a b) d -> p l a b d", b=2)[:, :, :, 0, :],
                in1=lam_sb[:].rearrange("p l (a b) d -> p l a b d", b=2)[:, :, :, 1, :], op=ALU.mult), reads=[lB], writes=[lB2])
            P.op("vector", lambda e: e.tensor_reduce(out=lsum[:], in_=lprod[:], axis=mybir.AxisListType.X, op=ALU.add),
                 reads=[lB2], writes=[lB3])
            P.op("scalar", lambda e: e.activation(out=lsum[:], in_=lsum[:], func=AF.Exp), reads=[lB3], writes=[lB3])
            gn_sb = sb(ph, "gn_sb", [128, DEPTH, 128], F32)
            gB = Buf()
            P.op("sync", lambda e: e.dma_start(out=gn_sb[:], in_=dgain), writes=[gB])
            for l in range(DEPTH):
                lam_init = 0.8 - 0.6 * math.exp(-0.3 * l)
                P.op("vector", lambda e, l=l, lam_init=lam_init: e.scalar_tensor_tensor(
                    out=neglam[:, l:l + 1], in0=lsum[:, l, 1:2], scalar=-lam_init, in1=lsum[:, l, 0:1],
                    op0=ALU.add, op1=ALU.subtract), reads=[lB3], pwrites=[cB])
                P.op("vector", lambda e, l=l, lam_init=lam_init: e.tensor_scalar(
                    out=gainb[:, l, :], in0=gn_sb[:, l, :], scalar1=(1.0 - lam_init), scalar2=None, op0=ALU.mult),
                    reads=[gB], pwrites=[cB])

        def mv(l, col, kind):
            return modv[:, l, col, kind, :]

        def norm_tile(ph_tiles, l, ti, kind0, want_f32=False):
            t0, w = TT[ti]
            col = 1 if ti == 8 else 0
            xt, xtb = ph_tiles["x"].next()
            sq, sqb = ph_tiles["sq"].next()
            rs, rsb = ph_tiles["rs"].next()
            ht, htb = ph_tiles["h"].next()
            P.op("sync", lambda e: e.dma_start(out=xt[:, :, 0:w], in_=xT.rearrange("(kc k) t -> k kc t", k=128)[:, :, t0:t0 + w]),
                 reads=[xb[ti]], writes=[xtb])
            P.op("scalar", lambda e: e.activation(out=sq[:, :, 0:w], in_=xt[:, :, 0:w], func=AF.Square), reads=[xtb], writes=[sqb])
            for kc in range(8):
                P.op("tensor", lambda e, kc=kc: e.matmul(psum[7][:, 0:w], lhsT=onesb[:], rhs=sq[:, kc, 0:w], start=(kc == 0), stop=(kc == 7)),
                     reads=[sqb, cB], writes=[psb[7]])
            P.op("scalar", lambda e: e.activation(out=rs[:, 0:w], in_=psum[7][:, 0:w], func=AF.Ln, scale=1.0 / D, bias=epsc[:, 0:1]),
                 reads=[psb[7], cB], writes=[rsb])
            P.op("scalar", lambda e: e.activation(out=rs[:, 0:w], in_=rs[:, 0:w], func=AF.Exp, scale=-0.5), reads=[rsb], writes=[rsb])
            hf = hfb = None
            if want_f32:
                hf, hfb = ph_tiles["hf"].next()
            for kc in range(8):
                tmp, tmpb = ph_tiles["tmp"].next()
                P.op("vector", lambda e, kc=kc, tmp=tmp: e.scalar_tensor_tensor(
                    out=tmp[:, 0:w], in0=xt[:, kc, 0:w], scalar=mv(l, col, kind0)[:, kc:kc + 1], in1=rs[:, 0:w],
                    op0=ALU.mult, op1=ALU.mult), reads=[xtb, rsb, cB], writes=[tmpb])
                if want_f32:
                    P.op("scalar", lambda e, kc=kc, tmp=tmp: e.activation(
                        out=hf[:, kc, 0:w], in_=tmp[:, 0:w], func=AF.Identity, bias=mv(l, col, kind0 + 1)[:, kc:kc + 1]),
                        reads=[tmpb, cB], pwrites=[hfb])
                    P.op("vector", lambda e, kc=kc: e.tensor_copy(out=ht[:, kc, 0:w], in_=hf[:, kc, 0:w]), reads=[hfb], pwrites=[htb])
                else:
                    P.op("scalar", lambda e, kc=kc, tmp=tmp: e.activation(
                        out=ht[:, kc, 0:w], in_=tmp[:, 0:w], func=AF.Identity, bias=mv(l, col, kind0 + 1)[:, kc:kc + 1]),
                        reads=[tmpb, cB], pwrites=[htb])
            return xt, xtb, ht, htb, hf, hfb

        evac_flip = [0]

        def evac_copy(out_ap, in_ap, reads, writes=(), pwrites=()):
            evac_flip[0] ^= 1
            if evac_flip[0] or os.environ.get("CASTV"):
                P.op("vector", lambda e: e.tensor_copy(out=out_ap, in_=in_ap), reads=reads, writes=writes, pwrites=pwrites)
            else:
                P.op("scalar", lambda e: e.copy(out=out_ap, in_=in_ap), reads=reads, writes=writes, pwrites=pwrites)

        for l in range(layers):
            ctx_out = l < DEPTH - 1
            lam_init = 0.8 - 0.6 * math.exp(-0.3 * l)
            ntt = 9 if True else 8

            if upto >= 1:
              with ExitStack() as ph:
                P.barrier()
                t1r = Rot([sb(ph, f"t1r{i}", [128, 512], F32) for i in range(2)])
                t2r = Rot([sb(ph, f"t2r{i}", [128, 512], F32) for i in range(2)])
                hall = sb(ph, "hall", [128, 8, T], BF16)
                hallb = Buf()
                ropeC = sb(ph, "ropeC", [128, NLAT], F32)
                ropeS = sb(ph, "ropeS", [128, NLAT], F32)
                rB = Buf()
                P.op("sync", lambda e: e.dma_start(out=ropeC[:], in_=ropeC_d), pwrites=[rB])
                P.op("sync", lambda e: e.dma_start(out=ropeS[:], in_=ropeS_d), pwrites=[rB])
                with ExitStack() as ph2:
                    P.barrier()
                    tiles = {
                        "x": Rot([sb(ph2, f"p1x{i}", [128, 8, 512], F32) for i in range(2)]),
                        "sq": Rot([sb(ph2, "p1sq", [128, 8, 512], BF16)]),
                        "rs": Rot([sb(ph2, f"p1rs{i}", [128, 512], F32) for i in range(2)]),
                        "tmp": Rot([sb(ph2, f"p1tmp{i}", [128, 512], F32) for i in range(8)]),
                    }
                    for ti, (t0, w) in enumerate(TT):
                        class _H:
                            @staticmethod
                            def next(t0=t0):
                                return hall[:, :, t0:t0 + 512 if t0 < 4096 else T], hallb
                        tiles["h"] = _H
                        norm_tile(tiles, l, ti, 0)
                P.barrier()
                wst = Rot([sb(ph, f"wst{i}", [128, 8, 512], F32) for i in range(2)])
                wbf = Rot([sb(ph, f"wbf{i}", [128, 8, 512], BF16) for i in range(2)])
                ev = Rot([sb(ph, f"ev{i}", [128, 512], BF16) for i in range(4)])
                xbt = Rot([sb(ph, f"xbt{i}", [128, 512], BF16) for i in range(2)])
                segs = [("fm", 0, 512, 0, "rope"), ("fm", 512, 128, 4, "rope"), ("tm", 640, 128, 0, "copy"),
                        ("fm", 768, 256, 5, "rope"), ("fm", 1024, 256, 7, "rope"), ("tm", 1280, 512, 128, "copy"),
                        ("tm", 1792, 512, 640, "silu"), ("fm", 2304, 512, 9, "rope"), ("fm", 2816, 512, 13, "rope"),
                        ("tm", 3328, 512, 1152, "copy")] + [("fm", 3840 + 512 * i, 512, 17 + 4 * i, "sigmoid") for i in range(6)]
                pi = [0]

                def nps():
                    pi[0] = (pi[0] + 1) % 6
                    return pi[0]
                castflip = 0
                for si, (kind, c0, ncol, d0, mode) in enumerate(segs):
                    if si >= sub:
                        break
                    ws, wsb = wst.next()
                    wb_, wbb = wbf.next()
                    P.op("sync", lambda e, ws=ws, c0=c0, ncol=ncol: e.dma_start(
                        out=ws[:, :, 0:ncol], in_=w_in[l].rearrange("(kc k) n -> k kc n", k=128)[:, :, c0:c0 + ncol]), writes=[wsb])
                    for kc in range(8):
                        castflip ^= 1
                        evac_copy(wb_[:, kc, 0:ncol], ws[:, kc, 0:ncol], [wsb], pwrites=[wbb])
                    if kind == "fm":
                        for cj in range(ncol // 128):
                            for ti, (t0, w) in enumerate(TT):
                                p = nps()
                                for kc in range(8):
                                    P.op("tensor", lambda e, p=p, wb_=wb_, kc=kc, cj=cj, t0=t0, w=w: e.matmul(
                                        psum[p][:, 0:w], lhsT=wb_[:, kc, cj * 128:(cj + 1) * 128], rhs=hall[:, kc, t0:t0 + w],
                                        start=(kc == 0), stop=(kc == 7)), reads=[wbb, hallb], writes=[psb[p]])
                                et, etb = ev.next()
                                dst = FM[(d0 + cj) * 128:(d0 + cj + 1) * 128, t0:t0 + w]
                                if mode == "sigmoid":
                                    P.op("scalar", lambda e, p=p, et=et, w=w: e.activation(out=et[:, 0:w], in_=psum[p][:, 0:w], func=AF.Sigmoid),
                                         reads=[psb[p]], writes=[etb])
                                elif mode == "rope" and ti < 8:
                                    xq, xqb = xbt.next()
                                    t1, t1b = t1r.next()
                                    t2, t2b = t2r.next()
                                    p2 = nps()
                                    RS = int(os.environ.get("ROPE_STAGE", "5"))
                                    P.op("scalar", lambda e, p=p, xq=xq: e.copy(out=xq[:], in_=psum[p][:]), reads=[psb[p]], writes=[xqb])
                                    if RS >= 2:
                                        P.op("vector", lambda e, p=p, t1=t1, t0=t0: e.tensor_tensor(
                                            out=t1[:], in0=psum[p][:], in1=ropeC[:, t0:t0 + 512], op=ALU.mult), reads=[psb[p], rB, xqb], writes=[t1b])
                                    if RS >= 3:
                                        P.op("tensor", lambda e, p2=p2, xq=xq: e.matmul(psum[p2][:], lhsT=rmatT[:], rhs=xq[:], start=True, stop=True),
                                             reads=[xqb, cB], writes=[psb[p2]])
                                    if RS >= 4:
                                        P.op("vector", lambda e, p2=p2, t2=t2, t0=t0: e.tensor_tensor(
                                            out=t2[:], in0=psum[p2][:], in1=ropeS[:, t0:t0 + 512], op=ALU.mult), reads=[psb[p2], rB], writes=[t2b])
                                    if RS >= 5:
                                        P.op("vector", lambda e, et=et, t1=t1, t2=t2: e.tensor_tensor(out=et[:], in0=t1[:], in1=t2[:], op=ALU.add),
                                             reads=[t1b, t2b], writes=[etb])
                                    else:
                                        P.op("vector", lambda e, et=et, xq=xq: e.tensor_copy(out=et[:], in_=xq[:]), reads=[xqb], writes=[etb])
                                else:
                                    evac_copy(et[:, 0:w], psum[p][:, 0:w], [psb[p]], writes=[etb])
                                P.op("pool", lambda e, et=et, dst=dst, w=w: e.dma_start(out=dst, in_=et[:, 0:w]), reads=[etb], pwrites=[FMb])
                    else:
                        for s in range(T // 128):
                            p = nps()
                            for kc in range(8):
                                P.op("tensor", lambda e, p=p, wb_=wb_, kc=kc, s=s, ncol=ncol: e.matmul(
                                    psum[p][:, 0:ncol], lhsT=hall[:, kc, s * 128:(s + 1) * 128], rhs=wb_[:, kc, 0:ncol],
                                    start=(kc == 0), stop=(kc == 7)), reads=[wbb, hallb], writes=[psb[p]])
                            et, etb = ev.next()
                            if mode == "silu":
                                P.op("scalar", lambda e, p=p, et=et, ncol=ncol: e.activation(out=et[:, 0:ncol], in_=psum[p][:, 0:ncol], func=AF.Silu),
                                     reads=[psb[p]], writes=[etb])
                            else:
                                evac_copy(et[:, 0:ncol], psum[p][:, 0:ncol], [psb[p]], writes=[etb])
                            P.op("pool", lambda e, et=et, s=s, d0=d0, ncol=ncol: e.dma_start(
                                out=TM[s * 128:(s + 1) * 128, d0:d0 + ncol], in_=et[:, 0:ncol]), reads=[etb], pwrites=[TMb])

            qblocks = list(range(32)) + ([32, 33] if ctx_out else [])

            def transpose_store(ph_t, src_tile, src_buf, nchunks, row0, s, acc):
                for cc in range(nchunks):
                    P.op("tensor", lambda e, cc=cc: e.transpose(
                        out=ph_t["pst"][:, cc, :], in_=src_tile[:, cc * 128:(cc + 1) * 128], identity=identb[:]),
                        reads=[src_buf, cB], writes=[ph_t["pstb"]] if cc == 0 else (), pwrites=[ph_t["pstb"]] if cc > 0 else ())
                if acc["cnt"] == 0:
                    acc["tile"], acc["buf"] = ph_t["brt"].next()
                    acc["s0"] = s
                k = acc["cnt"]
                tile_, buf_ = acc["tile"], acc["buf"]
                evac_copy(tile_[:, 0:nchunks, k * 128:(k + 1) * 128], ph_t["pst"][:, 0:nchunks, :], [ph_t["pstb"]],
                          writes=[buf_] if k == 0 else (), pwrites=[buf_] if k > 0 else ())
                acc["cnt"] += 1
                last = (s == 31) or (s == 33)
                if acc["cnt"] == 4 or last:
                    n = acc["cnt"]
                    s0 = acc["s0"]
                    for cc in range(nchunks):
                        P.op("pool", lambda e, cc=cc, n=n, s0=s0, tile_=tile_: e.dma_start(
                            out=BR[row0 + cc * 128:row0 + (cc + 1) * 128, s0 * 128:(s0 + n) * 128], in_=tile_[:, cc, 0:n * 128]),
                            reads=[buf_], pwrites=[BRb])
                    acc["cnt"] = 0

            if upto >= 2:
              with ExitStack() as ph:
                P.barrier()
                kA = sb(ph, "kA", [64, T], BF16)
                vA = sb(ph, "vA", [128, 34, 65], BF16)
                qA = sb(ph, "qA", [64, 4, T], BF16)
                wmask = sb(ph, "wmask", [128, 2, 512], BF16)
                wmB = Buf()
                P.op("sync", lambda e: e.dma_start(out=wmask[:], in_=wmask_d), writes=[wmB])
                Er = Rot([sb(ph, f"EA{i}", [128, 512], BF16) for i in range(10)])
                oat = Rot([sb(ph, f"oat{i}", [128, 256], BF16) for i in range(3)])
                den = Rot([sb(ph, f"denA{i}", [128, 4], F32) for i in range(3)])
                ph_t = {"pst": psum[6][:].rearrange("p (c t) -> p c t", t=128)[:, 0:2, :], "pstb": psb[6],
                        "brt": Rot([sb(ph, f"brtA{i}", [128, 2, 512], BF16) for i in range(2)])}
                kvB = Buf()
                for g in range(2):
                    P.op("sync", lambda e, g=g: e.dma_start(out=kA[:], in_=FM[4 * 128 + g * 64:4 * 128 + (g + 1) * 64, :]), reads=[FMb], writes=[kvB])
                    P.op("sync", lambda e, g=g: e.dma_start(out=vA[:, :, 0:64], in_=TM[:, g * 64:(g + 1) * 64].rearrange("(j p) c -> p j c", p=128)),
                         reads=[TMb], pwrites=[kvB])
                    P.op("vector", lambda e: e.memset(vA[:, :, 64:65], 1.0), pwrites=[kvB])
                    for r in range(4):
                        hh = g * 4 + r
                        P.op("sync", lambda e, r=r, hh=hh: e.dma_start(out=qA[:, r, :], in_=FM[hh * 64:(hh + 1) * 64, :]), reads=[FMb], pwrites=[kvB])
                    acc = {"cnt": 0}
                    def stA1(i):
                        if i < 32:
                            keys = ([(i - 1, 0)] if i > 0 else []) + [(i, None)] + ([(i + 1, 1)] if i < 31 else []) + [(32, None), (33, None)]
                        else:
                            keys = [(32, None), (33, None)]
                        Es = []
                        for (j, mk) in keys:
                            p = 0 + (Er.i % 4)
                            P.op("tensor", lambda e, p=p, j=j, i=i: e.matmul(
                                psum[p][:], lhsT=kA[:, j * 128:(j + 1) * 128], rhs=qA[:, :, i * 128:(i + 1) * 128], start=True, stop=True),
                                reads=[kvB], writes=[psb[p]])
                            Et, Eb = Er.next()
                            P.op("scalar", lambda e, p=p, Et=Et: e.activation(out=Et[:], in_=psum[p][:], func=AF.Exp, scale=0.125),
                                 reads=[psb[p]], writes=[Eb])
                            if mk is not None:
                                P.op("vector", lambda e, Et=Et, mk=mk: e.tensor_tensor(out=Et[:], in0=Et[:], in1=wmask[:, mk, :], op=ALU.mult),
                                     reads=[wmB], writes=[Eb])
                            Es.append((j, Et, Eb))
                        return Es

                    def stA2(i, Es):
                        po = 4 + (i % 2)
                        pov = psum[po][:, 0:260].rearrange("p (r c) -> p r c", c=65)
                        for r in range(4):
                            for n_, (j, Et, Eb) in enumerate(Es):
                                P.op("tensor", lambda e, r=r, j=j, Et=Et, n_=n_, pov=pov: e.matmul(
                                    pov[:, r, :], lhsT=Et[:, r * 128:(r + 1) * 128], rhs=vA[:, j, :], start=(n_ == 0), stop=(n_ == len(Es) - 1)),
                                    reads=[Eb, kvB], writes=[psb[po]])
                        dn, dnb = den.next()
                        P.op("vector", lambda e, dn=dn, pov=pov, g=g: e.tensor_tensor(
                            out=dn[:], in0=pov[:, :, 64], in1=esink[:, l * 8 + g * 4:l * 8 + g * 4 + 4], op=ALU.add),
                            reads=[psb[po], cB], writes=[dnb])
                        P.op("vector", lambda e, dn=dn: e.reciprocal(out=dn[:], in_=dn[:]), writes=[dnb])
                        ot, otb = oat.next()
                        for r in range(4):
                            P.op("vector", lambda e, r=r, ot=ot, pov=pov, dn=dn: e.tensor_scalar(
                                out=ot[:, r * 64:(r + 1) * 64], in0=pov[:, r, 0:64], scalar1=dn[:, r:r + 1], scalar2=None, op0=ALU.mult),
                                reads=[psb[po], dnb], writes=[otb] if r == 0 else (), pwrites=[otb] if r > 0 else ())
                        return ot, otb

                    nb = len(qblocks)
                    stash1, stash2 = {}, {}
                    for k in range(nb + 2):
                        if k < nb:
                            stash1[k] = stA1(qblocks[k])
                        if 0 <= k - 1 < nb:
                            stash2[k - 1] = stA2(qblocks[k - 1], stash1.pop(k - 1))
                        if 0 <= k - 2 < nb:
                            ot, otb = stash2.pop(k - 2)
                            transpose_store(ph_t, ot, otb, 2, g * 256, qblocks[k - 2], acc)

            if upto >= 3:
              with ExitStack() as ph:
                P.barrier()
                rt = sb(ph, "rt", [128, 8, 128], F32)
                rtB = Buf()
                P.op("sync", lambda e: e.dma_start(out=rt[:], in_=rettab_d), writes=[rtB])
                qB_ = sb(ph, "qB", [64, T], BF16)
                kB_ = sb(ph, "kB", [64, T], BF16)
                vB_ = sb(ph, "vB", [128, 34, 128], BF16)
                gB_ = sb(ph, "gB", [128, 34, 128], BF16)
                MT = sb(ph, "MT", [128, 128], F32)
                MT2 = sb(ph, "MT2", [128, 128], F32)
                kd = sb(ph, "kd", [128, 4], F32)
                qdf = sb(ph, "qdf", [64, 128], BF16)
                qdb = sb(ph, "qdb", [64, 128], BF16)
                kvF = sb(ph, "kvF", [64, 34, 128], F32)
                kvBk = sb(ph, "kvBk", [64, 34, 128], F32)
                Sin = sb(ph, "Sin", [64, 34, 128], F32)
                Tin = sb(ph, "Tin", [64, 34, 128], F32)
                SinB = sb(ph, "SinB", [64, 34, 128], BF16)
                TinB = sb(ph, "TinB", [64, 34, 128], BF16)
                Kf = Rot([sb(ph, f"Kf{i}", [128, 64], BF16) for i in range(2)])
                Kb = Rot([sb(ph, f"Kb{i}", [128, 64], BF16) for i in range(2)])
                Sm = Rot([sb(ph, f"Sm{i}", [128, 128], BF16) for i in range(3)])
                Qf = Rot([sb(ph, f"Qf{i}", [64, 128], BF16) for i in range(3)])
                Qb = Rot([sb(ph, f"Qb{i}", [64, 128], BF16) for i in range(3)])
                obt = Rot([sb(ph, f"obt{i}", [128, 128], BF16) for i in range(3)])
                ssr = Rot([sb(ph, f"ssr{i}", [128, 2], F32) for i in range(3)])
                junk = sb(ph, "junkB", [128, 128], F32)
                ph_t = {"pst": psum[6][:].rearrange("p (c t) -> p c t", t=128)[:, 0:1, :], "pstb": psb[6],
                        "brt": Rot([sb(ph, f"brtB{i}", [128, 1, 512], BF16) for i in range(2)])}
                ldB, tbB, kvb_, scB = Buf(), Buf(), Buf(), Buf()
                ptbufs = [Buf(), Buf()]
                for h in range(4):
                    ch, half = h // 2, h % 2
                    P.op("sync", lambda e, ch=ch, half=half: e.dma_start(out=qB_[:], in_=FM[(5 + ch) * 128 + half * 64:(5 + ch) * 128 + half * 64 + 64, :]),
                         reads=[FMb], writes=[ldB])
                    P.op("sync", lambda e, ch=ch, half=half: e.dma_start(out=kB_[:], in_=FM[(7 + ch) * 128 + half * 64:(7 + ch) * 128 + half * 64 + 64, :]),
                         reads=[FMb], pwrites=[ldB])
                    P.op("sync", lambda e, h=h: e.dma_start(out=vB_[:], in_=TM[:, 128 + h * 128:128 + (h + 1) * 128].rearrange("(j p) c -> p j c", p=128)),
                         reads=[TMb], pwrites=[ldB])
                    P.op("sync", lambda e, h=h: e.dma_start(out=gB_[:], in_=TM[:, 640 + h * 128:640 + (h + 1) * 128].rearrange("(j p) c -> p j c", p=128)),
                         reads=[TMb], pwrites=[ldB])
                    lf = lgt[:, l * 8 + h:l * 8 + h + 1]
                    lb = lgt[:, l * 8 + 4 + h:l * 8 + 4 + h + 1]
                    P.op("scalar", lambda e, lf=lf: e.activation(out=MT[:], in_=rt[:, 0, :], func=AF.Exp, scale=lf), reads=[rtB, cB], writes=[tbB])
                    P.op("scalar", lambda e, lb=lb: e.activation(out=MT2[:], in_=rt[:, 2, :], func=AF.Exp, scale=lb), reads=[rtB, cB], pwrites=[tbB])
                    P.op("vector", lambda e: e.tensor_tensor(out=MT[:], in0=MT[:], in1=rt[:, 1, :], op=ALU.mult), reads=[tbB, rtB], writes=[tbB])
                    P.op("vector", lambda e: e.tensor_tensor(out=MT2[:], in0=MT2[:], in1=rt[:, 3, :], op=ALU.mult), reads=[tbB], writes=[tbB])
                    P.op("vector", lambda e: e.scalar_tensor_tensor(out=MT[:], in0=MT[:], scalar=0.125, in1=MT2[:], op0=ALU.mult, op1=ALU.add),
                         reads=[tbB], writes=[tbB])
                    P.op("vector", lambda e: e.scalar_tensor_tensor(out=MT[:], in0=MT2[:], scalar=-0.875, in1=MT[:], op0=ALU.mult, op1=ALU.add),
                         reads=[tbB], writes=[tbB])
                    P.op("scalar", lambda e, lf=lf: e.activation(out=kd[:, 0:1], in_=rt[:, 6, 0:1], func=AF.Exp, scale=lf), reads=[tbB], writes=[tbB])
                    P.op("scalar", lambda e, lb=lb: e.activation(out=kd[:, 1:2], in_=rt[:, 6, 1:2], func=AF.Exp, scale=lb), reads=[tbB], writes=[tbB])
                    P.op("scalar", lambda e, lf=lf: e.activation(out=kd[:, 2:3], in_=rt[:, 6, 2:3], func=AF.Exp, scale=lf), reads=[tbB], writes=[tbB])
                    P.op("scalar", lambda e, lb=lb: e.activation(out=kd[:, 3:4], in_=rt[:, 6, 2:3], func=AF.Exp, scale=lb), reads=[tbB], writes=[tbB])
                    P.op("vector", lambda e: e.tensor_scalar(out=kd[:, 0:2], in0=kd[:, 0:2], scalar1=0.125, scalar2=None, op0=ALU.mult), writes=[tbB])
                    P.op("scalar", lambda e, lf=lf: e.activation(out=qdf[:], in_=rt[0:64, 4, :], func=AF.Exp, scale=lf[0:64]), reads=[tbB], writes=[tbB])
                    P.op("scalar", lambda e, lb=lb: e.activation(out=qdb[:], in_=rt[0:64, 5, :], func=AF.Exp, scale=lb[0:64]), reads=[tbB], writes=[tbB])
                    def stP1(n):
                        pt = psum[6][:, 0:64] if n % 2 == 0 else psum[7][:, 0:32].bitcast(BF16)
                        ptb = psb[6] if n % 2 == 0 else psb[7]
                        P.op("tensor", lambda e, n=n, pt=pt: e.transpose(out=pt, in_=kB_[:, n * 128:(n + 1) * 128], identity=identb[0:64, 0:64]),
                             reads=[ldB, cB], writes=[ptb])
                        kf, kfb = Kf.next()
                        kb, kbb = Kb.next()
                        P.op("vector", lambda e, kf=kf, pt=pt: e.tensor_scalar(out=kf[:], in0=pt, scalar1=kd[:, 0:1], scalar2=None, op0=ALU.mult),
                             reads=[ptb, tbB], writes=[kfb])
                        P.op("vector", lambda e, kb=kb, pt=pt: e.tensor_scalar(out=kb[:], in0=pt, scalar1=kd[:, 1:2], scalar2=None, op0=ALU.mult),
                             reads=[ptb, tbB], writes=[kbb])
                        return kf, kfb, kb, kbb

                    def stP2(n, st):
                        kf, kfb, kb, kbb = st
                        pa, pb = n % 2, 2 + n % 2
                        P.op("tensor", lambda e, n=n, kf=kf, pa=pa: e.matmul(psum[pa][0:64, 0:128], lhsT=kf[:], rhs=vB_[:, n, :], start=True, stop=True),
                             reads=[kfb, ldB], writes=[psb[pa]])
                        P.op("tensor", lambda e, n=n, kb=kb, pb=pb: e.matmul(psum[pb][0:64, 0:128], lhsT=kb[:], rhs=vB_[:, n, :], start=True, stop=True),
                             reads=[kbb, ldB], writes=[psb[pb]])
                        P.op("scalar", lambda e, n=n, pa=pa: e.copy(out=kvF[:, n, :], in_=psum[pa][0:64, 0:128]), reads=[psb[pa]], pwrites=[kvb_])
                        P.op("scalar", lambda e, n=n, pb=pb: e.copy(out=kvBk[:, n, :], in_=psum[pb][0:64, 0:128]), reads=[psb[pb]], pwrites=[kvb_])

                    shp = {}
                    for k in range(35):
                        if k < 34:
                            shp[k] = stP1(k)
                        if k >= 1:
                            stP2(k - 1, shp.pop(k - 1))
                    G, Gb = kd[0:64, 2:3], kd[0:64, 3:4]
                    P.op("vector", lambda e: e.memset(Sin[:, 32, :], 0.0), reads=[kvb_], writes=[scB])
                    P.op("vector", lambda e: e.memset(Tin[:, 33, :], 0.0), writes=[scB])
                    P.op("vector", lambda e: e.tensor_copy(out=Sin[:, 33, :], in_=kvF[:, 32, :]), reads=[kvb_], writes=[scB])
                    P.op("vector", lambda e: e.tensor_copy(out=Tin[:, 32, :], in_=kvBk[:, 33, :]), reads=[kvb_], writes=[scB])
                    P.op("vector", lambda e: e.scalar_tensor_tensor(out=Sin[:, 0, :], in0=Sin[:, 33, :], scalar=G, in1=kvF[:, 33, :], op0=ALU.mult, op1=ALU.add),
                         reads=[tbB, kvb_], writes=[scB])
                    P.op("vector", lambda e: e.scalar_tensor_tensor(out=Tin[:, 31, :], in0=Tin[:, 32, :], scalar=Gb, in1=kvBk[:, 32, :], op0=ALU.mult, op1=ALU.add),
                         reads=[kvb_], writes=[scB])
                    for n in range(31):
                        P.op("vector", lambda e, n=n: e.scalar_tensor_tensor(
                            out=Sin[:, n + 1, :], in0=Sin[:, n, :], scalar=G, in1=kvF[:, n, :], op0=ALU.mult, op1=ALU.add), reads=[kvb_], writes=[scB])
                        m = 31 - n
                        P.op("vector", lambda e, m=m: e.scalar_tensor_tensor(
                            out=Tin[:, m - 1, :], in0=Tin[:, m, :], scalar=Gb, in1=kvBk[:, m, :], op0=ALU.mult, op1=ALU.add), reads=[kvb_], writes=[scB])
                    P.op("vector", lambda e: e.tensor_copy(out=SinB[:], in_=Sin[:]), writes=[scB])
                    P.op("vector", lambda e: e.tensor_copy(out=TinB[:], in_=Tin[:]), reads=[scB], pwrites=[scB])
                    scB2 = scB
                    acc = {"cnt": 0}

                    def stB1(n):
                        ps_ = 0 + n % 2
                        P.op("tensor", lambda e, n=n, ps_=ps_: e.matmul(psum[ps_][:, 0:128], lhsT=kB_[:, n * 128:(n + 1) * 128], rhs=qB_[:, n * 128:(n + 1) * 128],
                                                                     start=True, stop=True), reads=[ldB], writes=[psb[ps_]])
                        sm, smb = Sm.next()
                        P.op("vector", lambda e, sm=sm, ps_=ps_: e.tensor_tensor(out=sm[:], in0=psum[ps_][:, 0:128], in1=MT[:], op=ALU.mult),
                             reads=[psb[ps_], tbB], writes=[smb])
                        qf, qfb = Qf.next()
                        qb, qbb = Qb.next()
                        P.op("vector", lambda e, n=n, qf=qf: e.tensor_tensor(out=qf[:], in0=qB_[:, n * 128:(n + 1) * 128], in1=qdf[:], op=ALU.mult),
                             reads=[ldB, tbB], writes=[qfb])
                        P.op("vector", lambda e, n=n, qb=qb: e.tensor_tensor(out=qb[:], in0=qB_[:, n * 128:(n + 1) * 128], in1=qdb[:], op=ALU.mult),
                             reads=[ldB, tbB], writes=[qbb])
                        return (sm, smb, qf, qfb, qb, qbb)

                    def stB2(n, st):
                        sm, smb, qf, qfb, qb, qbb = st
                        po_ = 2 + n % 2
                        P.op("tensor", lambda e, n=n, sm=sm, po_=po_: e.matmul(psum[po_][:, 0:128], lhsT=sm[:], rhs=vB_[:, n, :], start=True, stop=False),
                             reads=[smb, ldB], writes=[psb[po_]])
                        P.op("tensor", lambda e, n=n, qf=qf, po_=po_: e.matmul(psum[po_][:, 0:128], lhsT=qf[:], rhs=SinB[:, n, :], start=False, stop=False),
                             reads=[qfb, scB2], writes=[psb[po_]])
                        P.op("tensor", lambda e, n=n, qb=qb, po_=po_: e.matmul(psum[po_][:, 0:128], lhsT=qb[:], rhs=TinB[:, n, :], start=False, stop=True),
                             reads=[qbb, scB2], writes=[psb[po_]])
                        ss, ssb = ssr.next()
                        P.op("scalar", lambda e, ss=ss, po_=po_: e.activation(out=junk[:], in_=psum[po_][:, 0:128], func=AF.Square, accum_out=ss[:, 0:1]),
                             reads=[psb[po_]], writes=[ssb])
                        P.op("scalar", lambda e, ss=ss: e.activation(out=ss[:, 1:2], in_=ss[:, 0:1], func=AF.Ln, scale=1.0 / 128, bias=epsc[:, 0:1]), writes=[ssb])
                        P.op("scalar", lambda e, ss=ss: e.activation(out=ss[:, 1:2], in_=ss[:, 1:2], func=AF.Exp, scale=-0.5), writes=[ssb])
                        ob, obb = obt.next()
                        P.op("vector", lambda e, n=n, ob=ob, ss=ss, po_=po_: e.scalar_tensor_tensor(
                            out=ob[:], in0=psum[po_][:, 0:128], scalar=ss[:, 1:2], in1=gB_[:, n, :], op0=ALU.mult, op1=ALU.mult),
                            reads=[psb[po_], ssb, ldB], writes=[obb])
                        return ob, obb

                    nb = len(qblocks)
                    sh1, sh2 = {}, {}
                    for k in range(nb + 2):
                        if k < nb:
                            sh1[k] = stB1(qblocks[k])
                        if 0 <= k - 1 < nb:
                            sh2[k - 1] = stB2(qblocks[k - 1], sh1.pop(k - 1))
                        if 0 <= k - 2 < nb:
                            ob, obb = sh2.pop(k - 2)
                            transpose_store(ph_t, ob, obb, 1, 512 + h * 128, qblocks[k - 2], acc)

            if upto >= 4:
              with ExitStack() as ph:
                P.barrier()
                kD = [sb(ph, f"kD{c}", [64, T], BF16) for c in range(2)]
                qD = [sb(ph, f"qD{c}", [64, T], BF16) for c in range(2)]
                vD = sb(ph, "vD", [128, 34, 129], BF16)
                E = [sb(ph, f"ED{c}", [128, 34, 512], BF16) for c in range(2)]
                Ebuf = [Buf(), Buf()]
                t1c = Rot([sb(ph, f"t1c{i}", [128, 128], F32) for i in range(2)])
                occ = Rot([sb(ph, f"occ{i}", [128, 128], F32) for i in range(2)])
                oct_ = Rot([sb(ph, f"oct{i}", [128, 128], BF16) for i in range(2)])
                rcp = Rot([sb(ph, f"rcp{i}", [128, 4], F32) for i in range(2)])
                Osb = [sb(ph, f"Osb{c}", [128, 4, 129], F32) for c in range(2)]
                Osbb = [Buf(), Buf()]
                junk = sb(ph, "junkC", [128, 128], F32)
                ph_t = {"pst": psum[6][:].rearrange("p (c t) -> p c t", t=128)[:, 0:1, :], "pstb": psb[6],
                        "brt": Rot([sb(ph, f"brtC{i}", [128, 1, 512], BF16) for i in range(2)])}
                ldB = Buf()
                for h in range(4):
                    for c in range(2):
                        P.op("sync", lambda e, c=c, h=h: e.dma_start(out=kD[c][:], in_=FM[(13 + h) * 128 + c * 64:(13 + h) * 128 + c * 64 + 64, :]),
                             reads=[FMb], writes=[ldB] if c == 0 else (), pwrites=[ldB] if c else ())
                        P.op("sync", lambda e, c=c, h=h: e.dma_start(out=qD[c][:], in_=FM[(9 + h) * 128 + c * 64:(9 + h) * 128 + c * 64 + 64, :]),
                             reads=[FMb], pwrites=[ldB])
                    P.op("sync", lambda e, h=h: e.dma_start(out=vD[:, :, 0:128], in_=TM[:, 1152 + h * 128:1152 + (h + 1) * 128].rearrange("(j p) c -> p j c", p=128)),
                         reads=[TMb], pwrites=[ldB])
                    P.op("vector", lambda e: e.memset(vD[:, :, 128:129], 1.0), pwrites=[ldB])
                    acc = {"cnt": 0}
                    PVB = [4, 4, 5, 5]
                    pairs = [(ps01, 0, 1), (ps23, 2, 3)]
                    def finish_unit(ti, c):
                        t0, w = TT[ti]
                        nr = w // 128
                        for r in range(nr):
                            P.op("vector", lambda e, r=r, c=c: e.tensor_copy(out=Osb[c][:, r, :], in_=psum[PVB[r]][:, (r % 2) * 129:(r % 2) * 129 + 129]),
                                 reads=[psb[PVB[r]]], writes=[Osbb[c]] if r == 0 else (), pwrites=[Osbb[c]] if r else ())
                        if c == 0:
                            return
                        for r in range(nr):
                            o1 = Osb[0][:, r, :]
                            o2 = Osb[1][:, r, :]
                            rc, rcb = rcp.next()
                            P.op("vector", lambda e, rc=rc, o1=o1: e.reciprocal(out=rc[:, 0:1], in_=o1[:, 128:129]), reads=[Osbb[0]], writes=[rcb])
                            P.op("vector", lambda e, rc=rc, o2=o2: e.reciprocal(out=rc[:, 1:2], in_=o2[:, 128:129]), reads=[Osbb[1]], writes=[rcb])
                            P.op("vector", lambda e, rc=rc: e.tensor_tensor(out=rc[:, 1:2], in0=rc[:, 1:2], in1=neglam[:, l:l + 1], op=ALU.mult),
                                 reads=[cB], writes=[rcb])
                            t1, t1b = t1c.next()
                            oc_, ocb = occ.next()
                            P.op("vector", lambda e, t1=t1, rc=rc, o1=o1: e.tensor_scalar(out=t1[:], in0=o1[:, 0:128], scalar1=rc[:, 0:1], scalar2=None, op0=ALU.mult),
                                 reads=[Osbb[0], rcb], writes=[t1b])
                            P.op("vector", lambda e, t1=t1, rc=rc, o2=o2, oc_=oc_: e.scalar_tensor_tensor(
                                out=oc_[:], in0=o2[:, 0:128], scalar=rc[:, 1:2], in1=t1[:], op0=ALU.mult, op1=ALU.add),
                                reads=[Osbb[1], rcb, t1b], writes=[ocb])
                            P.op("scalar", lambda e, rc=rc, oc_=oc_: e.activation(out=junk[:], in_=oc_[:], func=AF.Square, accum_out=rc[:, 2:3]),
                                 reads=[ocb], writes=[rcb])
                            P.op("scalar", lambda e, rc=rc: e.activation(out=rc[:, 3:4], in_=rc[:, 2:3], func=AF.Ln, scale=1.0 / 128, bias=epsc[:, 0:1]), writes=[rcb])
                            P.op("scalar", lambda e, rc=rc: e.activation(out=rc[:, 3:4], in_=rc[:, 3:4], func=AF.Exp, scale=-0.5), writes=[rcb])
                            ot, otb = oct_.next()
                            P.op("vector", lambda e, ot=ot, oc_=oc_, rc=rc: e.scalar_tensor_tensor(
                                out=ot[:], in0=oc_[:], scalar=rc[:, 3:4], in1=gainb[:, l, :], op0=ALU.mult, op1=ALU.mult),
                                reads=[ocb, rcb, cB], writes=[otb])
                            transpose_store(ph_t, ot, otb, 1, 1024 + h * 128, t0 // 128 + r, acc)


                    pcount = 0
                    for ti, (t0, w) in enumerate(TT):
                        if ti == 8 and not ctx_out:
                            continue
                        keys = list(range(34)) if ti < 8 else [32, 33]
                        nr = w // 128
                        for c in range(2):
                            for jp in range(0, len(keys), 2):
                                pt_, pa_, pb_ = pairs[pcount % 2]
                                pcount += 1
                                for hh, pbk in ((0, pa_), (1, pb_)):
                                    j = keys[jp + hh]
                                    P.op("tensor", lambda e, c=c, j=j, pbk=pbk, t0=t0, w=w: e.matmul(
                                        psum[pbk][:, 0:w], lhsT=kD[c][:, j * 128:(j + 1) * 128], rhs=qD[c][:, t0:t0 + w], start=True, stop=True),
                                        reads=[ldB], writes=[psb[pbk]])
                                j0 = keys[jp]
                                P.op("scalar", lambda e, c=c, j0=j0, pt_=pt_, w=w: e.activation(
                                    out=E[c][:, j0:j0 + 2, 0:w], in_=pt_[:].rearrange("p (a b) -> p a b", b=512)[:, :, 0:w], func=AF.Exp, scale=0.125),
                                    reads=[psb[pa_], psb[pb_]], writes=[Ebuf[c]] if jp == 0 else (), pwrites=[Ebuf[c]] if jp else ())
                            for r in range(nr):
                                pov = psum[PVB[r]][:, (r % 2) * 129:(r % 2) * 129 + 129]
                                for n_, j in enumerate(keys):
                                    P.op("tensor", lambda e, c=c, r=r, j=j, n_=n_, pov=pov: e.matmul(
                                        pov, lhsT=E[c][:, j, r * 128:(r + 1) * 128], rhs=vD[:, j, :], start=(n_ == 0), stop=(n_ == len(keys) - 1)),
                                        reads=[Ebuf[c], ldB], writes=[psb[PVB[r]]])
                            finish_unit(ti, c)

            if upto >= 5:
              with ExitStack() as ph:
                P.barrier()
                wbr = sb(ph, "wbr", [128, 12, D], BF16)
                wo = sb(ph, "wo", [128, 8, D], BF16)
                wB = Buf()
                stg = Rot([sb(ph, f"stgM{i}", [128, D], F32) for i in range(3)])
                cf = 0
                for k in range(20):
                    st_, stb = stg.next()
                    src = w_branch[l].rearrange("i (kc k) n -> k (i kc) n", k=128)[:, k, :] if k < 12 else \
                        w_out[l].rearrange("(kc k) n -> k kc n", k=128)[:, k - 12, :]
                    dstw = wbr[:, k, :] if k < 12 else wo[:, k - 12, :]
                    P.op("sync", lambda e, st_=st_, src=src: e.dma_start(out=st_[:], in_=src), writes=[stb])
                    cf ^= 1
                    evac_copy(dstw, st_[:], [stb], pwrites=[wB])
                brr = Rot([sb(ph, f"brr{i}", [128, 12, 512], BF16) for i in range(2)])
                glr = Rot([sb(ph, f"glr{i}", [128, 24, 512], BF16) for i in range(2)])
                xr = Rot([sb(ph, f"xrM{i}", [128, 8, 512], F32) for i in range(2)])
                yr = Rot([sb(ph, f"yrM{i}", [128, 8, 512], BF16) for i in range(2)])
                ya = Rot([sb(ph, f"yaM{i}", [128, 512], F32) for i in range(2)])
                yb_ = Rot([sb(ph, f"ybM{i}", [128, 512], F32) for i in range(2)])
                pi = [0]

                def nps():
                    pi[0] = (pi[0] + 1) % 6
                    return pi[0]
                for ti, (t0, w) in enumerate(TT):
                    if ti == 8 and not ctx_out:
                        continue
                    col = 1 if ti == 8 else 0
                    br_, brb = brr.next()
                    gl_, glb = glr.next()
                    xt, xtb = xr.next()
                    yt, ytb = yr.next()
                    P.op("sync", lambda e, br_=br_, t0=t0, w=w: e.dma_start(out=br_[:, :, 0:w], in_=BR.rearrange("(c k) t -> k c t", k=128)[:, :, t0:t0 + w]),
                         reads=[BRb], writes=[brb])
                    P.op("sync", lambda e, gl_=gl_, t0=t0, w=w: e.dma_start(out=gl_[:, :, 0:w], in_=FM[17 * 128:41 * 128, :].rearrange("(c k) t -> k c t", k=128)[:, :, t0:t0 + w]),
                         reads=[FMb], writes=[glb])
                    P.op("sync", lambda e, xt=xt, t0=t0, w=w: e.dma_start(out=xt[:, :, 0:w], in_=xT.rearrange("(kc k) t -> k kc t", k=128)[:, :, t0:t0 + w]),
                         reads=[xb[ti]], writes=[xtb])
                    for oc in range(8):
                        yacc, yab = ya.next()
                        for i in range(3):
                            p = nps()
                            for kc in range(4):
                                P.op("tensor", lambda e, p=p, i=i, kc=kc, oc=oc, br_=br_, w=w: e.matmul(
                                    psum[p][:, 0:w], lhsT=wbr[:, i * 4 + kc, oc * 128:(oc + 1) * 128], rhs=br_[:, i * 4 + kc, 0:w], start=(kc == 0), stop=(kc == 3)),
                                    reads=[wB, brb], writes=[psb[p]])
                            if i == 0:
                                P.op("vector", lambda e, p=p, yacc=yacc, gl_=gl_, oc=oc, w=w: e.tensor_tensor(
                                    out=yacc[:, 0:w], in0=psum[p][:, 0:w], in1=gl_[:, oc, 0:w], op=ALU.mult), reads=[psb[p], glb], writes=[yab])
                            else:
                                y2, y2b = yb_.next()
                                P.op("vector", lambda e, p=p, y2=y2, gl_=gl_, oc=oc, i=i, w=w: e.tensor_tensor(
                                    out=y2[:, 0:w], in0=psum[p][:, 0:w], in1=gl_[:, i * 8 + oc, 0:w], op=ALU.mult), reads=[psb[p], glb], writes=[y2b])
                                if i == 1:
                                    P.op("vector", lambda e, yacc=yacc, y2=y2, w=w: e.tensor_tensor(out=yacc[:, 0:w], in0=yacc[:, 0:w], in1=y2[:, 0:w], op=ALU.add),
                                         reads=[y2b], writes=[yab])
                                else:
                                    P.op("vector", lambda e, yacc=yacc, y2=y2, yt=yt, oc=oc, w=w: e.tensor_tensor(
                                        out=yt[:, oc, 0:w], in0=yacc[:, 0:w], in1=y2[:, 0:w], op=ALU.add),
                                        reads=[y2b, yab], writes=[ytb] if oc == 0 else (), pwrites=[ytb] if oc else ())
                    for oc in range(8):
                        p = nps()
                        for kc in range(8):
                            P.op("tensor", lambda e, p=p, kc=kc, oc=oc, yt=yt, w=w: e.matmul(
                                psum[p][:, 0:w], lhsT=wo[:, kc, oc * 128:(oc + 1) * 128], rhs=yt[:, kc, 0:w], start=(kc == 0), stop=(kc == 7)),
                                reads=[wB, ytb], writes=[psb[p]])
                        P.op("vector", lambda e, p=p, oc=oc, xt=xt, col=col, w=w: e.scalar_tensor_tensor(
                            out=xt[:, oc, 0:w], in0=psum[p][:, 0:w], scalar=mv(l, col, 2)[:, oc:oc + 1], in1=xt[:, oc, 0:w], op0=ALU.mult, op1=ALU.add),
                            reads=[psb[p], cB], writes=[xtb])
                    P.op("pool", lambda e, xt=xt, t0=t0, w=w: e.dma_start(out=xT.rearrange("(kc k) t -> k kc t", k=128)[:, :, t0:t0 + w], in_=xt[:, :, 0:w]),
                         reads=[xtb], writes=[xb[ti]])

            is_moe = (l % 2 == 1)
            l2 = l // 2
            if upto >= 6:
              with ExitStack() as ph:
                P.barrier()
                tiles = {
                    "x": Rot([sb(ph, f"n2x{i}", [128, 8, 512], F32) for i in range(2)]),
                    "sq": Rot([sb(ph, "n2sq", [128, 8, 512], BF16)]),
                    "rs": Rot([sb(ph, f"n2rs{i}", [128, 512], F32) for i in range(2)]),
                    "tmp": Rot([sb(ph, f"n2tmp{i}", [128, 512], F32) for i in range(6)]),
                    "h": Rot([sb(ph, f"n2h{i}", [128, 8, 512], BF16) for i in range(2)]),
                    "hf": Rot([sb(ph, f"n2hf{i}", [128, 8, 512], F32) for i in range(2 if is_moe else 0)]),
                }
                if is_moe:
                    wr = sb(ph, "wr", [128, 8, NE], F32)
                    wrB = Buf()
                    P.op("sync", lambda e: e.dma_start(out=wr[:], in_=moe_router[l2].rearrange("(kc k) n -> k kc n", k=128)), writes=[wrB])
                    lg_ = Rot([sb(ph, f"lg{i}", [128, 8], F32) for i in range(2)])
                    mx_ = Rot([sb(ph, f"mx{i}", [128, 8], F32) for i in range(2)])
                    gt_ = Rot([sb(ph, f"gt{i}", [128, 8], F32) for i in range(2)])
                    g2_ = Rot([sb(ph, f"g2{i}", [128, 8], F32) for i in range(2)])
                    gT_ = Rot([sb(ph, f"gT{i}", [8, 512], F32) for i in range(2)])
                for ti, (t0, w) in enumerate(TT):
                    if ti == 8 and not ctx_out:
                        continue
                    xt, xtb, ht, htb, hf, hfb = norm_tile(tiles, l, ti, 3, want_f32=is_moe)
                    P.op("pool", lambda e, ht=ht, t0=t0, w=w: e.dma_start(out=H2.rearrange("(kc k) t -> k kc t", k=128)[:, :, t0:t0 + w], in_=ht[:, :, 0:w]),
                         reads=[htb], pwrites=[H2b])
                    if is_moe:
                        gT, gTb = gT_.next()
                        for s in range(w // 128):
                            for kc in range(8):
                                P.op("tensor", lambda e, s=s, kc=kc: e.matmul(psum[0][:, 0:8], lhsT=hf[:, kc, s * 128:(s + 1) * 128], rhs=wr[:, kc, :],
                                                                             start=(kc == 0), stop=(kc == 7)), reads=[hfb, wrB], writes=[psb[0]])
                            lg, lgb = lg_.next()
                            mx, mxb = mx_.next()
                            gt, gtb = gt_.next()
                            g2, g2b = g2_.next()
                            P.op("vector", lambda e, lg=lg: e.tensor_copy(out=lg[:], in_=psum[0][:, 0:8]), reads=[psb[0]], writes=[lgb])
                            P.op("vector", lambda e, lg=lg, mx=mx: e.max(out=mx[:], in_=lg[:]), reads=[lgb], writes=[mxb])
                            P.op("vector", lambda e, mx=mx: e.tensor_tensor(out=mx[:, 2:3], in0=mx[:, 0:1], in1=mx[:, 1:2], op=ALU.subtract), writes=[mxb])
                            P.op("scalar", lambda e, mx=mx: e.activation(out=mx[:, 3:4], in_=mx[:, 2:3], func=AF.Sigmoid), reads=[mxb], writes=[mxb])
                            P.op("scalar", lambda e, mx=mx: e.activation(out=mx[:, 4:5], in_=mx[:, 2:3], func=AF.Sigmoid, scale=-1.0), writes=[mxb])
                            P.op("vector", lambda e, lg=lg, mx=mx, gt=gt: e.tensor_scalar(
                                out=gt[:], in0=lg[:], scalar1=mx[:, 0:1], scalar2=mx[:, 3:4], op0=ALU.is_equal, op1=ALU.mult), reads=[lgb, mxb], writes=[gtb])
                            P.op("vector", lambda e, lg=lg, mx=mx, g2=g2: e.tensor_scalar(
                                out=g2[:], in0=lg[:], scalar1=mx[:, 1:2], scalar2=mx[:, 4:5], op0=ALU.is_equal, op1=ALU.mult), reads=[lgb, mxb], writes=[g2b])
                            P.op("vector", lambda e, gt=gt, g2=g2: e.tensor_tensor(out=gt[:], in0=gt[:], in1=g2[:], op=ALU.add), reads=[g2b], writes=[gtb])
                            P.op("tensor", lambda e, gt=gt: e.transpose(out=psum[1][0:8, 0:128], in_=gt[:], identity=identf[:]), reads=[gtb, cB], writes=[psb[1]])
                            P.op("vector", lambda e, gT=gT, s=s: e.tensor_copy(out=gT[:, s * 128:(s + 1) * 128], in_=psum[1][0:8, 0:128]),
                                 reads=[psb[1]], writes=[gTb] if s == 0 else (), pwrites=[gTb] if s else ())
                        P.op("pool", lambda e, gT=gT, t0=t0, w=w: e.dma_start(out=GT[:, t0:t0 + w], in_=gT[:, 0:w]), reads=[gTb], pwrites=[GTb])

            if upto >= 7:
              with ExitStack() as ph:
                P.barrier()
                FH = DFF // 2
                w1r = Rot([sb(ph, f"w1h{i}", [128, 8, FH], BF16) for i in range(2)])
                w3r = Rot([sb(ph, f"w3h{i}", [128, 8, FH], BF16) for i in range(2)])
                w2r = Rot([sb(ph, f"w2h{i}", [128, 11, D], BF16) for i in range(2)])
                stg = Rot([sb(ph, f"stgF{i}", [128, FH], F32) for i in range(3)])
                h2r = Rot([sb(ph, f"h2r{i}", [128, 8, 512], BF16) for i in range(2)])
                ur = Rot([sb(ph, f"ur{i}", [128, 11, 512], BF16) for i in range(2)])
                sr = Rot([sb(ph, f"sr{i}", [128, 512], F32) for i in range(2)])
                cr = Rot([sb(ph, f"cr{i}", [128, 512], F32) for i in range(4)])
                gr = Rot([sb(ph, f"gr{i}", [128, 512], F32) for i in range(2)])
                pi = [0]

                def nps():
                    pi[0] = (pi[0] + 1) % 7
                    return [0, 1, 2, 3, 4, 5, 7][pi[0]]
                experts = list(range(NE)) if is_moe else [None]
                pendF = [None]
                cf = 0
                for ex in experts:
                    if ex is None:
                        W1, W3, W2 = ffn_w1[l2], ffn_w3[l2], ffn_w2[l2]
                    else:
                        W1, W3, W2 = moe_w1[l2, ex], moe_w3[l2, ex], moe_w2[l2, ex]
                    for half in range(2):
                        w1h, w1b = w1r.next()
                        w3h, w3b = w3r.next()
                        w2h, w2b = w2r.next()
                        for (Wsrc, wdst, wbuf) in [(W1, w1h, w1b), (W3, w3h, w3b)]:
                            for kc in range(8):
                                st_, stb = stg.next()
                                P.op("sync", lambda e, st_=st_, Wsrc=Wsrc, kc=kc, half=half: e.dma_start(
                                    out=st_[:], in_=Wsrc[kc * 128:(kc + 1) * 128, half * FH:(half + 1) * FH]), writes=[stb])
                                cf ^= 1
                                evac_copy(wdst[:, kc, :], st_[:], [stb], writes=[wbuf] if kc == 0 else (), pwrites=[wbuf] if kc else ())
                        for j in range(11):
                            st_, stb = stg.next()
                            P.op("sync", lambda e, st_=st_, j=j, half=half, W2=W2: e.dma_start(
                                out=st_[:, 0:D], in_=W2[half * FH + j * 128:half * FH + (j + 1) * 128, :]), writes=[stb])
                            cf ^= 1
                            evac_copy(w2h[:, j, :], st_[:, 0:D], [stb], writes=[w2b] if j == 0 else (), pwrites=[w2b] if j else ())
                        def stF1(ti):
                            t0, w = TT[ti]
                            h2t, h2tb = h2r.next()
                            P.op("sync", lambda e, h2t=h2t, t0=t0, w=w: e.dma_start(out=h2t[:, :, 0:w], in_=H2.rearrange("(kc k) t -> k kc t", k=128)[:, :, t0:t0 + w]),
                                 reads=[H2b], writes=[h2tb])
                            gtile = gtb_ = None
                            if ex is not None:
                                gtile, gtb_ = gr.next()
                                P.op("sync", lambda e, gtile=gtile, t0=t0, w=w: e.dma_start(out=gtile[:, 0:w], in_=GT[ex:ex + 1, t0:t0 + w].broadcast_to([128, w])),
                                     reads=[GTb], writes=[gtb_])
                            ut, utb = ur.next()
                            for j in range(11):
                                p1, p3 = nps(), nps()
                                for kc in range(8):
                                    P.op("tensor", lambda e, p1=p1, kc=kc, j=j: e.matmul(
                                        psum[p1][:, 0:w], lhsT=w1h[:, kc, j * 128:(j + 1) * 128], rhs=h2t[:, kc, 0:w], start=(kc == 0), stop=(kc == 7)),
                                        reads=[w1b, h2tb], writes=[psb[p1]])
                                for kc in range(8):
                                    P.op("tensor", lambda e, p3=p3, kc=kc, j=j: e.matmul(
                                        psum[p3][:, 0:w], lhsT=w3h[:, kc, j * 128:(j + 1) * 128], rhs=h2t[:, kc, 0:w], start=(kc == 0), stop=(kc == 7)),
                                        reads=[w3b, h2tb], writes=[psb[p3]])
                                s_, s_b = sr.next()
                                P.op("scalar", lambda e, s_=s_, p1=p1: e.activation(out=s_[:, 0:w], in_=psum[p1][:, 0:w], func=AF.Silu), reads=[psb[p1]], writes=[s_b])
                                P.op("vector", lambda e, s_=s_, p3=p3, j=j: e.tensor_tensor(out=ut[:, j, 0:w], in0=psum[p3][:, 0:w], in1=s_[:, 0:w], op=ALU.mult),
                                     reads=[psb[p3], s_b], writes=[utb] if j == 0 else (), pwrites=[utb] if j else ())
                            return (ti, ut, utb, gtile, gtb_, w2h, w2b, ex)

                        def stF2(st):
                            ti, ut, utb, gtile, gtb_, w2h_, w2b_, ex_ = st
                            t0, w = TT[ti]
                            col = 1 if ti == 8 else 0
                            for oc in range(8):
                                p = nps()
                                for j in range(11):
                                    P.op("tensor", lambda e, p=p, j=j, oc=oc: e.matmul(
                                        psum[p][:, 0:w], lhsT=w2h_[:, j, oc * 128:(oc + 1) * 128], rhs=ut[:, j, 0:w], start=(j == 0), stop=(j == 10)),
                                        reads=[w2b_, utb], writes=[psb[p]])
                                ct, ctb = cr.next()
                                if ex_ is None:
                                    P.op("scalar", lambda e, ct=ct, p=p, oc=oc: e.activation(
                                        out=ct[:, 0:w], in_=psum[p][:, 0:w], func=AF.Identity, scale=mv(l, col, 5)[:, oc:oc + 1]), reads=[psb[p], cB], writes=[ctb])
                                else:
                                    P.op("vector", lambda e, ct=ct, p=p, oc=oc: e.scalar_tensor_tensor(
                                        out=ct[:, 0:w], in0=psum[p][:, 0:w], scalar=mv(l, col, 5)[:, oc:oc + 1], in1=gtile[:, 0:w], op0=ALU.mult, op1=ALU.mult),
                                        reads=[psb[p], cB, gtb_], writes=[ctb])
                                P.op("pool", lambda e, ct=ct, oc=oc: e.dma_start(
                                    out=xT[oc * 128:(oc + 1) * 128, t0:t0 + w], in_=ct[:, 0:w], accum_op=ALU.add), reads=[ctb], pwrites=[xb[ti]])

                        for ti in range(len(TT)):
                            if ti == 8 and not ctx_out:
                                continue
                            st_new = stF1(ti)
                            if pendF[0] is not None:
                                stF2(pendF[0])
                            pendF[0] = st_new
                if pendF[0] is not None:
                    stF2(pendF[0])
                    pendF[0] = None

        if upto >= 8:
          with ExitStack() as ph:
            P.barrier()
            xr = Rot([sb(ph, f"fx{i}", [128, 8, 512], F32) for i in range(2)])
            sqr = Rot([sb(ph, "fsq", [128, 8, 512], BF16)])
            rsr = Rot([sb(ph, f"frs{i}", [128, 512], F32) for i in range(2)])
            for ti in range(8):
                t0, w = TT[ti]
                xt, xtb = xr.next()
                sq, sqb = sqr.next()
                rs, rsb = rsr.next()
                P.op("sync", lambda e, xt=xt, t0=t0: e.dma_start(out=xt[:], in_=xT.rearrange("(kc k) t -> k kc t", k=128)[:, :, t0:t0 + 512]),
                     reads=[xb[ti]], writes=[xtb])
                P.op("scalar", lambda e, xt=xt, sq=sq: e.activation(out=sq[:], in_=xt[:], func=AF.Square), reads=[xtb], writes=[sqb])
                for kc in range(8):
                    P.op("tensor", lambda e, kc=kc, sq=sq: e.matmul(psum[7][:], lhsT=onesb[:], rhs=sq[:, kc, :], start=(kc == 0), stop=(kc == 7)),
                         reads=[sqb, cB], writes=[psb[7]])
                P.op("scalar", lambda e, rs=rs: e.activation(out=rs[:], in_=psum[7][:], func=AF.Ln, scale=1.0 / D, bias=epsc[:, 0:1]), reads=[psb[7], cB], writes=[rsb])
                P.op("scalar", lambda e, rs=rs: e.activation(out=rs[:], in_=rs[:], func=AF.Exp, scale=-0.5), writes=[rsb])
                for kc in range(8):
                    P.op("vector", lambda e, kc=kc, xt=xt, rs=rs: e.scalar_tensor_tensor(
                        out=xt[:, kc, :], in0=xt[:, kc, :], scalar=nfin_sb[:, kc:kc + 1], in1=rs[:], op0=ALU.mult, op1=ALU.mult),
                        reads=[rsb, cB], writes=[xtb])
                P.op("pool", lambda e, xt=xt, t0=t0: e.dma_start(out=outT.rearrange("(kc k) t -> k kc t", k=128)[:, :, t0:t0 + 512], in_=xt[:]),
                     reads=[xtb], pwrites=[outb])
        P.finish([outb, FMb, TMb, BRb, H2b, GTb] + xb)
        print(f"[build] ops={P.n_ops} waits={P.n_waits}")
    return nc


_CONSTS = None


def _prep(inputs, ncores=8):
    global _CONSTS
    if _CONSTS is None:
        _CONSTS = _consts()
    f32 = lambda a: np.ascontiguousarray(np.asarray(a, dtype=np.float32))
    x, c, ctx, c_ctx = f32(inputs["x"]), f32(inputs["c"]), f32(inputs["ctx"]), f32(inputs["c_ctx"])

    def pk(v):
        v = f32(v)
        lead = v.shape[:-1]
        return np.ascontiguousarray(np.moveaxis(v.reshape(lead + (8, 128)), -1, 0))
    shared = {
        "w_mod": f32(inputs["w_mod"]),
        "b_mod": np.ascontiguousarray(np.moveaxis(f32(inputs["b_mod"]).reshape(DEPTH, 48, 128), -1, 0)),
        "nmix": pk(inputs["norm_mix"]), "nffn": pk(inputs["norm_ffn"]), "nfin": pk(inputs["norm_final"]),
        "w_in": f32(inputs["w_in"]),
        "sink": np.ascontiguousarray(np.broadcast_to(f32(inputs["attn_sink"]).reshape(1, -1), (128, DEPTH * 8))),
        "decs": np.ascontiguousarray(np.broadcast_to(
            np.stack([f32(inputs["ret_decay_fwd"]), f32(inputs["ret_decay_bwd"])], 1).reshape(1, -1), (128, DEPTH * 8))),
        "dlam": np.ascontiguousarray(np.broadcast_to(f32(inputs["diff_lambda"])[None], (128, DEPTH, 4, 64))),
        "dgain": np.ascontiguousarray(np.broadcast_to(f32(inputs["diff_norm"])[None], (128, DEPTH, 128))),
        "w_branch": f32(inputs["w_branch"]), "w_out": f32(inputs["w_out"]),
        "ffn_w1": f32(inputs["ffn_w1"]), "ffn_w3": f32(inputs["ffn_w3"]), "ffn_w2": f32(inputs["ffn_w2"]),
        "moe_router": f32(inputs["moe_router"]),
        "moe_w1": f32(inputs["moe_w1"]), "moe_w3": f32(inputs["moe_w3"]), "moe_w2": f32(inputs["moe_w2"]),
    }
    shared.update(_CONSTS)
    maps = []
    for b in range(ncores):
        m = dict(shared)
        m["xT_in"] = np.ascontiguousarray(np.concatenate([x[b], ctx[b]], 0).T)
        c2 = np.stack([c[b], c_ctx], -1)
        m["c2"] = np.ascontiguousarray(c2.reshape(8, 128, 2).transpose(1, 0, 2))
        maps.append(m)
    return maps


_NC = None


def kernel(**inputs):
    global _NC
    if _NC is None:
        _NC = build()
    maps = _prep(inputs)
    res = run_bass_kernel_spmd(_NC, maps, core_ids=list(range(8)))
    out = np.stack([np.ascontiguousarray(res.results[b]["outT"].T) for b in range(8)], 0)
    return out.astype(np.float32)
```

```python
import math
import os
from contextlib import ExitStack
import numpy as np
import ml_dtypes
import concourse.bass as bass
import concourse.mybir as mybir
from concourse.bass_utils import run_bass_kernel_spmd

F32 = mybir.dt.float32
BF16 = mybir.dt.bfloat16
AF = mybir.ActivationFunctionType
ALU = mybir.AluOpType

T = 4352
NLAT = 4096
NCTX = 256
D = 1024
DEPTH = 4
DIN = 6912
DFF = 2816
NE = 8
EPS = 1e-6
TT = [(i * 512, 512) for i in range(8)] + [(4096, 256)]
NFM = 41
NTM = 1664


class Buf:
    __slots__ = ("w", "r", "rp")

    def __init__(self):
        self.w = {}
        self.r = {}
        self.rp = {}


class EngState:
    def __init__(self, name, handle, sem, inc, stream):
        self.name, self.h, self.sem, self.inc, self.stream = name, handle, sem, inc, stream
        self.count = 0


class Prog:
    def __init__(self, nc, stack, nq=4):
        self.nc = nc
        self.engs = {}
        for name in ["tensor", "vector", "scalar", "gpsimd"]:
            sem = stack.enter_context(nc.semaphore("s_" + name))
            self.engs[name] = EngState(name, getattr(nc, name), sem, 1, name)
        self.dmaq = {}
        for qname, issuer, stream in [("sync", nc.sync, "sync"), ("pool", nc.gpsimd, "gpsimd"), ("actq", nc.scalar, "scalar")]:
            lst = []
            for i in range(nq):
                sem = stack.enter_context(nc.semaphore(f"d_{qname}{i}"))
                st = EngState(f"{qname}{i}", issuer, sem, 16, stream)
                lst.append(st)
                self.engs[st.name] = st
            self.dmaq[qname] = [lst, 0]
        self.known = {s: {} for s in ["tensor", "vector", "scalar", "gpsimd", "sync"]}
        self.n_ops = 0
        self.n_waits = 0

    def op(self, eng, fn, reads=(), writes=(), pwrites=()):
        if eng in self.dmaq:
            lst, k = self.dmaq[eng]
            st = lst[k % len(lst)]
            self.dmaq[eng][1] = k + 1
            is_dma = True
        else:
            st = self.engs[eng]
            is_dma = False
        deps = {}

        def add(d):
            for e, i in d.items():
                if deps.get(e, -1) < i:
                    deps[e] = i
        for b in reads:
            add(b.w)
        for b in writes:
            add(b.w)
            add(b.r)
            add(b.rp)
        for b in pwrites:
            if b.r:
                b.rp = dict(b.r)
                b.r = {}
                b.w = {}
            add(b.rp)
        if is_dma and st.count > 0:
            deps[st.name] = max(deps.get(st.name, -1), st.count - 1)
        known = self.known[st.stream]
        for e, i in deps.items():
            if (not is_dma) and e == st.name and e == "tensor":
                continue
            if known.get(e, -1) >= i:
                continue
            es = self.engs[e]
            st.h.wait_ge(es.sem, (i + 1) * es.inc)
            known[e] = i
            self.n_waits += 1
        ins = fn(st.h)
        ins.then_inc(st.sem, st.inc)
        idx = st.count
        st.count += 1
        self.n_ops += 1
        for b in reads:
            if b.r.get(st.name, -1) < idx:
                b.r[st.name] = idx
        for b in writes:
            b.w = {st.name: idx}
            b.r = {}
            b.rp = {}
        for b in pwrites:
            b.w[st.name] = idx
        return idx

    def barrier(self):
        hs = {"tensor": self.nc.tensor, "vector": self.nc.vector, "scalar": self.nc.scalar, "gpsimd": self.nc.gpsimd, "sync": self.nc.sync}
        for sname, h in hs.items():
            known = self.known[sname]
            for e, es in self.engs.items():
                if es.count == 0 or known.get(e, -1) >= es.count - 1:
                    continue
                if e == sname and e == "tensor":
                    continue
                h.wait_ge(es.sem, es.count * es.inc)
                known[e] = es.count - 1
                self.n_waits += 1

    def finish(self, bufs):
        for b in bufs:
            for e, i in b.w.items():
                es = self.engs[e]
                self.nc.sync.wait_ge(es.sem, (i + 1) * es.inc)


class Rot:
    def __init__(self, tiles):
        self.tiles = tiles
        self.bufs = [Buf() for _ in tiles]
        self.i = 0

    def next(self):
        k = self.i % len(self.tiles)
        self.i += 1
        return self.tiles[k], self.bufs[k]


def _consts():
    c = {}
    rows = NLAT // 64
    row = np.repeat(np.arange(rows, dtype=np.float32), 64)
    col = np.tile(np.arange(64, dtype=np.float32), rows)
    inv = (1.0 / (np.float32(10000.0) ** (np.arange(16, dtype=np.float32) / np.float32(16)))).astype(np.float32)
    ang = np.stack([row[:, None] * inv, col[:, None] * inv], axis=1).astype(np.float32)
    cos, sin = np.cos(ang).astype(np.float32), np.sin(ang).astype(np.float32)
    C = np.zeros((64, NLAT), np.float32)
    S = np.zeros((64, NLAT), np.float32)
    for d in range(64):
        ax, f = d // 32, d % 16
        C[d] = cos[:, ax, f]
        S[d] = sin[:, ax, f]
    c["ropeC"] = np.concatenate([C, C], 0)
    c["ropeS"] = np.concatenate([S, S], 0)
    R = np.zeros((64, 64), np.float32)
    for m in range(64):
        if (m % 32) < 16:
            R[m, m + 16] = -1.0
        else:
            R[m, m - 16] = 1.0
    R2 = np.zeros((128, 128), np.float32)
    R2[:64, :64] = R
    R2[64:, 64:] = R
    c["rmatT"] = R2.T.copy().astype(ml_dtypes.bfloat16)
    c["identb"] = np.eye(128, dtype=np.float32).astype(ml_dtypes.bfloat16)
    c["identf"] = np.eye(128, dtype=np.float32)
    c["onesb"] = np.ones((128, 128), np.float32).astype(ml_dtypes.bfloat16)
    k = np.arange(128)[:, None].astype(np.float32)
    q = np.arange(128)[None, :].astype(np.float32)
    ret = np.zeros((128, 8, 128), np.float32)
    ret[:, 0] = np.maximum(q - k, 0)
    ret[:, 1] = (q >= k)
    ret[:, 2] = np.maximum(k - q, 0)
    ret[:, 3] = (k >= q)
    ret[:, 4] = q + 1.0
    ret[:, 5] = 128.0 - q
    ret[:, 6, 0] = 127.0 - k[:, 0]
    ret[:, 6, 1] = k[:, 0]
    ret[:, 6, 2] = 128.0
    c["rettab"] = ret
    mprev = (k >= q).astype(np.float32)
    mnext = (k <= q).astype(np.float32)
    c["wmask"] = np.stack([np.tile(mprev, (1, 4)), np.tile(mnext, (1, 4))], 1).astype(ml_dtypes.bfloat16)
    return c


def build(dbg=False, upto=99, layers=DEPTH, sub=99):
    nc = bass.Bass("TRN2", target_bir_lowering=False)

    def din(name, shape, dt=F32):
        return nc.dram_tensor(name, list(shape), dt, kind="ExternalInput").ap()

    def dscr(name, shape, dt):
        return nc.dram_tensor(name, list(shape), dt, kind=("ExternalOutput" if dbg else "Internal")).ap()

    xT_in = din("xT_in", [D, T])
    c2 = din("c2", [128, 8, 2])
    w_mod = din("w_mod", [DEPTH, D, 6 * D])
    b_mod = din("b_mod", [128, DEPTH, 48])
    nmix = din("nmix", [128, DEPTH, 8])
    nffn = din("nffn", [128, DEPTH, 8])
    nfin = din("nfin", [128, 8])
    w_in = din("w_in", [DEPTH, D, DIN])
    sink = din("sink", [128, DEPTH * 8])
    decs = din("decs", [128, DEPTH * 8])
    dlam = din("dlam", [128, DEPTH, 4, 64])
    dgain = din("dgain", [128, DEPTH, 128])
    w_branch = din("w_branch", [DEPTH, 3, 512, D])
    w_out = din("w_out", [DEPTH, D, D])
    ffn_w1 = din("ffn_w1", [2, D, DFF])
    ffn_w3 = din("ffn_w3", [2, D, DFF])
    ffn_w2 = din("ffn_w2", [2, DFF, D])
    if layers >= 2:
        moe_router = din("moe_router", [2, D, NE])
        moe_w1 = din("moe_w1", [2, NE, D, DFF])
        moe_w3 = din("moe_w3", [2, NE, D, DFF])
        moe_w2 = din("moe_w2", [2, NE, DFF, D])
    ropeC_d = din("ropeC", [128, NLAT])
    ropeS_d = din("ropeS", [128, NLAT])
    rmatT_d = din("rmatT", [128, 128], BF16)
    identb_d = din("identb", [128, 128], BF16)
    identf_d = din("identf", [128, 128])
    onesb_d = din("onesb", [128, 128], BF16)
    rettab_d = din("rettab", [128, 8, 128])
    wmask_d = din("wmask", [128, 2, 512], BF16)

    outT = nc.dram_tensor("outT", [D, NLAT], F32, kind="ExternalOutput").ap()
    xT = dscr("xT", [D, T], F32)
    FM = dscr("FM", [NFM * 128, T], BF16)
    TM = dscr("TM", [T, NTM], BF16)
    BR = dscr("BR", [1536, T], BF16)
    H2 = dscr("H2", [D, T], BF16)
    GT = dscr("GT", [NE, T], F32)

    with ExitStack() as top:
        P = Prog(nc, top)
        ps01 = top.enter_context(nc.psum_tensor("ps01", [128, 1024], F32))
        ps23 = top.enter_context(nc.psum_tensor("ps23", [128, 1024], F32))
        psum = [ps01[:, 0:512], ps01[:, 512:1024], ps23[:, 0:512], ps23[:, 512:1024]]
        for i in range(4, 8):
            psum.append(top.enter_context(nc.psum_tensor(f"ps{i}", [128, 1024], BF16) if i == 6 else nc.psum_tensor(f"ps{i}", [128, 512], F32)))
        psb = [Buf() for _ in range(8)]

        uid = [0]

        def sb(stack, name, shape, dt):
            uid[0] += 1
            return stack.enter_context(nc.sbuf_tensor(f"{name}_s{uid[0]}", list(shape), dt))

        identb = sb(top, "identb", [128, 128], BF16)
        identf = sb(top, "identf", [128, 128], F32)
        onesb = sb(top, "onesb", [128, 128], BF16)
        rmatT = sb(top, "rmatT", [128, 128], BF16)
        modv = sb(top, "modv", [128, DEPTH, 2, 6, 8], F32)
        lgt = sb(top, "lgt", [128, DEPTH * 8], F32)
        esink = sb(top, "esink", [128, DEPTH * 8], F32)
        neglam = sb(top, "neglam", [128, DEPTH], F32)
        gainb = sb(top, "gainb", [128, DEPTH, 128], F32)
        nfin_sb = sb(top, "nfin_sb", [128, 8], F32)
        epsc = sb(top, "epsc", [128, 1], F32)
        cB = Buf()
        xb = [Buf() for _ in TT]
        FMb, TMb, BRb, H2b, GTb, outb = Buf(), Buf(), Buf(), Buf(), Buf(), Buf()

        for dst, src in [(identb, identb_d), (identf, identf_d), (onesb, onesb_d), (rmatT, rmatT_d), (nfin_sb, nfin)]:
            P.op("sync", lambda e, dst=dst, src=src: e.dma_start(out=dst[:], in_=src), pwrites=[cB])
        P.op("vector", lambda e: e.memset(epsc[:], EPS), pwrites=[cB])

        with ExitStack() as ph:
            P.barrier()
            xcp = Rot([sb(ph, f"xcp{i}", [128, 8, 512], F32) for i in range(2)])
            for ti, (t0, w) in enumerate(TT):
                tl, tb = xcp.next()
                P.op("sync", lambda e, tl=tl, t0=t0, w=w: e.dma_start(
                    out=tl[:, :, 0:w], in_=xT_in.rearrange("(kc k) t -> k kc t", k=128)[:, :, t0:t0 + w]), writes=[tb])
                P.op("pool", lambda e, tl=tl, t0=t0, w=w: e.dma_start(
                    out=xT.rearrange("(kc k) t -> k kc t", k=128)[:, :, t0:t0 + w], in_=tl[:, :, 0:w]), reads=[tb], writes=[xb[ti]])

            c_sb = sb(ph, "c_sb", [128, 8, 2], F32)
            cs_sb = sb(ph, "cs_sb", [128, 8, 2], F32)
            bmod_sb = sb(ph, "bmod_sb", [128, DEPTH, 48], F32)
            nmix_sb = sb(ph, "nmix_sb", [128, DEPTH, 8], F32)
            nffn_sb = sb(ph, "nffn_sb", [128, DEPTH, 8], F32)
            modT = sb(ph, "modT", [128, 48, 2], F32)
            sB, mB = Buf(), Buf()
            for dst, src in [(c_sb, c2), (bmod_sb, b_mod), (nmix_sb, nmix), (nffn_sb, nffn)]:
                P.op("sync", lambda e, dst=dst, src=src: e.dma_start(out=dst[:], in_=src), pwrites=[sB])
            P.op("scalar", lambda e: e.activation(out=cs_sb[:], in_=c_sb[:], func=AF.Silu), reads=[sB], writes=[mB])
            wm = Rot([sb(ph, f"wm{i}", [128, 8, 512], F32) for i in range(2)])
            for l in range(layers):
                for n in range(12):
                    wt, wb = wm.next()
                    P.op("sync", lambda e, wt=wt, l=l, n=n: e.dma_start(
                        out=wt[:], in_=w_mod[l].rearrange("(kc k) n -> k kc n", k=128)[:, :, n * 512:(n + 1) * 512]), writes=[wb])
                    for jj in range(4):
                        j = n * 4 + jj
                        for kc in range(8):
                            P.op("tensor", lambda e, wt=wt, jj=jj, kc=kc, j=j: e.matmul(
                                psum[0][:, 2 * j:2 * j + 2], lhsT=wt[:, kc, jj * 128:(jj + 1) * 128], rhs=cs_sb[:, kc, :],
                                start=(kc == 0), stop=(kc == 7)), reads=[wb, mB], writes=[psb[0]])
                mtB = Buf()
                for col in range(2):
                    P.op("vector", lambda e, col=col, l=l: e.tensor_tensor(
                        out=modT[:, :, col], in0=psum[0][:, 0:96].rearrange("p (j c) -> p j c", c=2)[:, :, col],
                        in1=bmod_sb[:, l, :], op=ALU.add), reads=[psb[0], sB], pwrites=[mtB])
                for col in range(2):
                    for kind, (src0, nrm) in enumerate([(8, nmix_sb), (0, None), (16, None), (32, nffn_sb), (24, None), (40, None)]):
                        if nrm is not None:
                            P.op("vector", lambda e, col=col, l=l, kind=kind, src0=src0, nrm=nrm: e.scalar_tensor_tensor(
                                out=modv[:, l, col, kind, :], in0=modT[:, src0:src0 + 8, col], scalar=1.0, in1=nrm[:, l, :],
                                op0=ALU.add, op1=ALU.mult), reads=[mtB, sB], pwrites=[cB])
                        else:
                            P.op("vector", lambda e, col=col, l=l, kind=kind, src0=src0: e.tensor_copy(
                                out=modv[:, l, col, kind, :], in_=modT[:, src0:src0 + 8, col]), reads=[mtB], pwrites=[cB])
            dtmp = sb(ph, "dtmp", [128, DEPTH * 8], F32)
            dtmp2 = sb(ph, "dtmp2", [128, DEPTH * 8], F32)
            dB, dB2 = Buf(), Buf()
            P.op("sync", lambda e: e.dma_start(out=dtmp[:], in_=decs), writes=[dB])
            P.op("scalar", lambda e: e.activation(out=dtmp2[:], in_=dtmp[:], func=AF.Exp, scale=-1.0), reads=[dB], writes=[dB2])
            P.op("scalar", lambda e: e.activation(out=dtmp2[:], in_=dtmp2[:], func=AF.Ln, bias=1.0), reads=[dB2], writes=[dB2])
            P.op("vector", lambda e: e.tensor_scalar(out=lgt[:], in0=dtmp2[:], scalar1=-1.0, scalar2=None, op0=ALU.mult),
                 reads=[dB2], pwrites=[cB])
            stmp = sb(ph, "stmp", [128, DEPTH * 8], F32)
            sB2 = Buf()
            P.op("sync", lambda e: e.dma_start(out=stmp[:], in_=sink), writes=[sB2])
            P.op("scalar", lambda e: e.activation(out=esink[:], in_=stmp[:], func=AF.Exp), reads=[sB2], pwrites=[cB])
            lam_sb = sb(ph, "lam_sb", [128, DEPTH, 4, 64], F32)
            lprod = sb(ph, "lprod", [128, DEPTH, 2, 64], F32)
            lsum = sb(ph, "lsum", [128, DEPTH, 2], F32)
            lB, lB2, lB3 = Buf(), Buf(), Buf()
            P.op("sync", lambda e: e.dma_start(out=lam_sb[:], in_=dlam), writes=[lB])
            P.op("vector", lambda e: e.tensor_tensor(
                out=lprod[:], in0=lam_sb[:].rearrange("p l (a b) d -> p l a b d", b=2)[:, :, :, 0, :],
                in1=lam_sb[:].rearrange("p l (a b) d -> p l a b d", b=2)[:, :, :, 1, :], op=ALU.mult), reads=[lB], writes=[lB2])
            P.op("vector", lambda e: e.tensor_reduce(out=lsum[:], in_=lprod[:], axis=mybir.AxisListType.X, op=ALU.add),
                 reads=[lB2], writes=[lB3])
            P.op("scalar", lambda e: e.activation(out=lsum[:], in_=lsum[:], func=AF.Exp), reads=[lB3], writes=[lB3])
            gn_sb = sb(ph, "gn_sb", [128, DEPTH, 128], F32)
            gB = Buf()
            P.op("sync", lambda e: e.dma_start(out=gn_sb[:], in_=dgain), writes=[gB])
            for l in range(DEPTH):
                lam_init = 0.8 - 0.6 * math.exp(-0.3 * l)
                P.op("vector", lambda e, l=l, lam_init=lam_init: e.scalar_tensor_tensor(
                    out=neglam[:, l:l + 1], in0=lsum[:, l, 1:2], scalar=-lam_init, in1=lsum[:, l, 0:1],
                    op0=ALU.add, op1=ALU.subtract), reads=[lB3], pwrites=[cB])
                P.op("vector", lambda e, l=l, lam_init=lam_init: e.tensor_scalar(
                    out=gainb[:, l, :], in0=gn_sb[:, l, :], scalar1=(1.0 - lam_init), scalar2=None, op0=ALU.mult),
                    reads=[gB], pwrites=[cB])

        def mv(l, col, kind):
            return modv[:, l, col, kind, :]

        def norm_tile(ph_tiles, l, ti, kind0, want_f32=False):
            t0, w = TT[ti]
            col = 1 if ti == 8 else 0
            xt, xtb = ph_tiles["x"].next()
            sq, sqb = ph_tiles["sq"].next()
            rs, rsb = ph_tiles["rs"].next()
            ht, htb = ph_tiles["h"].next()
            P.op("sync", lambda e: e.dma_start(out=xt[:, :, 0:w], in_=xT.rearrange("(kc k) t -> k kc t", k=128)[:, :, t0:t0 + w]),
                 reads=[xb[ti]], writes=[xtb])
            P.op("scalar", lambda e: e.activation(out=sq[:, :, 0:w], in_=xt[:, :, 0:w], func=AF.Square), reads=[xtb], writes=[sqb])
            for kc in range(8):
                P.op("tensor", lambda e, kc=kc: e.matmul(psum[7][:, 0:w], lhsT=onesb[:], rhs=sq[:, kc, 0:w], start=(kc == 0), stop=(kc == 7)),
                     reads=[sqb, cB], writes=[psb[7]])
            P.op("scalar", lambda e: e.activation(out=rs[:, 0:w], in_=psum[7][:, 0:w], func=AF.Ln, scale=1.0 / D, bias=epsc[:, 0:1]),
                 reads=[psb[7], cB], writes=[rsb])
            P.op("scalar", lambda e: e.activation(out=rs[:, 0:w], in_=rs[:, 0:w], func=AF.Exp, scale=-0.5), reads=[rsb], writes=[rsb])
            hf = hfb = None
            if want_f32:
                hf, hfb = ph_tiles["hf"].next()
            for kc in range(8):
                tmp, tmpb = ph_tiles["tmp"].next()
                P.op("vector", lambda e, kc=kc, tmp=tmp: e.scalar_tensor_tensor(
                    out=tmp[:, 0:w], in0=xt[:, kc, 0:w], scalar=mv(l, col, kind0)[:, kc:kc + 1], in1=rs[:, 0:w],
                    op0=ALU.mult, op1=ALU.mult), reads=[xtb, rsb, cB], writes=[tmpb])
                if want_f32:
                    P.op("scalar", lambda e, kc=kc, tmp=tmp: e.activation(
                        out=hf[:, kc, 0:w], in_=tmp[:, 0:w], func=AF.Identity, bias=mv(l, col, kind0 + 1)[:, kc:kc + 1]),
                        reads=[tmpb, cB], pwrites=[hfb])
                    P.op("vector", lambda e, kc=kc: e.tensor_copy(out=ht[:, kc, 0:w], in_=hf[:, kc, 0:w]), reads=[hfb], pwrites=[htb])
                else:
                    P.op("scalar", lambda e, kc=kc, tmp=tmp: e.activation(
                        out=ht[:, kc, 0:w], in_=tmp[:, 0:w], func=AF.Identity, bias=mv(l, col, kind0 + 1)[:, kc:kc + 1]),
                        reads=[tmpb, cB], pwrites=[htb])
            return xt, xtb, ht, htb, hf, hfb

        evac_flip = [0]

        def evac_copy(out_ap, in_ap, reads, writes=(), pwrites=()):
            evac_flip[0] ^= 1
            if evac_flip[0] or os.environ.get("CASTV"):
                P.op("vector", lambda e: e.tensor_copy(out=out_ap, in_=in_ap), reads=reads, writes=writes, pwrites=pwrites)
            else:
                P.op("scalar", lambda e: e.copy(out=out_ap, in_=in_ap), reads=reads, writes=writes, pwrites=pwrites)

        for l in range(layers):
            ctx_out = l < DEPTH - 1
            lam_init = 0.8 - 0.6 * math.exp(-0.3 * l)
            ntt = 9 if True else 8

            if upto >= 1:
              with ExitStack() as ph:
                P.barrier()
                t1r = Rot([sb(ph, f"t1r{i}", [128, 512], F32) for i in range(2)])
                t2r = Rot([sb(ph, f"t2r{i}", [128, 512], F32) for i in range(2)])
                hall = sb(ph, "hall", [128, 8, T], BF16)
                hallb = Buf()
                ropeC = sb(ph, "ropeC", [128, NLAT], F32)
                ropeS = sb(ph, "ropeS", [128, NLAT], F32)
                rB = Buf()
                P.op("sync", lambda e: e.dma_start(out=ropeC[:], in_=ropeC_d), pwrites=[rB])
                P.op("sync", lambda e: e.dma_start(out=ropeS[:], in_=ropeS_d), pwrites=[rB])
                with ExitStack() as ph2:
                    P.barrier()
                    tiles = {
                        "x": Rot([sb(ph2, f"p1x{i}", [128, 8, 512], F32) for i in range(2)]),
                        "sq": Rot([sb(ph2, "p1sq", [128, 8, 512], BF16)]),
                        "rs": Rot([sb(ph2, f"p1rs{i}", [128, 512], F32) for i in range(2)]),
                        "tmp": Rot([sb(ph2, f"p1tmp{i}", [128, 512], F32) for i in range(8)]),
                    }
                    for ti, (t0, w) in enumerate(TT):
                        class _H:
                            @staticmethod
                            def next(t0=t0):
                                return hall[:, :, t0:t0 + 512 if t0 < 4096 else T], hallb
                        tiles["h"] = _H
                        norm_tile(tiles, l, ti, 0)
                P.barrier()
                wst = Rot([sb(ph, f"wst{i}", [128, 8, 512], F32) for i in range(2)])
                wbf = Rot([sb(ph, f"wbf{i}", [128, 8, 512], BF16) for i in range(2)])
                ev = Rot([sb(ph, f"ev{i}", [128, 512], BF16) for i in range(4)])
                xbt = Rot([sb(ph, f"xbt{i}", [128, 512], BF16) for i in range(2)])
                segs = [("fm", 0, 512, 0, "rope"), ("fm", 512, 128, 4, "rope"), ("tm", 640, 128, 0, "copy"),
                        ("fm", 768, 256, 5, "rope"), ("fm", 1024, 256, 7, "rope"), ("tm", 1280, 512, 128, "copy"),
                        ("tm", 1792, 512, 640, "silu"), ("fm", 2304, 512, 9, "rope"), ("fm", 2816, 512, 13, "rope"),
                        ("tm", 3328, 512, 1152, "copy")] + [("fm", 3840 + 512 * i, 512, 17 + 4 * i, "sigmoid") for i in range(6)]
                pi = [0]

                def nps():
                    pi[0] = (pi[0] + 1) % 6
                    return pi[0]
                castflip = 0
                for si, (kind, c0, ncol, d0, mode) in enumerate(segs):
                    if si >= sub:
                        break
                    ws, wsb = wst.next()
                    wb_, wbb = wbf.next()
                    P.op("sync", lambda e, ws=ws, c0=c0, ncol=ncol: e.dma_start(
                        out=ws[:, :, 0:ncol], in_=w_in[l].rearrange("(kc k) n -> k kc n", k=128)[:, :, c0:c0 + ncol]), writes=[wsb])
                    for kc in range(8):
                        castflip ^= 1
                        evac_copy(wb_[:, kc, 0:ncol], ws[:, kc, 0:ncol], [wsb], pwrites=[wbb])
                    if kind == "fm":
                        for cj in range(ncol // 128):
                            for ti, (t0, w) in enumerate(TT):
                                p = nps()
                                for kc in range(8):
                                    P.op("tensor", lambda e, p=p, wb_=wb_, kc=kc, cj=cj, t0=t0, w=w: e.matmul(
                                        psum[p][:, 0:w], lhsT=wb_[:, kc, cj * 128:(cj + 1) * 128], rhs=hall[:, kc, t0:t0 + w],
                                        start=(kc == 0), stop=(kc == 7)), reads=[wbb, hallb], writes=[psb[p]])
                                et, etb = ev.next()
                                dst = FM[(d0 + cj) * 128:(d0 + cj + 1) * 128, t0:t0 + w]
                                if mode == "sigmoid":
                                    P.op("scalar", lambda e, p=p, et=et, w=w: e.activation(out=et[:, 0:w], in_=psum[p][:, 0:w], func=AF.Sigmoid),
                                         reads=[psb[p]], writes=[etb])
                                elif mode == "rope" and ti < 8:
                                    xq, xqb = xbt.next()
                                    t1, t1b = t1r.next()
                                    t2, t2b = t2r.next()
                                    p2 = nps()
                                    RS = int(os.environ.get("ROPE_STAGE", "5"))
                                    P.op("scalar", lambda e, p=p, xq=xq: e.copy(out=xq[:], in_=psum[p][:]), reads=[psb[p]], writes=[xqb])
                                    if RS >= 2:
                                        P.op("vector", lambda e, p=p, t1=t1, t0=t0: e.tensor_tensor(
                                            out=t1[:], in0=psum[p][:], in1=ropeC[:, t0:t0 + 512], op=ALU.mult), reads=[psb[p], rB, xqb], writes=[t1b])
                                    if RS >= 3:
                                        P.op("tensor", lambda e, p2=p2, xq=xq: e.matmul(psum[p2][:], lhsT=rmatT[:], rhs=xq[:], start=True, stop=True),
                                             reads=[xqb, cB], writes=[psb[p2]])
                                    if RS >= 4:
                                        P.op("vector", lambda e, p2=p2, t2=t2, t0=t0: e.tensor_tensor(
                                            out=t2[:], in0=psum[p2][:], in1=ropeS[:, t0:t0 + 512], op=ALU.mult), reads=[psb[p2], rB], writes=[t2b])
                                    if RS >= 5:
                                        P.op("vector", lambda e, et=et, t1=t1, t2=t2: e.tensor_tensor(out=et[:], in0=t1[:], in1=t2[:], op=ALU.add),
                                             reads=[t1b, t2b], writes=[etb])
                                    else:
                                        P.op("vector", lambda e, et=et, xq=xq: e.tensor_copy(out=et[:], in_=xq[:]), reads=[xqb], writes=[etb])
                                else:
                                    evac_copy(et[:, 0:w], psum[p][:, 0:w], [psb[p]], writes=[etb])
                                P.op("pool", lambda e, et=et, dst=dst, w=w: e.dma_start(out=dst, in_=et[:, 0:w]), reads=[etb], pwrites=[FMb])
                    else:
                        for s in range(T // 128):
                            p = nps()
                            for kc in range(8):
                                P.op("tensor", lambda e, p=p, wb_=wb_, kc=kc, s=s, ncol=ncol: e.matmul(
                                    psum[p][:, 0:ncol], lhsT=hall[:, kc, s * 128:(s + 1) * 128], rhs=wb_[:, kc, 0:ncol],
                                    start=(kc == 0), stop=(kc == 7)), reads=[wbb, hallb], writes=[psb[p]])
                            et, etb = ev.next()
                            if mode == "silu":
                                P.op("scalar", lambda e, p=p, et=et, ncol=ncol: e.activation(out=et[:, 0:ncol], in_=psum[p][:, 0:ncol], func=AF.Silu),
                                     reads=[psb[p]], writes=[etb])
                            else:
                                evac_copy(et[:, 0:ncol], psum[p][:, 0:ncol], [psb[p]], writes=[etb])
                            P.op("pool", lambda e, et=et, s=s, d0=d0, ncol=ncol: e.dma_start(
                                out=TM[s * 128:(s + 1) * 128, d0:d0 + ncol], in_=et[:, 0:ncol]), reads=[etb], pwrites=[TMb])

            qblocks = list(range(32)) + ([32, 33] if ctx_out else [])

            def transpose_store(ph_t, src_tile, src_buf, nchunks, row0, s, acc):
                for cc in range(nchunks):
                    P.op("tensor", lambda e, cc=cc: e.transpose(
                        out=ph_t["pst"][:, cc, :], in_=src_tile[:, cc * 128:(cc + 1) * 128], identity=identb[:]),
                        reads=[src_buf, cB], writes=[ph_t["pstb"]] if cc == 0 else (), pwrites=[ph_t["pstb"]] if cc > 0 else ())
                if acc["cnt"] == 0:
                    acc["tile"], acc["buf"] = ph_t["brt"].next()
                    acc["s0"] = s
                k = acc["cnt"]
                tile_, buf_ = acc["tile"], acc["buf"]
                evac_copy(tile_[:, 0:nchunks, k * 128:(k + 1) * 128], ph_t["pst"][:, 0:nchunks, :], [ph_t["pstb"]],
                          writes=[buf_] if k == 0 else (), pwrites=[buf_] if k > 0 else ())
                acc["cnt"] += 1
                last = (s == 31) or (s == 33)
                if acc["cnt"] == 4 or last:
                    n = acc["cnt"]
                    s0 = acc["s0"]
                    for cc in range(nchunks):
                        P.op("pool", lambda e, cc=cc, n=n, s0=s0, tile_=tile_: e.dma_start(
                            out=BR[row0 + cc * 128:row0 + (cc + 1) * 128, s0 * 128:(s0 + n) * 128], in_=tile_[:, cc, 0:n * 128]),
                            reads=[buf_], pwrites=[BRb])
                    acc["cnt"] = 0

            if upto >= 2:
              with ExitStack() as ph:
                P.barrier()
                kA = sb(ph, "kA", [64, T], BF16)
                vA = sb(ph, "vA", [128, 34, 65], BF16)
                qA = sb(ph, "qA", [64, 4, T], BF16)
                wmask = sb(ph, "wmask", [128, 2, 512], BF16)
                wmB = Buf()
                P.op("sync", lambda e: e.dma_start(out=wmask[:], in_=wmask_d), writes=[wmB])
                Er = Rot([sb(ph, f"EA{i}", [128, 512], BF16) for i in range(10)])
                oat = Rot([sb(ph, f"oat{i}", [128, 256], BF16) for i in range(3)])
                den = Rot([sb(ph, f"denA{i}", [128, 4], F32) for i in range(3)])
                ph_t = {"pst": psum[6][:].rearrange("p (c t) -> p c t", t=128)[:, 0:2, :], "pstb": psb[6],
                        "brt": Rot([sb(ph, f"brtA{i}", [128, 2, 512], BF16) for i in range(2)])}
                kvB = Buf()
                for g in range(2):
                    P.op("sync", lambda e, g=g: e.dma_start(out=kA[:], in_=FM[4 * 128 + g * 64:4 * 128 + (g + 1) * 64, :]), reads=[FMb], writes=[kvB])
                    P.op("sync", lambda e, g=g: e.dma_start(out=vA[:, :, 0:64], in_=TM[:, g * 64:(g + 1) * 64].rearrange("(j p) c -> p j c", p=128)),
                         reads=[TMb], pwrites=[kvB])
                    P.op("vector", lambda e: e.memset(vA[:, :, 64:65], 1.0), pwrites=[kvB])
                    for r in range(4):
                        hh = g * 4 + r
                        P.op("sync", lambda e, r=r, hh=hh: e.dma_start(out=qA[:, r, :], in_=FM[hh * 64:(hh + 1) * 64, :]), reads=[FMb], pwrites=[kvB])
                    acc = {"cnt": 0}
                    def stA1(i):
                        if i < 32:
                            keys = ([(i - 1, 0)] if i > 0 else []) + [(i, None)] + ([(i + 1, 1)] if i < 31 else []) + [(32, None), (33, None)]
                        else:
                            keys = [(32, None), (33, None)]
                        Es = []
                        for (j, mk) in keys:
                            p = 0 + (Er.i % 4)
                            P.op("tensor", lambda e, p=p, j=j, i=i: e.matmul(
                                psum[p][:], lhsT=kA[:, j * 128:(j + 1) * 128], rhs=qA[:, :, i * 128:(i + 1) * 128], start=True, stop=True),
                                reads=[kvB], writes=[psb[p]])
                            Et, Eb = Er.next()
                            P.op("scalar", lambda e, p=p, Et=Et: e.activation(out=Et[:], in_=psum[p][:], func=AF.Exp, scale=0.125),
                                 reads=[psb[p]], writes=[Eb])
                            if mk is not None:
                                P.op("vector", lambda e, Et=Et, mk=mk: e.tensor_tensor(out=Et[:], in0=Et[:], in1=wmask[:, mk, :], op=ALU.mult),
                                     reads=[wmB], writes=[Eb])
                            Es.append((j, Et, Eb))
                        return Es

                    def stA2(i, Es):
                        po = 4 + (i % 2)
                        pov = psum[po][:, 0:260].rearrange("p (r c) -> p r c", c=65)
                        for r in range(4):
                            for n_, (j, Et, Eb) in enumerate(Es):
                                P.op("tensor", lambda e, r=r, j=j, Et=Et, n_=n_, pov=pov: e.matmul(
                                    pov[:, r, :], lhsT=Et[:, r * 128:(r + 1) * 128], rhs=vA[:, j, :], start=(n_ == 0), stop=(n_ == len(Es) - 1)),
                                    reads=[Eb, kvB], writes=[psb[po]])
                        dn, dnb = den.next()
                        P.op("vector", lambda e, dn=dn, pov=pov, g=g: e.tensor_tensor(
                            out=dn[:], in0=pov[:, :, 64], in1=esink[:, l * 8 + g * 4:l * 8 + g * 4 + 4], op=ALU.add),
                            reads=[psb[po], cB], writes=[dnb])
                        P.op("vector", lambda e, dn=dn: e.reciprocal(out=dn[:], in_=dn[:]), writes=[dnb])
                        ot, otb = oat.next()
                        for r in range(4):
                            P.op("vector", lambda e, r=r, ot=ot, pov=pov, dn=dn: e.tensor_scalar(
                                out=ot[:, r * 64:(r + 1) * 64], in0=pov[:, r, 0:64], scalar1=dn[:, r:r + 1], scalar2=None, op0=ALU.mult),
                                reads=[psb[po], dnb], writes=[otb] if r == 0 else (), pwrites=[otb] if r > 0 else ())
                        return ot, otb

                    nb = len(qblocks)
                    stash1, stash2 = {}, {}
                    for k in range(nb + 2):
                        if k < nb:
                            stash1[k] = stA1(qblocks[k])
                        if 0 <= k - 1 < nb:
                            stash2[k - 1] = stA2(qblocks[k - 1], stash1.pop(k - 1))
                        if 0 <= k - 2 < nb:
                            ot, otb = stash2.pop(k - 2)
                            transpose_store(ph_t, ot, otb, 2, g * 256, qblocks[k - 2], acc)

            if upto >= 3:
              with ExitStack() as ph:
                P.barrier()
                rt = sb(ph, "rt", [128, 8, 128], F32)
                rtB = Buf()
                P.op("sync", lambda e: e.dma_start(out=rt[:], in_=rettab_d), writes=[rtB])
                qB_ = sb(ph, "qB", [64, T], BF16)
                kB_ = sb(ph, "kB", [64, T], BF16)
                vB_ = sb(ph, "vB", [128, 34, 128], BF16)
                gB_ = sb(ph, "gB", [128, 34, 128], BF16)
                MT = sb(ph, "MT", [128, 128], F32)
                MT2 = sb(ph, "MT2", [128, 128], F32)
                kd = sb(ph, "kd", [128, 4], F32)
                qdf = sb(ph, "qdf", [64, 128], BF16)
                qdb = sb(ph, "qdb", [64, 128], BF16)
                kvF = sb(ph, "kvF", [64, 34, 128], F32)
                kvBk = sb(ph, "kvBk", [64, 34, 128], F32)
                Sin = sb(ph, "Sin", [64, 34, 128], F32)
                Tin = sb(ph, "Tin", [64, 34, 128], F32)
                SinB = sb(ph, "SinB", [64, 34, 128], BF16)
                TinB = sb(ph, "TinB", [64, 34, 128], BF16)
                Kf = Rot([sb(ph, f"Kf{i}", [128, 64], BF16) for i in range(2)])
                Kb = Rot([sb(ph, f"Kb{i}", [128, 64], BF16) for i in range(2)])
                Sm = Rot([sb(ph, f"Sm{i}", [128, 128], BF16) for i in range(3)])
                Qf = Rot([sb(ph, f"Qf{i}", [64, 128], BF16) for i in range(3)])
                Qb = Rot([sb(ph, f"Qb{i}", [64, 128], BF16) for i in range(3)])
                obt = Rot([sb(ph, f"obt{i}", [128, 128], BF16) for i in range(3)])
                ssr = Rot([sb(ph, f"ssr{i}", [128, 2], F32) for i in range(3)])
                junk = sb(ph, "junkB", [128, 128], F32)
                ph_t = {"pst": psum[6][:].rearrange("p (c t) -> p c t", t=128)[:, 0:1, :], "pstb": psb[6],
                        "brt": Rot([sb(ph, f"brtB{i}", [128, 1, 512], BF16) for i in range(2)])}
                ldB, tbB, kvb_, scB = Buf(), Buf(), Buf(), Buf()
                ptbufs = [Buf(), Buf()]
                for h in range(4):
                    ch, half = h // 2, h % 2
                    P.op("sync", lambda e, ch=ch, half=half: e.dma_start(out=qB_[:], in_=FM[(5 + ch) * 128 + half * 64:(5 + ch) * 128 + half * 64 + 64, :]),
                         reads=[FMb], writes=[ldB])
                    P.op("sync", lambda e, ch=ch, half=half: e.dma_start(out=kB_[:], in_=FM[(7 + ch) * 128 + half * 64:(7 + ch) * 128 + half * 64 + 64, :]),
                         reads=[FMb], pwrites=[ldB])
                    P.op("sync", lambda e, h=h: e.dma_start(out=vB_[:], in_=TM[:, 128 + h * 128:128 + (h + 1) * 128].rearrange("(j p) c -> p j c", p=128)),
                         reads=[TMb], pwrites=[ldB])
                    P.op("sync", lambda e, h=h: e.dma_start(out=gB_[:], in_=TM[:, 640 + h * 128:640 + (h + 1) * 128].rearrange("(j p) c -> p j c", p=128)),
                         reads=[TMb], pwrites=[ldB])
                    lf = lgt[:, l * 8 + h:l * 8 + h + 1]
                    lb = lgt[:, l * 8 + 4 + h:l * 8 + 4 + h + 1]
                    P.op("scalar", lambda e, lf=lf: e.activation(out=MT[:], in_=rt[:, 0, :], func=AF.Exp, scale=lf), reads=[rtB, cB], writes=[tbB])
                    P.op("scalar", lambda e, lb=lb: e.activation(out=MT2[:], in_=rt[:, 2, :], func=AF.Exp, scale=lb), reads=[rtB, cB], pwrites=[tbB])
                    P.op("vector", lambda e: e.tensor_tensor(out=MT[:], in0=MT[:], in1=rt[:, 1, :], op=ALU.mult), reads=[tbB, rtB], writes=[tbB])
                    P.op("vector", lambda e: e.tensor_tensor(out=MT2[:], in0=MT2[:], in1=rt[:, 3, :], op=ALU.mult), reads=[tbB], writes=[tbB])
                    P.op("vector", lambda e: e.scalar_tensor_tensor(out=MT[:], in0=MT[:], scalar=0.125, in1=MT2[:], op0=ALU.mult, op1=ALU.add),
                         reads=[tbB], writes=[tbB])
                    P.op("vector", lambda e: e.scalar_tensor_tensor(out=MT[:], in0=MT2[:], scalar=-0.875, in1=MT[:], op0=ALU.mult, op1=ALU.add),
                         reads=[tbB], writes=[tbB])
                    P.op("scalar", lambda e, lf=lf: e.activation(out=kd[:, 0:1], in_=rt[:, 6, 0:1], func=AF.Exp, scale=lf), reads=[tbB], writes=[tbB])
                    P.op("scalar", lambda e, lb=lb: e.activation(out=kd[:, 1:2], in_=rt[:, 6, 1:2], func=AF.Exp, scale=lb), reads=[tbB], writes=[tbB])
                    P.op("scalar", lambda e, lf=lf: e.activation(out=kd[:, 2:3], in_=rt[:, 6, 2:3], func=AF.Exp, scale=lf), reads=[tbB], writes=[tbB])
                    P.op("scalar", lambda e, lb=lb: e.activation(out=kd[:, 3:4], in_=rt[:, 6, 2:3], func=AF.Exp, scale=lb), reads=[tbB], writes=[tbB])
                    P.op("vector", lambda e: e.tensor_scalar(out=kd[:, 0:2], in0=kd[:, 0:2], scalar1=0.125, scalar2=None, op0=ALU.mult), writes=[tbB])
                    P.op("scalar", lambda e, lf=lf: e.activation(out=qdf[:], in_=rt[0:64, 4, :], func=AF.Exp, scale=lf[0:64]), reads=[tbB], writes=[tbB])
                    P.op("scalar", lambda e, lb=lb: e.activation(out=qdb[:], in_=rt[0:64, 5, :], func=AF.Exp, scale=lb[0:64]), reads=[tbB], writes=[tbB])
                    def stP1(n):
                        pt = psum[6][:, 0:64] if n % 2 == 0 else psum[7][:, 0:32].bitcast(BF16)
                        ptb = psb[6] if n % 2 == 0 else psb[7]
                        P.op("tensor", lambda e, n=n, pt=pt: e.transpose(out=pt, in_=kB_[:, n * 128:(n + 1) * 128], identity=identb[0:64, 0:64]),
                             reads=[ldB, cB], writes=[ptb])
                        kf, kfb = Kf.next()
                        kb, kbb = Kb.next()
                        P.op("vector", lambda e, kf=kf, pt=pt: e.tensor_scalar(out=kf[:], in0=pt, scalar1=kd[:, 0:1], scalar2=None, op0=ALU.mult),
                             reads=[ptb, tbB], writes=[kfb])
                        P.op("vector", lambda e, kb=kb, pt=pt: e.tensor_scalar(out=kb[:], in0=pt, scalar1=kd[:, 1:2], scalar2=None, op0=ALU.mult),
                             reads=[ptb, tbB], writes=[kbb])
                        return kf, kfb, kb, kbb

                    def stP2(n, st):
                        kf, kfb, kb, kbb = st
                        pa, pb = n % 2, 2 + n % 2
                        P.op("tensor", lambda e, n=n, kf=kf, pa=pa: e.matmul(psum[pa][0:64, 0:128], lhsT=kf[:], rhs=vB_[:, n, :], start=True, stop=True),
                             reads=[kfb, ldB], writes=[psb[pa]])
                        P.op("tensor", lambda e, n=n, kb=kb, pb=pb: e.matmul(psum[pb][0:64, 0:128], lhsT=kb[:], rhs=vB_[:, n, :], start=True, stop=True),
                             reads=[kbb, ldB], writes=[psb[pb]])
                        P.op("scalar", lambda e, n=n, pa=pa: e.copy(out=kvF[:, n, :], in_=psum[pa][0:64, 0:128]), reads=[psb[pa]], pwrites=[kvb_])
                        P.op("scalar", lambda e, n=n, pb=pb: e.copy(out=kvBk[:, n, :], in_=psum[pb][0:64, 0:128]), reads=[psb[pb]], pwrites=[kvb_])

                    shp = {}
                    for k in range(35):
                        if k < 34:
                            shp[k] = stP1(k)
                        if k >= 1:
                            stP2(k - 1, shp.pop(k - 1))
                    G, Gb = kd[0:64, 2:3], kd[0:64, 3:4]
                    P.op("vector", lambda e: e.memset(Sin[:, 32, :], 0.0), reads=[kvb_], writes=[scB])
                    P.op("vector", lambda e: e.memset(Tin[:, 33, :], 0.0), writes=[scB])
                    P.op("vector", lambda e: e.tensor_copy(out=Sin[:, 33, :], in_=kvF[:, 32, :]), reads=[kvb_], writes=[scB])
                    P.op("vector", lambda e: e.tensor_copy(out=Tin[:, 32, :], in_=kvBk[:, 33, :]), reads=[kvb_], writes=[scB])
                    P.op("vector", lambda e: e.scalar_tensor_tensor(out=Sin[:, 0, :], in0=Sin[:, 33, :], scalar=G, in1=kvF[:, 33, :], op0=ALU.mult, op1=ALU.add),
                         reads=[tbB, kvb_], writes=[scB])
                    P.op("vector", lambda e: e.scalar_tensor_tensor(out=Tin[:, 31, :], in0=Tin[:, 32, :], scalar=Gb, in1=kvBk[:, 32, :], op0=ALU.mult, op1=ALU.add),
                         reads=[kvb_], writes=[scB])
                    for n in range(31):
                        P.op("vector", lambda e, n=n: e.scalar_tensor_tensor(
                            out=Sin[:, n + 1, :], in0=Sin[:, n, :], scalar=G, in1=kvF[:, n, :], op0=ALU.mult, op1=ALU.add), reads=[kvb_], writes=[scB])
                        m = 31 - n
                        P.op("vector", lambda e, m=m: e.scalar_tensor_tensor(
                            out=Tin[:, m - 1, :], in0=Tin[:, m, :], scalar=Gb, in1=kvBk[:, m, :], op0=ALU.mult, op1=ALU.add), reads=[kvb_], writes=[scB])
                    P.op("vector", lambda e: e.tensor_copy(out=SinB[:], in_=Sin[:]), writes=[scB])
                    P.op("vector", lambda e: e.tensor_copy(out=TinB[:], in_=Tin[:]), reads=[scB], pwrites=[scB])
                    scB2 = scB
                    acc = {"cnt": 0}

                    def stB1(n):
                        ps_ = 0 + n % 2
                        P.op("tensor", lambda e, n=n, ps_=ps_: e.matmul(psum[ps_][:, 0:128], lhsT=kB_[:, n * 128:(n + 1) * 128], rhs=qB_[:, n * 128:(n + 1) * 128],
                                                                     start=True, stop=True), reads=[ldB], writes=[psb[ps_]])
                        sm, smb = Sm.next()
                        P.op("vector", lambda e, sm=sm, ps_=ps_: e.tensor_tensor(out=sm[:], in0=psum[ps_][:, 0:128], in1=MT[:], op=ALU.mult),
                             reads=[psb[ps_], tbB], writes=[smb])
                        qf, qfb = Qf.next()
                        qb, qbb = Qb.next()
                        P.op("vector", lambda e, n=n, qf=qf: e.tensor_tensor(out=qf[:], in0=qB_[:, n * 128:(n + 1) * 128], in1=qdf[:], op=ALU.mult),
                             reads=[ldB, tbB], writes=[qfb])
                        P.op("vector", lambda e, n=n, qb=qb: e.tensor_tensor(out=qb[:], in0=qB_[:, n * 128:(n + 1) * 128], in1=qdb[:], op=ALU.mult),
                             reads=[ldB, tbB], writes=[qbb])
                        return (sm, smb, qf, qfb, qb, qbb)

                    def stB2(n, st):
                        sm, smb, qf, qfb, qb, qbb = st
                        po_ = 2 + n % 2
                        P.op("tensor", lambda e, n=n, sm=sm, po_=po_: e.matmul(psum[po_][:, 0:128], lhsT=sm[:], rhs=vB_[:, n, :], start=True, stop=False),
                             reads=[smb, ldB], writes=[psb[po_]])
                        P.op("tensor", lambda e, n=n, qf=qf, po_=po_: e.matmul(psum[po_][:, 0:128], lhsT=qf[:], rhs=SinB[:, n, :], start=False, stop=False),
                             reads=[qfb, scB2], writes=[psb[po_]])
                        P.op("tensor", lambda e, n=n, qb=qb, po_=po_: e.matmul(psum[po_][:, 0:128], lhsT=qb[:], rhs=TinB[:, n, :], start=False, stop=True),
                             reads=[qbb, scB2], writes=[psb[po_]])
                        ss, ssb = ssr.next()
                        P.op("scalar", lambda e, ss=ss, po_=po_: e.activation(out=junk[:], in_=psum[po_][:, 0:128], func=AF.Square, accum_out=ss[:, 0:1]),
                             reads=[psb[po_]], writes=[ssb])
                        P.op("scalar", lambda e, ss=ss: e.activation(out=ss[:, 1:2], in_=ss[:, 0:1], func=AF.Ln, scale=1.0 / 128, bias=epsc[:, 0:1]), writes=[ssb])
                        P.op("scalar", lambda e, ss=ss: e.activation(out=ss[:, 1:2], in_=ss[:, 1:2], func=AF.Exp, scale=-0.5), writes=[ssb])
                        ob, obb = obt.next()
                        P.op("vector", lambda e, n=n, ob=ob, ss=ss, po_=po_: e.scalar_tensor_tensor(
                            out=ob[:], in0=psum[po_][:, 0:128], scalar=ss[:, 1:2], in1=gB_[:, n, :], op0=ALU.mult, op1=ALU.mult),
                            reads=[psb[po_], ssb, ldB], writes=[obb])
                        return ob, obb

                    nb = len(qblocks)
                    sh1, sh2 = {}, {}
                    for k in range(nb + 2):
                        if k < nb:
                            sh1[k] = stB1(qblocks[k])
                        if 0 <= k - 1 < nb:
                            sh2[k - 1] = stB2(qblocks[k - 1], sh1.pop(k - 1))
                        if 0 <= k - 2 < nb:
                            ob, obb = sh2.pop(k - 2)
                            transpose_store(ph_t, ob, obb, 1, 512 + h * 128, qblocks[k - 2], acc)

            if upto >= 4:
              with ExitStack() as ph:
                P.barrier()
                Osb = [sb(ph, f"Osb{c}", [128, 4, 129], F32) for c in range(2)]
                Osbb = [Buf(), Buf()]
                t1c = Rot([sb(ph, f"t1c{i}", [128, 128], F32) for i in range(2)])
                occ = Rot([sb(ph, f"occ{i}", [128, 128], F32) for i in range(2)])
                junk = sb(ph, "junkC", [128, 128], F32)
                kD2 = sb(ph, "kD2", [128, T], BF16)
                qD = [sb(ph, f"qD{c}", [128, T], BF16) for c in range(2)]
                vD = sb(ph, "vD", [128, 34, 129], BF16)
                E = [sb(ph, f"ED{c}", [128, 34, 512], BF16) for c in range(2)]
                Ebuf = [Buf(), Buf()]
                oct_ = Rot([sb(ph, f"oct{i}", [128, 128], BF16) for i in range(2)])
                rcp = Rot([sb(ph, f"rcp{i}", [128, 4], F32) for i in range(2)])
                ph_t = {"pst": psum[6][:].rearrange("p (c t) -> p c t", t=128)[:, 0:1, :], "pstb": psb[6],
                        "brt": Rot([sb(ph, f"brtC{i}", [128, 1, 512], BF16) for i in range(2)])}
                ldB, zB = Buf(), Buf()
                P.op("vector", lambda e: e.memset(qD[0][64:128, :], 0.0), pwrites=[zB])
                P.op("vector", lambda e: e.memset(qD[1][0:64, :], 0.0), pwrites=[zB])
                P.op("vector", lambda e: e.memset(vD[:, :, 128:129], 1.0), pwrites=[zB])
                for h in range(4):
                    P.op("sync", lambda e, h=h: e.dma_start(out=kD2[:], in_=FM[(13 + h) * 128:(14 + h) * 128, :]), reads=[FMb], writes=[ldB])
                    P.op("sync", lambda e, h=h: e.dma_start(out=qD[0][0:64, :], in_=FM[(9 + h) * 128:(9 + h) * 128 + 64, :]), reads=[FMb], pwrites=[ldB])
                    P.op("sync", lambda e, h=h: e.dma_start(out=qD[1][64:128, :], in_=FM[(9 + h) * 128 + 64:(10 + h) * 128, :]), reads=[FMb], pwrites=[ldB])
                    P.op("sync", lambda e, h=h: e.dma_start(out=vD[:, :, 0:128], in_=TM[:, 1152 + h * 128:1152 + (h + 1) * 128].rearrange("(j p) c -> p j c", p=128)),
                         reads=[TMb], pwrites=[ldB])
                    acc = {"cnt": 0}
                    PVB = [4, 4, 5, 5]
                    pairs = [(ps01, 0, 1), (ps23, 2, 3)]
                    def finish_unit(ti, c):
                        t0, w = TT[ti]
                        nr = w // 128
                        for r in range(nr):
                            P.op("vector", lambda e, r=r, c=c: e.tensor_copy(out=Osb[c][:, r, :], in_=psum[PVB[r]][:, (r % 2) * 129:(r % 2) * 129 + 129]),
                                 reads=[psb[PVB[r]]], writes=[Osbb[c]] if r == 0 else (), pwrites=[Osbb[c]] if r else ())
                        if c == 0:
                            return
                        for r in range(nr):
                            o1 = Osb[0][:, r, :]
                            o2 = Osb[1][:, r, :]
                            rc, rcb = rcp.next()
                            P.op("vector", lambda e, rc=rc, o1=o1: e.reciprocal(out=rc[:, 0:1], in_=o1[:, 128:129]), reads=[Osbb[0]], writes=[rcb])
                            P.op("vector", lambda e, rc=rc, o2=o2: e.reciprocal(out=rc[:, 1:2], in_=o2[:, 128:129]), reads=[Osbb[1]], writes=[rcb])
                            P.op("vector", lambda e, rc=rc: e.tensor_tensor(out=rc[:, 1:2], in0=rc[:, 1:2], in1=neglam[:, l:l + 1], op=ALU.mult),
                                 reads=[cB], writes=[rcb])
                            t1, t1b = t1c.next()
                            oc_, ocb = occ.next()
                            P.op("vector", lambda e, t1=t1, rc=rc, o1=o1: e.tensor_scalar(out=t1[:], in0=o1[:, 0:128], scalar1=rc[:, 0:1], scalar2=None, op0=ALU.mult),
                                 reads=[Osbb[0], rcb], writes=[t1b])
                            P.op("vector", lambda e, t1=t1, rc=rc, o2=o2, oc_=oc_: e.scalar_tensor_tensor(
                                out=oc_[:], in0=o2[:, 0:128], scalar=rc[:, 1:2], in1=t1[:], op0=ALU.mult, op1=ALU.add),
                                reads=[Osbb[1], rcb, t1b], writes=[ocb])
                            P.op("scalar", lambda e, rc=rc, oc_=oc_: e.activation(out=junk[:], in_=oc_[:], func=AF.Square, accum_out=rc[:, 2:3]),
                                 reads=[ocb], writes=[rcb])
                            P.op("scalar", lambda e, rc=rc: e.activation(out=rc[:, 3:4], in_=rc[:, 2:3], func=AF.Ln, scale=1.0 / 128, bias=epsc[:, 0:1]), writes=[rcb])
                            P.op("scalar", lambda e, rc=rc: e.activation(out=rc[:, 3:4], in_=rc[:, 3:4], func=AF.Exp, scale=-0.5), writes=[rcb])
                            ot, otb = oct_.next()
                            P.op("vector", lambda e, ot=ot, oc_=oc_, rc=rc: e.scalar_tensor_tensor(
                                out=ot[:], in0=oc_[:], scalar=rc[:, 3:4], in1=gainb[:, l, :], op0=ALU.mult, op1=ALU.mult),
                                reads=[ocb, rcb, cB], writes=[otb])
                            transpose_store(ph_t, ot, otb, 1, 1024 + h * 128, t0 // 128 + r, acc)


                    units = []
                    for ti, (t0, w) in enumerate(TT):
                        if ti == 8 and not ctx_out:
                            continue
                        for c in range(2):
                            units.append((ti, c))

                    def pv_ops(u_idx):
                        ti, c = units[u_idx]
                        t0, w = TT[ti]
                        keys = list(range(34)) if ti < 8 else [32, 33]
                        ops = []
                        for r in range(w // 128):
                            for n_, j in enumerate(keys):
                                ops.append((r, j, n_ == 0, n_ == len(keys) - 1, c))
                        return ops

                    def emit_pv(op):
                        r, j, st_, sp_, eb = op
                        P.op("tensor", lambda e: e.matmul(
                            psum[PVB[r]][:, (r % 2) * 129:(r % 2) * 129 + 129], lhsT=E[eb][:, j, r * 128:(r + 1) * 128], rhs=vD[:, j, :], start=st_, stop=sp_),
                            reads=[Ebuf[eb], ldB, zB], writes=[psb[PVB[r]]])

                    pcount = 0
                    for u_idx in range(len(units) + 1):
                        pend = pv_ops(u_idx - 1) if u_idx >= 1 else []
                        if u_idx < len(units):
                            ti, c = units[u_idx]
                            t0, w = TT[ti]
                            keys = list(range(34)) if ti < 8 else [32, 33]
                            npairs = len(keys) // 2
                            per = -(-len(pend) // npairs) if pend else 0
                            for jp in range(0, len(keys), 2):
                                pt_, pa_, pb_ = pairs[pcount % 2]
                                pcount += 1
                                for hh, pbk in ((0, pa_), (1, pb_)):
                                    j = keys[jp + hh]
                                    P.op("tensor", lambda e, c=c, j=j, pbk=pbk: e.matmul(
                                        psum[pbk][:, 0:w], lhsT=kD2[:, j * 128:(j + 1) * 128], rhs=qD[c][:, t0:t0 + w], start=True, stop=True),
                                        reads=[ldB, zB], writes=[psb[pbk]])
                                j0 = keys[jp]
                                P.op("scalar", lambda e, c=c, j0=j0, pt_=pt_: e.activation(
                                    out=E[c][:, j0:j0 + 2, 0:w], in_=pt_[:].rearrange("p (a b) -> p a b", b=512)[:, :, 0:w], func=AF.Exp, scale=0.125),
                                    reads=[psb[pa_], psb[pb_]], writes=[Ebuf[c]] if jp == 0 else (), pwrites=[Ebuf[c]] if jp else ())
                                for _ in range(per):
                                    if pend:
                                        emit_pv(pend.pop(0))
                        while pend:
                            emit_pv(pend.pop(0))
                        if u_idx >= 1:
                            finish_unit(*units[u_idx - 1])

            if upto >= 5:
              with ExitStack() as ph:
                P.barrier()
                wbr = sb(ph, "wbr", [128, 12, D], BF16)
                wo = sb(ph, "wo", [128, 8, D], BF16)
                wB = Buf()
                stg = Rot([sb(ph, f"stgM{i}", [128, D], F32) for i in range(3)])
                cf = 0
                for k in range(20):
                    st_, stb = stg.next()
                    src = w_branch[l].rearrange("i (kc k) n -> k (i kc) n", k=128)[:, k, :] if k < 12 else \
                        w_out[l].rearrange("(kc k) n -> k kc n", k=128)[:, k - 12, :]
                    dstw = wbr[:, k, :] if k < 12 else wo[:, k - 12, :]
                    P.op("sync", lambda e, st_=st_, src=src: e.dma_start(out=st_[:], in_=src), writes=[stb])
                    cf ^= 1
                    evac_copy(dstw, st_[:], [stb], pwrites=[wB])
                brr = Rot([sb(ph, f"brr{i}", [128, 12, 512], BF16) for i in range(2)])
                glr = Rot([sb(ph, f"glr{i}", [128, 24, 512], BF16) for i in range(2)])
                xr = Rot([sb(ph, f"xrM{i}", [128, 8, 512], F32) for i in range(2)])
                yr = Rot([sb(ph, f"yrM{i}", [128, 8, 512], BF16) for i in range(2)])
                ya = Rot([sb(ph, f"yaM{i}", [128, 512], F32) for i in range(2)])
                yb_ = Rot([sb(ph, f"ybM{i}", [128, 512], F32) for i in range(2)])
                pi = [0]

                def nps():
                    pi[0] = (pi[0] + 1) % 6
                    return pi[0]
                for ti, (t0, w) in enumerate(TT):
                    if ti == 8 and not ctx_out:
                        continue
                    col = 1 if ti == 8 else 0
                    br_, brb = brr.next()
                    gl_, glb = glr.next()
                    xt, xtb = xr.next()
                    yt, ytb = yr.next()
                    P.op("sync", lambda e, br_=br_, t0=t0, w=w: e.dma_start(out=br_[:, :, 0:w], in_=BR.rearrange("(c k) t -> k c t", k=128)[:, :, t0:t0 + w]),
                         reads=[BRb], writes=[brb])
                    P.op("sync", lambda e, gl_=gl_, t0=t0, w=w: e.dma_start(out=gl_[:, :, 0:w], in_=FM[17 * 128:41 * 128, :].rearrange("(c k) t -> k c t", k=128)[:, :, t0:t0 + w]),
                         reads=[FMb], writes=[glb])
                    P.op("sync", lambda e, xt=xt, t0=t0, w=w: e.dma_start(out=xt[:, :, 0:w], in_=xT.rearrange("(kc k) t -> k kc t", k=128)[:, :, t0:t0 + w]),
                         reads=[xb[ti]], writes=[xtb])
                    for oc in range(8):
                        yacc, yab = ya.next()
                        for i in range(3):
                            p = nps()
                            for kc in range(4):
                                P.op("tensor", lambda e, p=p, i=i, kc=kc, oc=oc, br_=br_, w=w: e.matmul(
                                    psum[p][:, 0:w], lhsT=wbr[:, i * 4 + kc, oc * 128:(oc + 1) * 128], rhs=br_[:, i * 4 + kc, 0:w], start=(kc == 0), stop=(kc == 3)),
                                    reads=[wB, brb], writes=[psb[p]])
                            if i == 0:
                                P.op("vector", lambda e, p=p, yacc=yacc, gl_=gl_, oc=oc, w=w: e.tensor_tensor(
                                    out=yacc[:, 0:w], in0=psum[p][:, 0:w], in1=gl_[:, oc, 0:w], op=ALU.mult), reads=[psb[p], glb], writes=[yab])
                            else:
                                y2, y2b = yb_.next()
                                P.op("vector", lambda e, p=p, y2=y2, gl_=gl_, oc=oc, i=i, w=w: e.tensor_tensor(
                                    out=y2[:, 0:w], in0=psum[p][:, 0:w], in1=gl_[:, i * 8 + oc, 0:w], op=ALU.mult), reads=[psb[p], glb], writes=[y2b])
                                if i == 1:
                                    P.op("vector", lambda e, yacc=yacc, y2=y2, w=w: e.tensor_tensor(out=yacc[:, 0:w], in0=yacc[:, 0:w], in1=y2[:, 0:w], op=ALU.add),
                                         reads=[y2b], writes=[yab])
                                else:
                                    P.op("vector", lambda e, yacc=yacc, y2=y2, yt=yt, oc=oc, w=w: e.tensor_tensor(
                                        out=yt[:, oc, 0:w], in0=yacc[:, 0:w], in1=y2[:, 0:w], op=ALU.add),
                                        reads=[y2b, yab], writes=[ytb] if oc == 0 else (), pwrites=[ytb] if oc else ())
                    for oc in range(8):
                        p = nps()
                        for kc in range(8):
                            P.op("tensor", lambda e, p=p, kc=kc, oc=oc, yt=yt, w=w: e.matmul(
                                psum[p][:, 0:w], lhsT=wo[:, kc, oc * 128:(oc + 1) * 128], rhs=yt[:, kc, 0:w], start=(kc == 0), stop=(kc == 7)),
                                reads=[wB, ytb], writes=[psb[p]])
                        P.op("vector", lambda e, p=p, oc=oc, xt=xt, col=col, w=w: e.scalar_tensor_tensor(
                            out=xt[:, oc, 0:w], in0=psum[p][:, 0:w], scalar=mv(l, col, 2)[:, oc:oc + 1], in1=xt[:, oc, 0:w], op0=ALU.mult, op1=ALU.add),
                            reads=[psb[p], cB], writes=[xtb])
                    P.op("pool", lambda e, xt=xt, t0=t0, w=w: e.dma_start(out=xT.rearrange("(kc k) t -> k kc t", k=128)[:, :, t0:t0 + w], in_=xt[:, :, 0:w]),
                         reads=[xtb], writes=[xb[ti]])

            is_moe = (l % 2 == 1)
            l2 = l // 2
            if upto >= 6:
              with ExitStack() as ph:
                P.barrier()
                tiles = {
                    "x": Rot([sb(ph, f"n2x{i}", [128, 8, 512], F32) for i in range(2)]),
                    "sq": Rot([sb(ph, "n2sq", [128, 8, 512], BF16)]),
                    "rs": Rot([sb(ph, f"n2rs{i}", [128, 512], F32) for i in range(2)]),
                    "tmp": Rot([sb(ph, f"n2tmp{i}", [128, 512], F32) for i in range(6)]),
                    "h": Rot([sb(ph, f"n2h{i}", [128, 8, 512], BF16) for i in range(2)]),
                    "hf": Rot([sb(ph, f"n2hf{i}", [128, 8, 512], F32) for i in range(2 if is_moe else 0)]),
                }
                if is_moe:
                    wr = sb(ph, "wr", [128, 8, NE], F32)
                    wrB = Buf()
                    P.op("sync", lambda e: e.dma_start(out=wr[:], in_=moe_router[l2].rearrange("(kc k) n -> k kc n", k=128)), writes=[wrB])
                    lg_ = Rot([sb(ph, f"lg{i}", [128, 8], F32) for i in range(2)])
                    mx_ = Rot([sb(ph, f"mx{i}", [128, 8], F32) for i in range(2)])
                    gt_ = Rot([sb(ph, f"gt{i}", [128, 8], F32) for i in range(2)])
                    g2_ = Rot([sb(ph, f"g2{i}", [128, 8], F32) for i in range(2)])
                    gT_ = Rot([sb(ph, f"gT{i}", [8, 512], F32) for i in range(2)])
                for ti, (t0, w) in enumerate(TT):
                    if ti == 8 and not ctx_out:
                        continue
                    xt, xtb, ht, htb, hf, hfb = norm_tile(tiles, l, ti, 3, want_f32=is_moe)
                    P.op("pool", lambda e, ht=ht, t0=t0, w=w: e.dma_start(out=H2.rearrange("(kc k) t -> k kc t", k=128)[:, :, t0:t0 + w], in_=ht[:, :, 0:w]),
                         reads=[htb], pwrites=[H2b])
                    if is_moe:
                        gT, gTb = gT_.next()
                        for s in range(w // 128):
                            for kc in range(8):
                                P.op("tensor", lambda e, s=s, kc=kc: e.matmul(psum[0][:, 0:8], lhsT=hf[:, kc, s * 128:(s + 1) * 128], rhs=wr[:, kc, :],
                                                                             start=(kc == 0), stop=(kc == 7)), reads=[hfb, wrB], writes=[psb[0]])
                            lg, lgb = lg_.next()
                            mx, mxb = mx_.next()
                            gt, gtb = gt_.next()
                            g2, g2b = g2_.next()
                            P.op("vector", lambda e, lg=lg: e.tensor_copy(out=lg[:], in_=psum[0][:, 0:8]), reads=[psb[0]], writes=[lgb])
                            P.op("vector", lambda e, lg=lg, mx=mx: e.max(out=mx[:], in_=lg[:]), reads=[lgb], writes=[mxb])
                            P.op("vector", lambda e, mx=mx: e.tensor_tensor(out=mx[:, 2:3], in0=mx[:, 0:1], in1=mx[:, 1:2], op=ALU.subtract), writes=[mxb])
                            P.op("scalar", lambda e, mx=mx: e.activation(out=mx[:, 3:4], in_=mx[:, 2:3], func=AF.Sigmoid), reads=[mxb], writes=[mxb])
                            P.op("scalar", lambda e, mx=mx: e.activation(out=mx[:, 4:5], in_=mx[:, 2:3], func=AF.Sigmoid, scale=-1.0), writes=[mxb])
                            P.op("vector", lambda e, lg=lg, mx=mx, gt=gt: e.tensor_scalar(
                                out=gt[:], in0=lg[:], scalar1=mx[:, 0:1], scalar2=mx[:, 3:4], op0=ALU.is_equal, op1=ALU.mult), reads=[lgb, mxb], writes=[gtb])
                            P.op("vector", lambda e, lg=lg, mx=mx, g2=g2: e.tensor_scalar(
                                out=g2[:], in0=lg[:], scalar1=mx[:, 1:2], scalar2=mx[:, 4:5], op0=ALU.is_equal, op1=ALU.mult), reads=[lgb, mxb], writes=[g2b])
                            P.op("vector", lambda e, gt=gt, g2=g2: e.tensor_tensor(out=gt[:], in0=gt[:], in1=g2[:], op=ALU.add), reads=[g2b], writes=[gtb])
                            P.op("tensor", lambda e, gt=gt: e.transpose(out=psum[1][0:8, 0:128], in_=gt[:], identity=identf[:]), reads=[gtb, cB], writes=[psb[1]])
                            P.op("vector", lambda e, gT=gT, s=s: e.tensor_copy(out=gT[:, s * 128:(s + 1) * 128], in_=psum[1][0:8, 0:128]),
                                 reads=[psb[1]], writes=[gTb] if s == 0 else (), pwrites=[gTb] if s else ())
                        P.op("pool", lambda e, gT=gT, t0=t0, w=w: e.dma_start(out=GT[:, t0:t0 + w], in_=gT[:, 0:w]), reads=[gTb], pwrites=[GTb])

            if upto >= 7:
              with ExitStack() as ph:
                P.barrier()
                FH = DFF // 2
                w1r = Rot([sb(ph, f"w1h{i}", [128, 8, FH], BF16) for i in range(2)])
                w3r = Rot([sb(ph, f"w3h{i}", [128, 8, FH], BF16) for i in range(2)])
                w2r = Rot([sb(ph, f"w2h{i}", [128, 11, D], BF16) for i in range(2)])
                stg = Rot([sb(ph, f"stgF{i}", [128, FH], F32) for i in range(3)])
                h2r = Rot([sb(ph, f"h2r{i}", [128, 8, 512], BF16) for i in range(2)])
                ur = Rot([sb(ph, f"ur{i}", [128, 11, 512], BF16) for i in range(2)])
                sr = Rot([sb(ph, f"sr{i}", [128, 512], F32) for i in range(2)])
                cr = Rot([sb(ph, f"cr{i}", [128, 512], F32) for i in range(4)])
                gr = Rot([sb(ph, f"gr{i}", [128, 512], F32) for i in range(2)])
                pi = [0]

                def nps():
                    pi[0] = (pi[0] + 1) % 7
                    return [0, 1, 2, 3, 4, 5, 7][pi[0]]
                experts = list(range(NE)) if is_moe else [None]
                pendF = [None]
                cf = 0
                for ex in experts:
                    if ex is None:
                        W1, W3, W2 = ffn_w1[l2], ffn_w3[l2], ffn_w2[l2]
                    else:
                        W1, W3, W2 = moe_w1[l2, ex], moe_w3[l2, ex], moe_w2[l2, ex]
                    for half in range(2):
                        w1h, w1b = w1r.next()
                        w3h, w3b = w3r.next()
                        w2h, w2b = w2r.next()
                        for (Wsrc, wdst, wbuf) in [(W1, w1h, w1b), (W3, w3h, w3b)]:
                            for kc in range(8):
                                st_, stb = stg.next()
                                P.op("sync", lambda e, st_=st_, Wsrc=Wsrc, kc=kc, half=half: e.dma_start(
                                    out=st_[:], in_=Wsrc[kc * 128:(kc + 1) * 128, half * FH:(half + 1) * FH]), writes=[stb])
                                cf ^= 1
                                evac_copy(wdst[:, kc, :], st_[:], [stb], writes=[wbuf] if kc == 0 else (), pwrites=[wbuf] if kc else ())
                        for j in range(11):
                            st_, stb = stg.next()
                            P.op("sync", lambda e, st_=st_, j=j, half=half, W2=W2: e.dma_start(
                                out=st_[:, 0:D], in_=W2[half * FH + j * 128:half * FH + (j + 1) * 128, :]), writes=[stb])
                            cf ^= 1
                            evac_copy(w2h[:, j, :], st_[:, 0:D], [stb], writes=[w2b] if j == 0 else (), pwrites=[w2b] if j else ())
                        def stF1(ti):
                            t0, w = TT[ti]
                            h2t, h2tb = h2r.next()
                            P.op("sync", lambda e, h2t=h2t, t0=t0, w=w: e.dma_start(out=h2t[:, :, 0:w], in_=H2.rearrange("(kc k) t -> k kc t", k=128)[:, :, t0:t0 + w]),
                                 reads=[H2b], writes=[h2tb])
                            gtile = gtb_ = None
                            if ex is not None:
                                gtile, gtb_ = gr.next()
                                P.op("sync", lambda e, gtile=gtile, t0=t0, w=w: e.dma_start(out=gtile[:, 0:w], in_=GT[ex:ex + 1, t0:t0 + w].broadcast_to([128, w])),
                                     reads=[GTb], writes=[gtb_])
                            ut, utb = ur.next()
                            for j in range(11):
                                p1, p3 = nps(), nps()
                                for kc in range(8):
                                    P.op("tensor", lambda e, p1=p1, kc=kc, j=j: e.matmul(
                                        psum[p1][:, 0:w], lhsT=w1h[:, kc, j * 128:(j + 1) * 128], rhs=h2t[:, kc, 0:w], start=(kc == 0), stop=(kc == 7)),
                                        reads=[w1b, h2tb], writes=[psb[p1]])
                                for kc in range(8):
                                    P.op("tensor", lambda e, p3=p3, kc=kc, j=j: e.matmul(
                                        psum[p3][:, 0:w], lhsT=w3h[:, kc, j * 128:(j + 1) * 128], rhs=h2t[:, kc, 0:w], start=(kc == 0), stop=(kc == 7)),
                                        reads=[w3b, h2tb], writes=[psb[p3]])
                                s_, s_b = sr.next()
                                P.op("scalar", lambda e, s_=s_, p1=p1: e.activation(out=s_[:, 0:w], in_=psum[p1][:, 0:w], func=AF.Silu), reads=[psb[p1]], writes=[s_b])
                                P.op("vector", lambda e, s_=s_, p3=p3, j=j: e.tensor_tensor(out=ut[:, j, 0:w], in0=psum[p3][:, 0:w], in1=s_[:, 0:w], op=ALU.mult),
                                     reads=[psb[p3], s_b], writes=[utb] if j == 0 else (), pwrites=[utb] if j else ())
                            return (ti, ut, utb, gtile, gtb_, w2h, w2b, ex)

                        def stF2(st):
                            ti, ut, utb, gtile, gtb_, w2h_, w2b_, ex_ = st
                            t0, w = TT[ti]
                            col = 1 if ti == 8 else 0
                            for oc in range(8):
                                p = nps()
                                for j in range(11):
                                    P.op("tensor", lambda e, p=p, j=j, oc=oc: e.matmul(
                                        psum[p][:, 0:w], lhsT=w2h_[:, j, oc * 128:(oc + 1) * 128], rhs=ut[:, j, 0:w], start=(j == 0), stop=(j == 10)),
                                        reads=[w2b_, utb], writes=[psb[p]])
                                ct, ctb = cr.next()
                                if ex_ is None:
                                    P.op("scalar", lambda e, ct=ct, p=p, oc=oc: e.activation(
                                        out=ct[:, 0:w], in_=psum[p][:, 0:w], func=AF.Identity, scale=mv(l, col, 5)[:, oc:oc + 1]), reads=[psb[p], cB], writes=[ctb])
                                else:
                                    P.op("vector", lambda e, ct=ct, p=p, oc=oc: e.scalar_tensor_tensor(
                                        out=ct[:, 0:w], in0=psum[p][:, 0:w], scalar=mv(l, col, 5)[:, oc:oc + 1], in1=gtile[:, 0:w], op0=ALU.mult, op1=ALU.mult),
                                        reads=[psb[p], cB, gtb_], writes=[ctb])
                                P.op("pool", lambda e, ct=ct, oc=oc: e.dma_start(
                                    out=xT[oc * 128:(oc + 1) * 128, t0:t0 + w], in_=ct[:, 0:w], accum_op=ALU.add), reads=[ctb], pwrites=[xb[ti]])

                        for ti in range(len(TT)):
                            if ti == 8 and not ctx_out:
                                continue
                            st_new = stF1(ti)
                            if pendF[0] is not None:
                                stF2(pendF[0])
                            pendF[0] = st_new
                if pendF[0] is not None:
                    stF2(pendF[0])
                    pendF[0] = None

        if upto >= 8:
          with ExitStack() as ph:
            P.barrier()
            xr = Rot([sb(ph, f"fx{i}", [128, 8, 512], F32) for i in range(2)])
            sqr = Rot([sb(ph, "fsq", [128, 8, 512], BF16)])
            rsr = Rot([sb(ph, f"frs{i}", [128, 512], F32) for i in range(2)])
            for ti in range(8):
                t0, w = TT[ti]
                xt, xtb = xr.next()
                sq, sqb = sqr.next()
                rs, rsb = rsr.next()
                P.op("sync", lambda e, xt=xt, t0=t0: e.dma_start(out=xt[:], in_=xT.rearrange("(kc k) t -> k kc t", k=128)[:, :, t0:t0 + 512]),
                     reads=[xb[ti]], writes=[xtb])
                P.op("scalar", lambda e, xt=xt, sq=sq: e.activation(out=sq[:], in_=xt[:], func=AF.Square), reads=[xtb], writes=[sqb])
                for kc in range(8):
                    P.op("tensor", lambda e, kc=kc, sq=sq: e.matmul(psum[7][:], lhsT=onesb[:], rhs=sq[:, kc, :], start=(kc == 0), stop=(kc == 7)),
                         reads=[sqb, cB], writes=[psb[7]])
                P.op("scalar", lambda e, rs=rs: e.activation(out=rs[:], in_=psum[7][:], func=AF.Ln, scale=1.0 / D, bias=epsc[:, 0:1]), reads=[psb[7], cB], writes=[rsb])
                P.op("scalar", lambda e, rs=rs: e.activation(out=rs[:], in_=rs[:], func=AF.Exp, scale=-0.5), writes=[rsb])
                for kc in range(8):
                    P.op("vector", lambda e, kc=kc, xt=xt, rs=rs: e.scalar_tensor_tensor(
                        out=xt[:, kc, :], in0=xt[:, kc, :], scalar=nfin_sb[:, kc:kc + 1], in1=rs[:], op0=ALU.mult, op1=ALU.mult),
                        reads=[rsb, cB], writes=[xtb])
                P.op("pool", lambda e, xt=xt, t0=t0: e.dma_start(out=outT.rearrange("(kc k) t -> k kc t", k=128)[:, :, t0:t0 + 512], in_=xt[:]),
                     reads=[xtb], pwrites=[outb])
        P.finish([outb, FMb, TMb, BRb, H2b, GTb] + xb)
        print(f"[build] ops={P.n_ops} waits={P.n_waits}")
    return nc


_CONSTS = None


def _prep(inputs, ncores=8):
    global _CONSTS
    if _CONSTS is None:
        _CONSTS = _consts()
    f32 = lambda a: np.ascontiguousarray(np.asarray(a, dtype=np.float32))
    x, c, ctx, c_ctx = f32(inputs["x"]), f32(inputs["c"]), f32(inputs["ctx"]), f32(inputs["c_ctx"])

    def pk(v):
        v = f32(v)
        lead = v.shape[:-1]
        return np.ascontiguousarray(np.moveaxis(v.reshape(lead + (8, 128)), -1, 0))
    shared = {
        "w_mod": f32(inputs["w_mod"]),
        "b_mod": np.ascontiguousarray(np.moveaxis(f32(inputs["b_mod"]).reshape(DEPTH, 48, 128), -1, 0)),
        "nmix": pk(inputs["norm_mix"]), "nffn": pk(inputs["norm_ffn"]), "nfin": pk(inputs["norm_final"]),
        "w_in": f32(inputs["w_in"]),
        "sink": np.ascontiguousarray(np.broadcast_to(f32(inputs["attn_sink"]).reshape(1, -1), (128, DEPTH * 8))),
        "decs": np.ascontiguousarray(np.broadcast_to(
            np.stack([f32(inputs["ret_decay_fwd"]), f32(inputs["ret_decay_bwd"])], 1).reshape(1, -1), (128, DEPTH * 8))),
        "dlam": np.ascontiguousarray(np.broadcast_to(f32(inputs["diff_lambda"])[None], (128, DEPTH, 4, 64))),
        "dgain": np.ascontiguousarray(np.broadcast_to(f32(inputs["diff_norm"])[None], (128, DEPTH, 128))),
        "w_branch": f32(inputs["w_branch"]), "w_out": f32(inputs["w_out"]),
        "ffn_w1": f32(inputs["ffn_w1"]), "ffn_w3": f32(inputs["ffn_w3"]), "ffn_w2": f32(inputs["ffn_w2"]),
        "moe_router": f32(inputs["moe_router"]),
        "moe_w1": f32(inputs["moe_w1"]), "moe_w3": f32(inputs["moe_w3"]), "moe_w2": f32(inputs["moe_w2"]),
    }
    shared.update(_CONSTS)
    maps = []
    for b in range(ncores):
        m = dict(shared)
        m["xT_in"] = np.ascontiguousarray(np.concatenate([x[b], ctx[b]], 0).T)
        c2 = np.stack([c[b], c_ctx], -1)
        m["c2"] = np.ascontiguousarray(c2.reshape(8, 128, 2).transpose(1, 0, 2))
        maps.append(m)
    return maps


_NC = None


def kernel(**inputs):
    global _NC
    if _NC is None:
        _NC = build()
    maps = _prep(inputs)
    res = run_bass_kernel_spmd(_NC, maps, core_ids=list(range(8)))
    out = np.stack([np.ascontiguousarray(res.results[b]["outT"].T) for b in range(8)], 0)
    return out.astype(np.float32)
```

```python
import math
import os
from contextlib import ExitStack
import numpy as np
import ml_dtypes
import concourse.bass as bass
import concourse.mybir as mybir
from concourse.bass_utils import run_bass_kernel_spmd

F32 = mybir.dt.float32
BF16 = mybir.dt.bfloat16
AF = mybir.ActivationFunctionType
ALU = mybir.AluOpType

T = 4352
NLAT = 4096
NCTX = 256
D = 1024
DEPTH = 4
DIN = 6912
DFF = 2816
NE = 8
EPS = 1e-6
TT = [(i * 512, 512) for i in range(8)] + [(4096, 256)]
NFM = 41
NTM = 1664


class Buf:
    __slots__ = ("w", "r", "rp")

    def __init__(self):
        self.w = {}
        self.r = {}
        self.rp = {}


class EngState:
    def __init__(self, name, handle, sem, inc, stream):
        self.name, self.h, self.sem, self.inc, self.stream = name, handle, sem, inc, stream
        self.count = 0


class Prog:
    def __init__(self, nc, stack, nq=4):
        self.nc = nc
        self.engs = {}
        for name in ["tensor", "vector", "scalar", "gpsimd"]:
            sem = stack.enter_context(nc.semaphore("s_" + name))
            self.engs[name] = EngState(name, getattr(nc, name), sem, 1, name)
        self.dmaq = {}
        for qname, issuer, stream in [("sync", nc.sync, "sync"), ("pool", nc.gpsimd, "gpsimd"), ("actq", nc.scalar, "scalar")]:
            lst = []
            for i in range(nq):
                sem = stack.enter_context(nc.semaphore(f"d_{qname}{i}"))
                st = EngState(f"{qname}{i}", issuer, sem, 16, stream)
                lst.append(st)
                self.engs[st.name] = st
            self.dmaq[qname] = [lst, 0]
        self.known = {s: {} for s in ["tensor", "vector", "scalar", "gpsimd", "sync"]}
        self.n_ops = 0
        self.n_waits = 0

    def op(self, eng, fn, reads=(), writes=(), pwrites=()):
        if eng in self.dmaq:
            lst, k = self.dmaq[eng]
            st = lst[k % len(lst)]
            self.dmaq[eng][1] = k + 1
            is_dma = True
        else:
            st = self.engs[eng]
            is_dma = False
        deps = {}

        def add(d):
            for e, i in d.items():
                if deps.get(e, -1) < i:
                    deps[e] = i
        for b in reads:
            add(b.w)
        for b in writes:
            add(b.w)
            add(b.r)
            add(b.rp)
        for b in pwrites:
            if b.r:
                b.rp = dict(b.r)
                b.r = {}
                b.w = {}
            add(b.rp)
        if is_dma and st.count > 0:
            deps[st.name] = max(deps.get(st.name, -1), st.count - 1)
        known = self.known[st.stream]
        for e, i in deps.items():
            if (not is_dma) and e == st.name and e == "tensor":
                continue
            if known.get(e, -1) >= i:
                continue
            es = self.engs[e]
            st.h.wait_ge(es.sem, (i + 1) * es.inc)
            known[e] = i
            self.n_waits += 1
        ins = fn(st.h)
        ins.then_inc(st.sem, st.inc)
        idx = st.count
        st.count += 1
        self.n_ops += 1
        for b in reads:
            if b.r.get(st.name, -1) < idx:
                b.r[st.name] = idx
        for b in writes:
            b.w = {st.name: idx}
            b.r = {}
            b.rp = {}
        for b in pwrites:
            b.w[st.name] = idx
        return idx

    def barrier(self):
        hs = {"tensor": self.nc.tensor, "vector": self.nc.vector, "scalar": self.nc.scalar, "gpsimd": self.nc.gpsimd, "sync": self.nc.sync}
        for sname, h in hs.items():
            known = self.known[sname]
            for e, es in self.engs.items():
                if es.count == 0 or known.get(e, -1) >= es.count - 1:
                    continue
                if e == sname and e == "tensor":
                    continue
                h.wait_ge(es.sem, es.count * es.inc)
                known[e] = es.count - 1
                self.n_waits += 1

    def finish(self, bufs):
        for b in bufs:
            for e, i in b.w.items():
                es = self.engs[e]
                self.nc.sync.wait_ge(es.sem, (i + 1) * es.inc)


class Rot:
    def __init__(self, tiles):
        self.tiles = tiles
        self.bufs = [Buf() for _ in tiles]
        self.i = 0

    def next(self):
        k = self.i % len(self.tiles)
        self.i += 1
        return self.tiles[k], self.bufs[k]


def _consts():
    c = {}
    rows = NLAT // 64
    row = np.repeat(np.arange(rows, dtype=np.float32), 64)
    col = np.tile(np.arange(64, dtype=np.float32), rows)
    inv = (1.0 / (np.float32(10000.0) ** (np.arange(16, dtype=np.float32) / np.float32(16)))).astype(np.float32)
    ang = np.stack([row[:, None] * inv, col[:, None] * inv], axis=1).astype(np.float32)
    cos, sin = np.cos(ang).astype(np.float32), np.sin(ang).astype(np.float32)
    C = np.zeros((64, NLAT), np.float32)
    S = np.zeros((64, NLAT), np.float32)
    for d in range(64):
        ax, f = d // 32, d % 16
        C[d] = cos[:, ax, f]
        S[d] = sin[:, ax, f]
    c["ropeC"] = np.concatenate([C, C], 0)
    c["ropeS"] = np.concatenate([S, S], 0)
    R = np.zeros((64, 64), np.float32)
    for m in range(64):
        if (m % 32) < 16:
            R[m, m + 16] = -1.0
        else:
            R[m, m - 16] = 1.0
    R2 = np.zeros((128, 128), np.float32)
    R2[:64, :64] = R
    R2[64:, 64:] = R
    c["rmatT"] = R2.T.copy().astype(ml_dtypes.bfloat16)
    c["identb"] = np.eye(128, dtype=np.float32).astype(ml_dtypes.bfloat16)
    c["identf"] = np.eye(128, dtype=np.float32)
    c["onesb"] = np.ones((128, 128), np.float32).astype(ml_dtypes.bfloat16)
    k = np.arange(128)[:, None].astype(np.float32)
    q = np.arange(128)[None, :].astype(np.float32)
    ret = np.zeros((128, 8, 128), np.float32)
    ret[:, 0] = np.maximum(q - k, 0)
    ret[:, 1] = (q >= k)
    ret[:, 2] = np.maximum(k - q, 0)
    ret[:, 3] = (k >= q)
    ret[:, 4] = q + 1.0
    ret[:, 5] = 128.0 - q
    ret[:, 6, 0] = 127.0 - k[:, 0]
    ret[:, 6, 1] = k[:, 0]
    ret[:, 6, 2] = 128.0
    c["rettab"] = ret
    mprev = (k >= q).astype(np.float32)
    mnext = (k <= q).astype(np.float32)
    c["wmask"] = np.stack([np.tile(mprev, (1, 4)), np.tile(mnext, (1, 4))], 1).astype(ml_dtypes.bfloat16)
    return c


def build(dbg=False, upto=99, layers=DEPTH, sub=99):
    nc = bass.Bass("TRN2", target_bir_lowering=False)

    def din(name, shape, dt=F32):
        return nc.dram_tensor(name, list(shape), dt, kind="ExternalInput").ap()

    def dscr(name, shape, dt):
        return nc.dram_tensor(name, list(shape), dt, kind=("ExternalOutput" if dbg else "Internal")).ap()

    xT_in = din("xT_in", [D, T])
    c2 = din("c2", [128, 8, 2])
    w_mod = din("w_mod", [DEPTH, D, 6 * D])
    b_mod = din("b_mod", [128, DEPTH, 48])
    nmix = din("nmix", [128, DEPTH, 8])
    nffn = din("nffn", [128, DEPTH, 8])
    nfin = din("nfin", [128, 8])
    w_in = din("w_in", [DEPTH, D, DIN])
    sink = din("sink", [128, DEPTH * 8])
    decs = din("decs", [128, DEPTH * 8])
    dlam = din("dlam", [128, DEPTH, 4, 64])
    dgain = din("dgain", [128, DEPTH, 128])
    w_branch = din("w_branch", [DEPTH, 3, 512, D])
    w_out = din("w_out", [DEPTH, D, D])
    ffn_w1 = din("ffn_w1", [2, D, DFF])
    ffn_w3 = din("ffn_w3", [2, D, DFF])
    ffn_w2 = din("ffn_w2", [2, DFF, D])
    if layers >= 2:
        moe_router = din("moe_router", [2, D, NE])
        moe_w1 = din("moe_w1", [2, NE, D, DFF])
        moe_w3 = din("moe_w3", [2, NE, D, DFF])
        moe_w2 = din("moe_w2", [2, NE, DFF, D])
    ropeC_d = din("ropeC", [128, NLAT])
    ropeS_d = din("ropeS", [128, NLAT])
    rmatT_d = din("rmatT", [128, 128], BF16)
    identb_d = din("identb", [128, 128], BF16)
    identf_d = din("identf", [128, 128])
    onesb_d = din("onesb", [128, 128], BF16)
    rettab_d = din("rettab", [128, 8, 128])
    wmask_d = din("wmask", [128, 2, 512], BF16)

    outT = nc.dram_tensor("outT", [D, NLAT], F32, kind="ExternalOutput").ap()
    xT = dscr("xT", [D, T], F32)
    FM = dscr("FM", [NFM * 128, T], BF16)
    TM = dscr("TM", [T, NTM], BF16)
    BR = dscr("BR", [1536, T], BF16)
    H2 = dscr("H2", [D, T], BF16)
    GT = dscr("GT", [NE, T], F32)

    with ExitStack() as top:
        P = Prog(nc, top)
        ps01 = top.enter_context(nc.psum_tensor("ps01", [128, 1024], F32))
        ps23 = top.enter_context(nc.psum_tensor("ps23", [128, 1024], F32))
        psum = [ps01[:, 0:512], ps01[:, 512:1024], ps23[:, 0:512], ps23[:, 512:1024]]
        for i in range(4, 8):
            psum.append(top.enter_context(nc.psum_tensor(f"ps{i}", [128, 1024], BF16) if i == 6 else nc.psum_tensor(f"ps{i}", [128, 512], F32)))
        psb = [Buf() for _ in range(8)]

        uid = [0]

        def sb(stack, name, shape, dt):
            uid[0] += 1
            return stack.enter_context(nc.sbuf_tensor(f"{name}_s{uid[0]}", list(shape), dt))

        identb = sb(top, "identb", [128, 128], BF16)
        identf = sb(top, "identf", [128, 128], F32)
        onesb = sb(top, "onesb", [128, 128], BF16)
        rmatT = sb(top, "rmatT", [128, 128], BF16)
        modv = sb(top, "modv", [128, DEPTH, 2, 6, 8], F32)
        lgt = sb(top, "lgt", [128, DEPTH * 8], F32)
        esink = sb(top, "esink", [128, DEPTH * 8], F32)
        neglam = sb(top, "neglam", [128, DEPTH], F32)
        gainb = sb(top, "gainb", [128, DEPTH, 128], F32)
        nfin_sb = sb(top, "nfin_sb", [128, 8], F32)
        epsc = sb(top, "epsc", [128, 1], F32)
        cB = Buf()
        xb = [Buf() for _ in TT]
        FMb, TMb, BRb, H2b, GTb, outb = Buf(), Buf(), Buf(), Buf(), Buf(), Buf()

        for dst, src in [(identb, identb_d), (identf, identf_d), (onesb, onesb_d), (rmatT, rmatT_d), (nfin_sb, nfin)]:
            P.op("sync", lambda e, dst=dst, src=src: e.dma_start(out=dst[:], in_=src), pwrites=[cB])
        P.op("vector", lambda e: e.memset(epsc[:], EPS), pwrites=[cB])

        with ExitStack() as ph:
            P.barrier()
            xcp = Rot([sb(ph, f"xcp{i}", [128, 8, 512], F32) for i in range(2)])
            for ti, (t0, w) in enumerate(TT):
                tl, tb = xcp.next()
                P.op("sync", lambda e, tl=tl, t0=t0, w=w: e.dma_start(
                    out=tl[:, :, 0:w], in_=xT_in.rearrange("(kc k) t -> k kc t", k=128)[:, :, t0:t0 + w]), writes=[tb])
                P.op("pool", lambda e, tl=tl, t0=t0, w=w: e.dma_start(
                    out=xT.rearrange("(kc k) t -> k kc t", k=128)[:, :, t0:t0 + w], in_=tl[:, :, 0:w]), reads=[tb], writes=[xb[ti]])

            c_sb = sb(ph, "c_sb", [128, 8, 2], F32)
            cs_sb = sb(ph, "cs_sb", [128, 8, 2], F32)
            bmod_sb = sb(ph, "bmod_sb", [128, DEPTH, 48], F32)
            nmix_sb = sb(ph, "nmix_sb", [128, DEPTH, 8], F32)
            nffn_sb = sb(ph, "nffn_sb", [128, DEPTH, 8], F32)
            modT = sb(ph, "modT", [128, 48, 2], F32)
            sB, mB = Buf(), Buf()
            mtB = Buf()
            for dst, src in [(c_sb, c2), (bmod_sb, b_mod), (nmix_sb, nmix), (nffn_sb, nffn)]:
                P.op("sync", lambda e, dst=dst, src=src: e.dma_start(out=dst[:], in_=src), pwrites=[sB])
            P.op("scalar", lambda e: e.activation(out=cs_sb[:], in_=c_sb[:], func=AF.Silu), reads=[sB], writes=[mB])
            wm = Rot([sb(ph, f"wm{i}", [128, 8, 512], F32) for i in range(2)])
            for l in range(layers):
                for n in range(12):
                    wt, wb = wm.next()
                    P.op("sync", lambda e, wt=wt, l=l, n=n: e.dma_start(
                        out=wt[:], in_=w_mod[l].rearrange("(kc k) n -> k kc n", k=128)[:, :, n * 512:(n + 1) * 512]), writes=[wb])
                    for jj in range(4):
                        j = n * 4 + jj
                        for kc in range(8):
                            P.op("tensor", lambda e, wt=wt, jj=jj, kc=kc, j=j: e.matmul(
                                psum[0][:, 2 * j:2 * j + 2], lhsT=wt[:, kc, jj * 128:(jj + 1) * 128], rhs=cs_sb[:, kc, :],
                                start=(kc == 0), stop=(kc == 7)), reads=[wb, mB], writes=[psb[0]])
                for col in range(2):
                    P.op("vector", lambda e, col=col, l=l: e.tensor_tensor(
                        out=modT[:, :, col], in0=psum[0][:, 0:96].rearrange("p (j c) -> p j c", c=2)[:, :, col],
                        in1=bmod_sb[:, l, :], op=ALU.add), reads=[psb[0], sB], pwrites=[mtB])
                for col in range(2):
                    for kind, (src0, nrm) in enumerate([(8, nmix_sb), (0, None), (16, None), (32, nffn_sb), (24, None), (40, None)]):
                        if nrm is not None:
                            P.op("vector", lambda e, col=col, l=l, kind=kind, src0=src0, nrm=nrm: e.scalar_tensor_tensor(
                                out=modv[:, l, col, kind, :], in0=modT[:, src0:src0 + 8, col], scalar=1.0, in1=nrm[:, l, :],
                                op0=ALU.add, op1=ALU.mult), reads=[mtB, sB], pwrites=[cB])
                        else:
                            P.op("vector", lambda e, col=col, l=l, kind=kind, src0=src0: e.tensor_copy(
                                out=modv[:, l, col, kind, :], in_=modT[:, src0:src0 + 8, col]), reads=[mtB], pwrites=[cB])
            dtmp = sb(ph, "dtmp", [128, DEPTH * 8], F32)
            dtmp2 = sb(ph, "dtmp2", [128, DEPTH * 8], F32)
            dB, dB2 = Buf(), Buf()
            P.op("sync", lambda e: e.dma_start(out=dtmp[:], in_=decs), writes=[dB])
            P.op("scalar", lambda e: e.activation(out=dtmp2[:], in_=dtmp[:], func=AF.Exp, scale=-1.0), reads=[dB], writes=[dB2])
            P.op("scalar", lambda e: e.activation(out=dtmp2[:], in_=dtmp2[:], func=AF.Ln, bias=1.0), reads=[dB2], writes=[dB2])
            P.op("vector", lambda e: e.tensor_scalar(out=lgt[:], in0=dtmp2[:], scalar1=-1.0, scalar2=None, op0=ALU.mult),
                 reads=[dB2], pwrites=[cB])
            stmp = sb(ph, "stmp", [128, DEPTH * 8], F32)
            sB2 = Buf()
            P.op("sync", lambda e: e.dma_start(out=stmp[:], in_=sink), writes=[sB2])
            P.op("scalar", lambda e: e.activation(out=esink[:], in_=stmp[:], func=AF.Exp), reads=[sB2], pwrites=[cB])
            lam_sb = sb(ph, "lam_sb", [128, DEPTH, 4, 64], F32)
            lprod = sb(ph, "lprod", [128, DEPTH, 2, 64], F32)
            lsum = sb(ph, "lsum", [128, DEPTH, 2], F32)
            lB, lB2, lB3 = Buf(), Buf(), Buf()
            P.op("sync", lambda e: e.dma_start(out=lam_sb[:], in_=dlam), writes=[lB])
            P.op("vector", lambda e: e.tensor_tensor(
                out=lprod[:], in0=lam_sb[:].rearrange("p l (a b) d -> p l a b d", b=2)[:, :, :, 0, :],
                in1=lam_sb[:].rearrange("p l (a b) d -> p l a b d", b=2)[:, :, :, 1, :], op=ALU.mult), reads=[lB], writes=[lB2])
            P.op("vector", lambda e: e.tensor_reduce(out=lsum[:], in_=lprod[:], axis=mybir.AxisListType.X, op=ALU.add),
                 reads=[lB2], writes=[lB3])
            P.op("scalar", lambda e: e.activation(out=lsum[:], in_=lsum[:], func=AF.Exp), reads=[lB3], writes=[lB3])
            gn_sb = sb(ph, "gn_sb", [128, DEPTH, 128], F32)
            gB = Buf()
            P.op("sync", lambda e: e.dma_start(out=gn_sb[:], in_=dgain), writes=[gB])
            for l in range(DEPTH):
                lam_init = 0.8 - 0.6 * math.exp(-0.3 * l)
                P.op("vector", lambda e, l=l, lam_init=lam_init: e.scalar_tensor_tensor(
                    out=neglam[:, l:l + 1], in0=lsum[:, l, 1:2], scalar=-lam_init, in1=lsum[:, l, 0:1],
                    op0=ALU.add, op1=ALU.subtract), reads=[lB3], pwrites=[cB])
                P.op("vector", lambda e, l=l, lam_init=lam_init: e.tensor_scalar(
                    out=gainb[:, l, :], in0=gn_sb[:, l, :], scalar1=(1.0 - lam_init), scalar2=None, op0=ALU.mult),
                    reads=[gB], pwrites=[cB])

        def mv(l, col, kind):
            return modv[:, l, col, kind, :]

        def norm_tile(ph_tiles, l, ti, kind0, want_f32=False):
            t0, w = TT[ti]
            col = 1 if ti == 8 else 0
            xt, xtb = ph_tiles["x"].next()
            sq, sqb = ph_tiles["sq"].next()
            rs, rsb = ph_tiles["rs"].next()
            ht, htb = ph_tiles["h"].next()
            P.op("sync", lambda e: e.dma_start(out=xt[:, :, 0:w], in_=xT.rearrange("(kc k) t -> k kc t", k=128)[:, :, t0:t0 + w]),
                 reads=[xb[ti]], writes=[xtb])
            P.op("scalar", lambda e: e.activation(out=sq[:, :, 0:w], in_=xt[:, :, 0:w], func=AF.Square), reads=[xtb], writes=[sqb])
            for kc in range(8):
                P.op("tensor", lambda e, kc=kc: e.matmul(psum[7][:, 0:w], lhsT=onesb[:], rhs=sq[:, kc, 0:w], start=(kc == 0), stop=(kc == 7)),
                     reads=[sqb, cB], writes=[psb[7]])
            P.op("scalar", lambda e: e.activation(out=rs[:, 0:w], in_=psum[7][:, 0:w], func=AF.Ln, scale=1.0 / D, bias=epsc[:, 0:1]),
                 reads=[psb[7], cB], writes=[rsb])
            P.op("scalar", lambda e: e.activation(out=rs[:, 0:w], in_=rs[:, 0:w], func=AF.Exp, scale=-0.5), reads=[rsb], writes=[rsb])
            hf = hfb = None
            if want_f32:
                hf, hfb = ph_tiles["hf"].next()
            for kc in range(8):
                tmp, tmpb = ph_tiles["tmp"].next()
                P.op("vector", lambda e, kc=kc, tmp=tmp: e.scalar_tensor_tensor(
                    out=tmp[:, 0:w], in0=xt[:, kc, 0:w], scalar=mv(l, col, kind0)[:, kc:kc + 1], in1=rs[:, 0:w],
                    op0=ALU.mult, op1=ALU.mult), reads=[xtb, rsb, cB], writes=[tmpb])
                if want_f32:
                    P.op("scalar", lambda e, kc=kc, tmp=tmp: e.activation(
                        out=hf[:, kc, 0:w], in_=tmp[:, 0:w], func=AF.Identity, bias=mv(l, col, kind0 + 1)[:, kc:kc + 1]),
                        reads=[tmpb, cB], pwrites=[hfb])
                    P.op("vector", lambda e, kc=kc: e.tensor_copy(out=ht[:, kc, 0:w], in_=hf[:, kc, 0:w]), reads=[hfb], pwrites=[htb])
                else:
                    P.op("scalar", lambda e, kc=kc, tmp=tmp: e.activation(
                        out=ht[:, kc, 0:w], in_=tmp[:, 0:w], func=AF.Identity, bias=mv(l, col, kind0 + 1)[:, kc:kc + 1]),
                        reads=[tmpb, cB], pwrites=[htb])
            return xt, xtb, ht, htb, hf, hfb

        evac_flip = [0]

        def evac_copy(out_ap, in_ap, reads, writes=(), pwrites=()):
            evac_flip[0] ^= 1
            if evac_flip[0] or os.environ.get("CASTV"):
                P.op("vector", lambda e: e.tensor_copy(out=out_ap, in_=in_ap), reads=reads, writes=writes, pwrites=pwrites)
            else:
                P.op("scalar", lambda e: e.copy(out=out_ap, in_=in_ap), reads=reads, writes=writes, pwrites=pwrites)

        for l in range(layers):
            ctx_out = l < DEPTH - 1
            lam_init = 0.8 - 0.6 * math.exp(-0.3 * l)
            ntt = 9 if True else 8

            if upto >= 1:
              with ExitStack() as ph:
                P.barrier()
                t1r = Rot([sb(ph, f"t1r{i}", [128, 512], F32) for i in range(2)])
                t2r = Rot([sb(ph, f"t2r{i}", [128, 512], F32) for i in range(2)])
                hall = sb(ph, "hall", [128, 8, T], BF16)
                hallb = Buf()
                ropeC = sb(ph, "ropeC", [128, NLAT], F32)
                ropeS = sb(ph, "ropeS", [128, NLAT], F32)
                rB = Buf()
                P.op("sync", lambda e: e.dma_start(out=ropeC[:], in_=ropeC_d), pwrites=[rB])
                P.op("sync", lambda e: e.dma_start(out=ropeS[:], in_=ropeS_d), pwrites=[rB])
                with ExitStack() as ph2:
                    P.barrier()
                    tiles = {
                        "x": Rot([sb(ph2, f"p1x{i}", [128, 8, 512], F32) for i in range(2)]),
                        "sq": Rot([sb(ph2, "p1sq", [128, 8, 512], BF16)]),
                        "rs": Rot([sb(ph2, f"p1rs{i}", [128, 512], F32) for i in range(2)]),
                        "tmp": Rot([sb(ph2, f"p1tmp{i}", [128, 512], F32) for i in range(8)]),
                    }
                    for ti, (t0, w) in enumerate(TT):
                        class _H:
                            @staticmethod
                            def next(t0=t0):
                                return hall[:, :, t0:t0 + 512 if t0 < 4096 else T], hallb
                        tiles["h"] = _H
                        norm_tile(tiles, l, ti, 0)
                P.barrier()
                wst = Rot([sb(ph, f"wst{i}", [128, 8, 512], F32) for i in range(2)])
                wbf = Rot([sb(ph, f"wbf{i}", [128, 8, 512], BF16) for i in range(2)])
                ev = Rot([sb(ph, f"ev{i}", [128, 512], BF16) for i in range(4)])
                xbt = Rot([sb(ph, f"xbt{i}", [128, 512], BF16) for i in range(2)])
                segs = [("fm", 0, 512, 0, "rope"), ("fm", 512, 128, 4, "rope"), ("tm", 640, 128, 0, "copy"),
                        ("fm", 768, 256, 5, "rope"), ("fm", 1024, 256, 7, "rope"), ("tm", 1280, 512, 128, "copy"),
                        ("tm", 1792, 512, 640, "silu"), ("fm", 2304, 512, 9, "rope"), ("fm", 2816, 512, 13, "rope"),
                        ("tm", 3328, 512, 1152, "copy")] + [("fm", 3840 + 512 * i, 512, 17 + 4 * i, "sigmoid") for i in range(6)]
                pi = [0]

                def nps():
                    pi[0] = (pi[0] + 1) % 6
                    return pi[0]
                castflip = 0
                for si, (kind, c0, ncol, d0, mode) in enumerate(segs):
                    if si >= sub:
                        break
                    ws, wsb = wst.next()
                    wb_, wbb = wbf.next()
                    P.op("sync", lambda e, ws=ws, c0=c0, ncol=ncol: e.dma_start(
                        out=ws[:, :, 0:ncol], in_=w_in[l].rearrange("(kc k) n -> k kc n", k=128)[:, :, c0:c0 + ncol]), writes=[wsb])
                    for kc in range(8):
                        castflip ^= 1
                        evac_copy(wb_[:, kc, 0:ncol], ws[:, kc, 0:ncol], [wsb], pwrites=[wbb])
                    if kind == "fm":
                        for cj in range(ncol // 128):
                            for ti, (t0, w) in enumerate(TT):
                                p = nps()
                                for kc in range(8):
                                    P.op("tensor", lambda e, p=p, wb_=wb_, kc=kc, cj=cj, t0=t0, w=w: e.matmul(
                                        psum[p][:, 0:w], lhsT=wb_[:, kc, cj * 128:(cj + 1) * 128], rhs=hall[:, kc, t0:t0 + w],
                                        start=(kc == 0), stop=(kc == 7)), reads=[wbb, hallb], writes=[psb[p]])
                                et, etb = ev.next()
                                dst = FM[(d0 + cj) * 128:(d0 + cj + 1) * 128, t0:t0 + w]
                                if mode == "sigmoid":
                                    P.op("scalar", lambda e, p=p, et=et, w=w: e.activation(out=et[:, 0:w], in_=psum[p][:, 0:w], func=AF.Sigmoid),
                                         reads=[psb[p]], writes=[etb])
                                elif mode == "rope" and ti < 8:
                                    xq, xqb = xbt.next()
                                    t1, t1b = t1r.next()
                                    t2, t2b = t2r.next()
                                    p2 = nps()
                                    RS = int(os.environ.get("ROPE_STAGE", "5"))
                                    P.op("scalar", lambda e, p=p, xq=xq: e.copy(out=xq[:], in_=psum[p][:]), reads=[psb[p]], writes=[xqb])
                                    if RS >= 2:
                                        P.op("vector", lambda e, p=p, t1=t1, t0=t0: e.tensor_tensor(
                                            out=t1[:], in0=psum[p][:], in1=ropeC[:, t0:t0 + 512], op=ALU.mult), reads=[psb[p], rB, xqb], writes=[t1b])
                                    if RS >= 3:
                                        P.op("tensor", lambda e, p2=p2, xq=xq: e.matmul(psum[p2][:], lhsT=rmatT[:], rhs=xq[:], start=True, stop=True),
                                             reads=[xqb, cB], writes=[psb[p2]])
                                    if RS >= 4:
                                        P.op("vector", lambda e, p2=p2, t2=t2, t0=t0: e.tensor_tensor(
                                            out=t2[:], in0=psum[p2][:], in1=ropeS[:, t0:t0 + 512], op=ALU.mult), reads=[psb[p2], rB], writes=[t2b])
                                    if RS >= 5:
                                        P.op("vector", lambda e, et=et, t1=t1, t2=t2: e.tensor_tensor(out=et[:], in0=t1[:], in1=t2[:], op=ALU.add),
                                             reads=[t1b, t2b], writes=[etb])
                                    else:
                                        P.op("vector", lambda e, et=et, xq=xq: e.tensor_copy(out=et[:], in_=xq[:]), reads=[xqb], writes=[etb])
                                else:
                                    evac_copy(et[:, 0:w], psum[p][:, 0:w], [psb[p]], writes=[etb])
                                P.op("pool", lambda e, et=et, dst=dst, w=w: e.dma_start(out=dst, in_=et[:, 0:w]), reads=[etb], pwrites=[FMb])
                    else:
                        for s in range(T // 128):
                            p = nps()
                            for kc in range(8):
                                P.op("tensor", lambda e, p=p, wb_=wb_, kc=kc, s=s, ncol=ncol: e.matmul(
                                    psum[p][:, 0:ncol], lhsT=hall[:, kc, s * 128:(s + 1) * 128], rhs=wb_[:, kc, 0:ncol],
                                    start=(kc == 0), stop=(kc == 7)), reads=[wbb, hallb], writes=[psb[p]])
                            et, etb = ev.next()
                            if mode == "silu":
                                P.op("scalar", lambda e, p=p, et=et, ncol=ncol: e.activation(out=et[:, 0:ncol], in_=psum[p][:, 0:ncol], func=AF.Silu),
                                     reads=[psb[p]], writes=[etb])
                            else:
                                evac_copy(et[:, 0:ncol], psum[p][:, 0:ncol], [psb[p]], writes=[etb])
                            P.op("pool", lambda e, et=et, s=s, d0=d0, ncol=ncol: e.dma_start(
                                out=TM[s * 128:(s + 1) * 128, d0:d0 + ncol], in_=et[:, 0:ncol]), reads=[etb], pwrites=[TMb])

            qblocks = list(range(32)) + ([32, 33] if ctx_out else [])

            def transpose_store(ph_t, src_tile, src_buf, nchunks, row0, s, acc):
                for cc in range(nchunks):
                    P.op("tensor", lambda e, cc=cc: e.transpose(
                        out=ph_t["pst"][:, cc, :], in_=src_tile[:, cc * 128:(cc + 1) * 128], identity=identb[:]),
                        reads=[src_buf, cB], writes=[ph_t["pstb"]] if cc == 0 else (), pwrites=[ph_t["pstb"]] if cc > 0 else ())
                if acc["cnt"] == 0:
                    acc["tile"], acc["buf"] = ph_t["brt"].next()
                    acc["s0"] = s
                k = acc["cnt"]
                tile_, buf_ = acc["tile"], acc["buf"]
                evac_copy(tile_[:, 0:nchunks, k * 128:(k + 1) * 128], ph_t["pst"][:, 0:nchunks, :], [ph_t["pstb"]],
                          writes=[buf_] if k == 0 else (), pwrites=[buf_] if k > 0 else ())
                acc["cnt"] += 1
                last = (s == 31) or (s == 33)
                if acc["cnt"] == 4 or last:
                    n = acc["cnt"]
                    s0 = acc["s0"]
                    for cc in range(nchunks):
                        P.op("pool", lambda e, cc=cc, n=n, s0=s0, tile_=tile_: e.dma_start(
                            out=BR[row0 + cc * 128:row0 + (cc + 1) * 128, s0 * 128:(s0 + n) * 128], in_=tile_[:, cc, 0:n * 128]),
                            reads=[buf_], pwrites=[BRb])
                    acc["cnt"] = 0

            if upto >= 2:
              with ExitStack() as ph:
                P.barrier()
                kA = sb(ph, "kA", [64, T], BF16)
                vA = sb(ph, "vA", [128, 34, 65], BF16)
                qA = sb(ph, "qA", [64, 4, T], BF16)
                wmask = sb(ph, "wmask", [128, 2, 512], BF16)
                wmB = Buf()
                P.op("sync", lambda e: e.dma_start(out=wmask[:], in_=wmask_d), writes=[wmB])
                Er = Rot([sb(ph, f"EA{i}", [128, 512], BF16) for i in range(10)])
                oat = Rot([sb(ph, f"oat{i}", [128, 256], BF16) for i in range(3)])
                den = Rot([sb(ph, f"denA{i}", [128, 4], F32) for i in range(3)])
                ph_t = {"pst": psum[6][:].rearrange("p (c t) -> p c t", t=128)[:, 0:2, :], "pstb": psb[6],
                        "brt": Rot([sb(ph, f"brtA{i}", [128, 2, 512], BF16) for i in range(2)])}
                kvB = Buf()
                for g in range(2):
                    P.op("sync", lambda e, g=g: e.dma_start(out=kA[:], in_=FM[4 * 128 + g * 64:4 * 128 + (g + 1) * 64, :]), reads=[FMb], writes=[kvB])
                    P.op("sync", lambda e, g=g: e.dma_start(out=vA[:, :, 0:64], in_=TM[:, g * 64:(g + 1) * 64].rearrange("(j p) c -> p j c", p=128)),
                         reads=[TMb], pwrites=[kvB])
                    P.op("vector", lambda e: e.memset(vA[:, :, 64:65], 1.0), pwrites=[kvB])
                    for r in range(4):
                        hh = g * 4 + r
                        P.op("sync", lambda e, r=r, hh=hh: e.dma_start(out=qA[:, r, :], in_=FM[hh * 64:(hh + 1) * 64, :]), reads=[FMb], pwrites=[kvB])
                    acc = {"cnt": 0}
                    def stA1(i):
                        if i < 32:
                            keys = ([(i - 1, 0)] if i > 0 else []) + [(i, None)] + ([(i + 1, 1)] if i < 31 else []) + [(32, None), (33, None)]
                        else:
                            keys = [(32, None), (33, None)]
                        Es = []
                        for (j, mk) in keys:
                            p = 0 + (Er.i % 4)
                            P.op("tensor", lambda e, p=p, j=j, i=i: e.matmul(
                                psum[p][:], lhsT=kA[:, j * 128:(j + 1) * 128], rhs=qA[:, :, i * 128:(i + 1) * 128], start=True, stop=True),
                                reads=[kvB], writes=[psb[p]])
                            Et, Eb = Er.next()
                            P.op("scalar", lambda e, p=p, Et=Et: e.activation(out=Et[:], in_=psum[p][:], func=AF.Exp, scale=0.125),
                                 reads=[psb[p]], writes=[Eb])
                            if mk is not None:
                                P.op("vector", lambda e, Et=Et, mk=mk: e.tensor_tensor(out=Et[:], in0=Et[:], in1=wmask[:, mk, :], op=ALU.mult),
                                     reads=[wmB], writes=[Eb])
                            Es.append((j, Et, Eb))
                        return Es

                    def stA2(i, Es):
                        po = 4 + (i % 2)
                        pov = psum[po][:, 0:260].rearrange("p (r c) -> p r c", c=65)
                        for r in range(4):
                            for n_, (j, Et, Eb) in enumerate(Es):
                                P.op("tensor", lambda e, r=r, j=j, Et=Et, n_=n_, pov=pov: e.matmul(
                                    pov[:, r, :], lhsT=Et[:, r * 128:(r + 1) * 128], rhs=vA[:, j, :], start=(n_ == 0), stop=(n_ == len(Es) - 1)),
                                    reads=[Eb, kvB], writes=[psb[po]])
                        dn, dnb = den.next()
                        P.op("vector", lambda e, dn=dn, pov=pov, g=g: e.tensor_tensor(
                            out=dn[:], in0=pov[:, :, 64], in1=esink[:, l * 8 + g * 4:l * 8 + g * 4 + 4], op=ALU.add),
                            reads=[psb[po], cB], writes=[dnb])
                        P.op("vector", lambda e, dn=dn: e.reciprocal(out=dn[:], in_=dn[:]), writes=[dnb])
                        ot, otb = oat.next()
                        for r in range(4):
                            P.op("vector", lambda e, r=r, ot=ot, pov=pov, dn=dn: e.tensor_scalar(
                                out=ot[:, r * 64:(r + 1) * 64], in0=pov[:, r, 0:64], scalar1=dn[:, r:r + 1], scalar2=None, op0=ALU.mult),
                                reads=[psb[po], dnb], writes=[otb] if r == 0 else (), pwrites=[otb] if r > 0 else ())
                        return ot, otb

                    nb = len(qblocks)
                    stash1, stash2 = {}, {}
                    for k in range(nb + 2):
                        if k < nb:
                            stash1[k] = stA1(qblocks[k])
                        if 0 <= k - 1 < nb:
                            stash2[k - 1] = stA2(qblocks[k - 1], stash1.pop(k - 1))
                        if 0 <= k - 2 < nb:
                            ot, otb = stash2.pop(k - 2)
                            transpose_store(ph_t, ot, otb, 2, g * 256, qblocks[k - 2], acc)

            if upto >= 3:
              with ExitStack() as ph:
                P.barrier()
                rt = sb(ph, "rt", [128, 8, 128], F32)
                rtB = Buf()
                P.op("sync", lambda e: e.dma_start(out=rt[:], in_=rettab_d), writes=[rtB])
                qB_ = sb(ph, "qB", [64, T], BF16)
                kB_ = sb(ph, "kB", [64, T], BF16)
                vB_ = sb(ph, "vB", [128, 34, 128], BF16)
                gB_ = sb(ph, "gB", [128, 34, 128], BF16)
                MT = sb(ph, "MT", [128, 128], F32)
                MT2 = sb(ph, "MT2", [128, 128], F32)
                kd = sb(ph, "kd", [128, 4], F32)
                qdf = sb(ph, "qdf", [64, 128], BF16)
                qdb = sb(ph, "qdb", [64, 128], BF16)
                kvF = sb(ph, "kvF", [64, 34, 128], F32)
                kvBk = sb(ph, "kvBk", [64, 34, 128], F32)
                Sin = sb(ph, "Sin", [64, 34, 128], F32)
                Tin = sb(ph, "Tin", [64, 34, 128], F32)
                SinB = sb(ph, "SinB", [64, 34, 128], BF16)
                TinB = sb(ph, "TinB", [64, 34, 128], BF16)
                Kf = Rot([sb(ph, f"Kf{i}", [128, 64], BF16) for i in range(2)])
                Kb = Rot([sb(ph, f"Kb{i}", [128, 64], BF16) for i in range(2)])
                Sm = Rot([sb(ph, f"Sm{i}", [128, 128], BF16) for i in range(3)])
                Qf = Rot([sb(ph, f"Qf{i}", [64, 128], BF16) for i in range(3)])
                Qb = Rot([sb(ph, f"Qb{i}", [64, 128], BF16) for i in range(3)])
                obt = Rot([sb(ph, f"obt{i}", [128, 128], BF16) for i in range(3)])
                ssr = Rot([sb(ph, f"ssr{i}", [128, 2], F32) for i in range(3)])
                junk = sb(ph, "junkB", [128, 128], F32)
                ph_t = {"pst": psum[6][:].rearrange("p (c t) -> p c t", t=128)[:, 0:1, :], "pstb": psb[6],
                        "brt": Rot([sb(ph, f"brtB{i}", [128, 1, 512], BF16) for i in range(2)])}
                ldB, tbB, kvb_, scB = Buf(), Buf(), Buf(), Buf()
                ptbufs = [Buf(), Buf()]
                for h in range(4):
                    ch, half = h // 2, h % 2
                    P.op("sync", lambda e, ch=ch, half=half: e.dma_start(out=qB_[:], in_=FM[(5 + ch) * 128 + half * 64:(5 + ch) * 128 + half * 64 + 64, :]),
                         reads=[FMb], writes=[ldB])
                    P.op("sync", lambda e, ch=ch, half=half: e.dma_start(out=kB_[:], in_=FM[(7 + ch) * 128 + half * 64:(7 + ch) * 128 + half * 64 + 64, :]),
                         reads=[FMb], pwrites=[ldB])
                    P.op("sync", lambda e, h=h: e.dma_start(out=vB_[:], in_=TM[:, 128 + h * 128:128 + (h + 1) * 128].rearrange("(j p) c -> p j c", p=128)),
                         reads=[TMb], pwrites=[ldB])
                    P.op("sync", lambda e, h=h: e.dma_start(out=gB_[:], in_=TM[:, 640 + h * 128:640 + (h + 1) * 128].rearrange("(j p) c -> p j c", p=128)),
                         reads=[TMb], pwrites=[ldB])
                    lf = lgt[:, l * 8 + h:l * 8 + h + 1]
                    lb = lgt[:, l * 8 + 4 + h:l * 8 + 4 + h + 1]
                    P.op("scalar", lambda e, lf=lf: e.activation(out=MT[:], in_=rt[:, 0, :], func=AF.Exp, scale=lf), reads=[rtB, cB], writes=[tbB])
                    P.op("scalar", lambda e, lb=lb: e.activation(out=MT2[:], in_=rt[:, 2, :], func=AF.Exp, scale=lb), reads=[rtB, cB], pwrites=[tbB])
                    P.op("vector", lambda e: e.tensor_tensor(out=MT[:], in0=MT[:], in1=rt[:, 1, :], op=ALU.mult), reads=[tbB, rtB], writes=[tbB])
                    P.op("vector", lambda e: e.tensor_tensor(out=MT2[:], in0=MT2[:], in1=rt[:, 3, :], op=ALU.mult), reads=[tbB], writes=[tbB])
                    P.op("vector", lambda e: e.scalar_tensor_tensor(out=MT[:], in0=MT[:], scalar=0.125, in1=MT2[:], op0=ALU.mult, op1=ALU.add),
                         reads=[tbB], writes=[tbB])
                    P.op("vector", lambda e: e.scalar_tensor_tensor(out=MT[:], in0=MT2[:], scalar=-0.875, in1=MT[:], op0=ALU.mult, op1=ALU.add),
                         reads=[tbB], writes=[tbB])
                    P.op("scalar", lambda e, lf=lf: e.activation(out=kd[:, 0:1], in_=rt[:, 6, 0:1], func=AF.Exp, scale=lf), reads=[tbB], writes=[tbB])
                    P.op("scalar", lambda e, lb=lb: e.activation(out=kd[:, 1:2], in_=rt[:, 6, 1:2], func=AF.Exp, scale=lb), reads=[tbB], writes=[tbB])
                    P.op("scalar", lambda e, lf=lf: e.activation(out=kd[:, 2:3], in_=rt[:, 6, 2:3], func=AF.Exp, scale=lf), reads=[tbB], writes=[tbB])
                    P.op("scalar", lambda e, lb=lb: e.activation(out=kd[:, 3:4], in_=rt[:, 6, 2:3], func=AF.Exp, scale=lb), reads=[tbB], writes=[tbB])
                    P.op("vector", lambda e: e.tensor_scalar(out=kd[:, 0:2], in0=kd[:, 0:2], scalar1=0.125, scalar2=None, op0=ALU.mult), writes=[tbB])
                    P.op("scalar", lambda e, lf=lf: e.activation(out=qdf[:], in_=rt[0:64, 4, :], func=AF.Exp, scale=lf[0:64]), reads=[tbB], writes=[tbB])
                    P.op("scalar", lambda e, lb=lb: e.activation(out=qdb[:], in_=rt[0:64, 5, :], func=AF.Exp, scale=lb[0:64]), reads=[tbB], writes=[tbB])
                    def stP1(n):
                        pt = psum[6][:, 0:64] if n % 2 == 0 else psum[7][:, 0:32].bitcast(BF16)
                        ptb = psb[6] if n % 2 == 0 else psb[7]
                        P.op("tensor", lambda e, n=n, pt=pt: e.transpose(out=pt, in_=kB_[:, n * 128:(n + 1) * 128], identity=identb[0:64, 0:64]),
                             reads=[ldB, cB], writes=[ptb])
                        kf, kfb = Kf.next()
                        kb, kbb = Kb.next()
                        P.op("vector", lambda e, kf=kf, pt=pt: e.tensor_scalar(out=kf[:], in0=pt, scalar1=kd[:, 0:1], scalar2=None, op0=ALU.mult),
                             reads=[ptb, tbB], writes=[kfb])
                        P.op("vector", lambda e, kb=kb, pt=pt: e.tensor_scalar(out=kb[:], in0=pt, scalar1=kd[:, 1:2], scalar2=None, op0=ALU.mult),
                             reads=[ptb, tbB], writes=[kbb])
                        return kf, kfb, kb, kbb

                    def stP2(n, st):
                        kf, kfb, kb, kbb = st
                        pa, pb = n % 2, 2 + n % 2
                        P.op("tensor", lambda e, n=n, kf=kf, pa=pa: e.matmul(psum[pa][0:64, 0:128], lhsT=kf[:], rhs=vB_[:, n, :], start=True, stop=True),
                             reads=[kfb, ldB], writes=[psb[pa]])
                        P.op("tensor", lambda e, n=n, kb=kb, pb=pb: e.matmul(psum[pb][0:64, 0:128], lhsT=kb[:], rhs=vB_[:, n, :], start=True, stop=True),
                             reads=[kbb, ldB], writes=[psb[pb]])
                        P.op("scalar", lambda e, n=n, pa=pa: e.copy(out=kvF[:, n, :], in_=psum[pa][0:64, 0:128]), reads=[psb[pa]], pwrites=[kvb_])
                        P.op("scalar", lambda e, n=n, pb=pb: e.copy(out=kvBk[:, n, :], in_=psum[pb][0:64, 0:128]), reads=[psb[pb]], pwrites=[kvb_])

                    shp = {}
                    for k in range(35):
                        if k < 34:
                            shp[k] = stP1(k)
                        if k >= 1:
                            stP2(k - 1, shp.pop(k - 1))
                    G, Gb = kd[0:64, 2:3], kd[0:64, 3:4]
                    P.op("vector", lambda e: e.memset(Sin[:, 32, :], 0.0), reads=[kvb_], writes=[scB])
                    P.op("vector", lambda e: e.memset(Tin[:, 33, :], 0.0), writes=[scB])
                    P.op("vector", lambda e: e.tensor_copy(out=Sin[:, 33, :], in_=kvF[:, 32, :]), reads=[kvb_], writes=[scB])
                    P.op("vector", lambda e: e.tensor_copy(out=Tin[:, 32, :], in_=kvBk[:, 33, :]), reads=[kvb_], writes=[scB])
                    P.op("vector", lambda e: e.scalar_tensor_tensor(out=Sin[:, 0, :], in0=Sin[:, 33, :], scalar=G, in1=kvF[:, 33, :], op0=ALU.mult, op1=ALU.add),
                         reads=[tbB, kvb_], writes=[scB])
                    P.op("vector", lambda e: e.scalar_tensor_tensor(out=Tin[:, 31, :], in0=Tin[:, 32, :], scalar=Gb, in1=kvBk[:, 32, :], op0=ALU.mult, op1=ALU.add),
                         reads=[kvb_], writes=[scB])
                    for n in range(31):
                        P.op("vector", lambda e, n=n: e.scalar_tensor_tensor(
                            out=Sin[:, n + 1, :], in0=Sin[:, n, :], scalar=G, in1=kvF[:, n, :], op0=ALU.mult, op1=ALU.add), reads=[kvb_], writes=[scB])
                        m = 31 - n
                        P.op("vector", lambda e, m=m: e.scalar_tensor_tensor(
                            out=Tin[:, m - 1, :], in0=Tin[:, m, :], scalar=Gb, in1=kvBk[:, m, :], op0=ALU.mult, op1=ALU.add), reads=[kvb_], writes=[scB])
                    P.op("vector", lambda e: e.tensor_copy(out=SinB[:], in_=Sin[:]), writes=[scB])
                    P.op("vector", lambda e: e.tensor_copy(out=TinB[:], in_=Tin[:]), reads=[scB], pwrites=[scB])
                    scB2 = scB
                    acc = {"cnt": 0}

                    def stB1(n):
                        ps_ = 0 + n % 2
                        P.op("tensor", lambda e, n=n, ps_=ps_: e.matmul(psum[ps_][:, 0:128], lhsT=kB_[:, n * 128:(n + 1) * 128], rhs=qB_[:, n * 128:(n + 1) * 128],
                                                                     start=True, stop=True), reads=[ldB], writes=[psb[ps_]])
                        sm, smb = Sm.next()
                        P.op("vector", lambda e, sm=sm, ps_=ps_: e.tensor_tensor(out=sm[:], in0=psum[ps_][:, 0:128], in1=MT[:], op=ALU.mult),
                             reads=[psb[ps_], tbB], writes=[smb])
                        qf, qfb = Qf.next()
                        qb, qbb = Qb.next()
                        P.op("vector", lambda e, n=n, qf=qf: e.tensor_tensor(out=qf[:], in0=qB_[:, n * 128:(n + 1) * 128], in1=qdf[:], op=ALU.mult),
                             reads=[ldB, tbB], writes=[qfb])
                        P.op("vector", lambda e, n=n, qb=qb: e.tensor_tensor(out=qb[:], in0=qB_[:, n * 128:(n + 1) * 128], in1=qdb[:], op=ALU.mult),
                             reads=[ldB, tbB], writes=[qbb])
                        return (sm, smb, qf, qfb, qb, qbb)

                    def stB2(n, st):
                        sm, smb, qf, qfb, qb, qbb = st
                        po_ = 2 + n % 2
                        P.op("tensor", lambda e, n=n, sm=sm, po_=po_: e.matmul(psum[po_][:, 0:128], lhsT=sm[:], rhs=vB_[:, n, :], start=True, stop=False),
                             reads=[smb, ldB], writes=[psb[po_]])
                        P.op("tensor", lambda e, n=n, qf=qf, po_=po_: e.matmul(psum[po_][:, 0:128], lhsT=qf[:], rhs=SinB[:, n, :], start=False, stop=False),
                             reads=[qfb, scB2], writes=[psb[po_]])
                        P.op("tensor", lambda e, n=n, qb=qb, po_=po_: e.matmul(psum[po_][:, 0:128], lhsT=qb[:], rhs=TinB[:, n, :], start=False, stop=True),
                             reads=[qbb, scB2], writes=[psb[po_]])
                        ss, ssb = ssr.next()
                        P.op("scalar", lambda e, ss=ss, po_=po_: e.activation(out=junk[:], in_=psum[po_][:, 0:128], func=AF.Square, accum_out=ss[:, 0:1]),
                             reads=[psb[po_]], writes=[ssb])
                        P.op("scalar", lambda e, ss=ss: e.activation(out=ss[:, 1:2], in_=ss[:, 0:1], func=AF.Ln, scale=1.0 / 128, bias=epsc[:, 0:1]), writes=[ssb])
                        P.op("scalar", lambda e, ss=ss: e.activation(out=ss[:, 1:2], in_=ss[:, 1:2], func=AF.Exp, scale=-0.5), writes=[ssb])
                        ob, obb = obt.next()
                        P.op("vector", lambda e, n=n, ob=ob, ss=ss, po_=po_: e.scalar_tensor_tensor(
                            out=ob[:], in0=psum[po_][:, 0:128], scalar=ss[:, 1:2], in1=gB_[:, n, :], op0=ALU.mult, op1=ALU.mult),
                            reads=[psb[po_], ssb, ldB], writes=[obb])
                        return ob, obb

                    nb = len(qblocks)
                    sh1, sh2 = {}, {}
                    for k in range(nb + 2):
                        if k < nb:
                            sh1[k] = stB1(qblocks[k])
                        if 0 <= k - 1 < nb:
                            sh2[k - 1] = stB2(qblocks[k - 1], sh1.pop(k - 1))
                        if 0 <= k - 2 < nb:
                            ob, obb = sh2.pop(k - 2)
                            transpose_store(ph_t, ob, obb, 1, 512 + h * 128, qblocks[k - 2], acc)

            if upto >= 4:
              with ExitStack() as ph:
                P.barrier()
                Osb = [sb(ph, f"Osb{c}", [128, 4, 129], F32) for c in range(2)]
                Osbb = [Buf(), Buf()]
                t1c = Rot([sb(ph, f"t1c{i}", [128, 128], F32) for i in range(2)])
                occ = Rot([sb(ph, f"occ{i}", [128, 128], F32) for i in range(2)])
                junk = sb(ph, "junkC", [128, 128], F32)
                kD2 = sb(ph, "kD2", [128, T], BF16)
                qD = [sb(ph, f"qD{c}", [128, T], BF16) for c in range(2)]
                vD = sb(ph, "vD", [128, 34, 129], BF16)
                E = [sb(ph, f"ED{c}", [128, 34, 512], BF16) for c in range(2)]
                Ebuf = [Buf(), Buf()]
                oct_ = Rot([sb(ph, f"oct{i}", [128, 128], BF16) for i in range(2)])
                rcp = Rot([sb(ph, f"rcp{i}", [128, 4], F32) for i in range(2)])
                ph_t = {"pst": psum[6][:].rearrange("p (c t) -> p c t", t=128)[:, 0:1, :], "pstb": psb[6],
                        "brt": Rot([sb(ph, f"brtC{i}", [128, 1, 512], BF16) for i in range(2)])}
                ldB, zB = Buf(), Buf()
                P.op("vector", lambda e: e.memset(qD[0][64:128, :], 0.0), pwrites=[zB])
                P.op("vector", lambda e: e.memset(qD[1][0:64, :], 0.0), pwrites=[zB])
                P.op("vector", lambda e: e.memset(vD[:, :, 128:129], 1.0), pwrites=[zB])
                for h in range(4):
                    P.op("sync", lambda e, h=h: e.dma_start(out=kD2[:], in_=FM[(13 + h) * 128:(14 + h) * 128, :]), reads=[FMb], writes=[ldB])
                    P.op("sync", lambda e, h=h: e.dma_start(out=qD[0][0:64, :], in_=FM[(9 + h) * 128:(9 + h) * 128 + 64, :]), reads=[FMb], pwrites=[ldB])
                    P.op("sync", lambda e, h=h: e.dma_start(out=qD[1][64:128, :], in_=FM[(9 + h) * 128 + 64:(10 + h) * 128, :]), reads=[FMb], pwrites=[ldB])
                    P.op("sync", lambda e, h=h: e.dma_start(out=vD[:, :, 0:128], in_=TM[:, 1152 + h * 128:1152 + (h + 1) * 128].rearrange("(j p) c -> p j c", p=128)),
                         reads=[TMb], pwrites=[ldB])
                    acc = {"cnt": 0}
                    PVB = [4, 4, 5, 5]
                    pairs = [(ps01, 0, 1), (ps23, 2, 3)]
                    def finish_unit(ti, c):
                        t0, w = TT[ti]
                        nr = w // 128
                        for r in range(nr):
                            P.op("vector", lambda e, r=r, c=c: e.tensor_copy(out=Osb[c][:, r, :], in_=psum[PVB[r]][:, (r % 2) * 129:(r % 2) * 129 + 129]),
                                 reads=[psb[PVB[r]]], writes=[Osbb[c]] if r == 0 else (), pwrites=[Osbb[c]] if r else ())
                        if c == 0:
                            return
                        for r in range(nr):
                            o1 = Osb[0][:, r, :]
                            o2 = Osb[1][:, r, :]
                            rc, rcb = rcp.next()
                            P.op("vector", lambda e, rc=rc, o1=o1: e.reciprocal(out=rc[:, 0:1], in_=o1[:, 128:129]), reads=[Osbb[0]], writes=[rcb])
                            P.op("vector", lambda e, rc=rc, o2=o2: e.reciprocal(out=rc[:, 1:2], in_=o2[:, 128:129]), reads=[Osbb[1]], writes=[rcb])
                            P.op("vector", lambda e, rc=rc: e.tensor_tensor(out=rc[:, 1:2], in0=rc[:, 1:2], in1=neglam[:, l:l + 1], op=ALU.mult),
                                 reads=[cB], writes=[rcb])
                            t1, t1b = t1c.next()
                            oc_, ocb = occ.next()
                            P.op("vector", lambda e, t1=t1, rc=rc, o1=o1: e.tensor_scalar(out=t1[:], in0=o1[:, 0:128], scalar1=rc[:, 0:1], scalar2=None, op0=ALU.mult),
                                 reads=[Osbb[0], rcb], writes=[t1b])
                            P.op("vector", lambda e, t1=t1, rc=rc, o2=o2, oc_=oc_: e.scalar_tensor_tensor(
                                out=oc_[:], in0=o2[:, 0:128], scalar=rc[:, 1:2], in1=t1[:], op0=ALU.mult, op1=ALU.add),
                                reads=[Osbb[1], rcb, t1b], writes=[ocb])
                            P.op("scalar", lambda e, rc=rc, oc_=oc_: e.activation(out=junk[:], in_=oc_[:], func=AF.Square, accum_out=rc[:, 2:3]),
                                 reads=[ocb], writes=[rcb])
                            P.op("scalar", lambda e, rc=rc: e.activation(out=rc[:, 3:4], in_=rc[:, 2:3], func=AF.Ln, scale=1.0 / 128, bias=epsc[:, 0:1]), writes=[rcb])
                            P.op("scalar", lambda e, rc=rc: e.activation(out=rc[:, 3:4], in_=rc[:, 3:4], func=AF.Exp, scale=-0.5), writes=[rcb])
                            ot, otb = oct_.next()
                            P.op("vector", lambda e, ot=ot, oc_=oc_, rc=rc: e.scalar_tensor_tensor(
                                out=ot[:], in0=oc_[:], scalar=rc[:, 3:4], in1=gainb[:, l, :], op0=ALU.mult, op1=ALU.mult),
                                reads=[ocb, rcb, cB], writes=[otb])
                            transpose_store(ph_t, ot, otb, 1, 1024 + h * 128, t0 // 128 + r, acc)


                    units = []
                    for ti, (t0, w) in enumerate(TT):
                        if ti == 8 and not ctx_out:
                            continue
                        for c in range(2):
                            units.append((ti, c))

                    def pv_ops(u_idx):
                        ti, c = units[u_idx]
                        t0, w = TT[ti]
                        keys = list(range(34)) if ti < 8 else [32, 33]
                        ops = []
                        for r in range(w // 128):
                            for n_, j in enumerate(keys):
                                ops.append((r, j, n_ == 0, n_ == len(keys) - 1, c))
                        return ops

                    def emit_pv(op):
                        r, j, st_, sp_, eb = op
                        P.op("tensor", lambda e: e.matmul(
                            psum[PVB[r]][:, (r % 2) * 129:(r % 2) * 129 + 129], lhsT=E[eb][:, j, r * 128:(r + 1) * 128], rhs=vD[:, j, :], start=st_, stop=sp_),
                            reads=[Ebuf[eb], ldB, zB], writes=[psb[PVB[r]]])

                    pcount = 0
                    for u_idx in range(len(units) + 1):
                        pend = pv_ops(u_idx - 1) if u_idx >= 1 else []
                        if u_idx < len(units):
                            ti, c = units[u_idx]
                            t0, w = TT[ti]
                            keys = list(range(34)) if ti < 8 else [32, 33]
                            npairs = len(keys) // 2
                            per = -(-len(pend) // npairs) if pend else 0
                            for jp in range(0, len(keys), 2):
                                pt_, pa_, pb_ = pairs[pcount % 2]
                                pcount += 1
                                for hh, pbk in ((0, pa_), (1, pb_)):
                                    j = keys[jp + hh]
                                    P.op("tensor", lambda e, c=c, j=j, pbk=pbk: e.matmul(
                                        psum[pbk][:, 0:w], lhsT=kD2[:, j * 128:(j + 1) * 128], rhs=qD[c][:, t0:t0 + w], start=True, stop=True),
                                        reads=[ldB, zB], writes=[psb[pbk]])
                                j0 = keys[jp]
                                P.op("scalar", lambda e, c=c, j0=j0, pt_=pt_: e.activation(
                                    out=E[c][:, j0:j0 + 2, 0:w], in_=pt_[:].rearrange("p (a b) -> p a b", b=512)[:, :, 0:w], func=AF.Exp, scale=0.125),
                                    reads=[psb[pa_], psb[pb_]], writes=[Ebuf[c]] if jp == 0 else (), pwrites=[Ebuf[c]] if jp else ())
                                for _ in range(per):
                                    if pend:
                                        emit_pv(pend.pop(0))
                        while pend:
                            emit_pv(pend.pop(0))
                        if u_idx >= 1:
                            finish_unit(*units[u_idx - 1])

            if upto >= 5:
              with ExitStack() as ph:
                P.barrier()
                wbr = sb(ph, "wbr", [128, 12, D], BF16)
                wo = sb(ph, "wo", [128, 8, D], BF16)
                wB = Buf()
                stg = Rot([sb(ph, f"stgM{i}", [128, D], F32) for i in range(3)])
                cf = 0
                for k in range(20):
                    st_, stb = stg.next()
                    src = w_branch[l].rearrange("i (kc k) n -> k (i kc) n", k=128)[:, k, :] if k < 12 else \
                        w_out[l].rearrange("(kc k) n -> k kc n", k=128)[:, k - 12, :]
                    dstw = wbr[:, k, :] if k < 12 else wo[:, k - 12, :]
                    P.op("sync", lambda e, st_=st_, src=src: e.dma_start(out=st_[:], in_=src), writes=[stb])
                    cf ^= 1
                    evac_copy(dstw, st_[:], [stb], pwrites=[wB])
                brr = Rot([sb(ph, f"brr{i}", [128, 12, 512], BF16) for i in range(2)])
                glr = Rot([sb(ph, f"glr{i}", [128, 24, 512], BF16) for i in range(2)])
                xr = Rot([sb(ph, f"xrM{i}", [128, 8, 512], F32) for i in range(2)])
                yr = Rot([sb(ph, f"yrM{i}", [128, 8, 512], BF16) for i in range(2)])
                ya = Rot([sb(ph, f"yaM{i}", [128, 512], F32) for i in range(2)])
                yb_ = Rot([sb(ph, f"ybM{i}", [128, 512], F32) for i in range(2)])
                pi = [0]

                def nps():
                    pi[0] = (pi[0] + 1) % 6
                    return pi[0]
                for ti, (t0, w) in enumerate(TT):
                    if ti == 8 and not ctx_out:
                        continue
                    col = 1 if ti == 8 else 0
                    br_, brb = brr.next()
                    gl_, glb = glr.next()
                    xt, xtb = xr.next()
                    yt, ytb = yr.next()
                    P.op("sync", lambda e, br_=br_, t0=t0, w=w: e.dma_start(out=br_[:, :, 0:w], in_=BR.rearrange("(c k) t -> k c t", k=128)[:, :, t0:t0 + w]),
                         reads=[BRb], writes=[brb])
                    P.op("sync", lambda e, gl_=gl_, t0=t0, w=w: e.dma_start(out=gl_[:, :, 0:w], in_=FM[17 * 128:41 * 128, :].rearrange("(c k) t -> k c t", k=128)[:, :, t0:t0 + w]),
                         reads=[FMb], writes=[glb])
                    P.op("sync", lambda e, xt=xt, t0=t0, w=w: e.dma_start(out=xt[:, :, 0:w], in_=xT.rearrange("(kc k) t -> k kc t", k=128)[:, :, t0:t0 + w]),
                         reads=[xb[ti]], writes=[xtb])
                    for oc in range(8):
                        yacc, yab = ya.next()
                        for i in range(3):
                            p = nps()
                            for kc in range(4):
                                P.op("tensor", lambda e, p=p, i=i, kc=kc, oc=oc, br_=br_, w=w: e.matmul(
                                    psum[p][:, 0:w], lhsT=wbr[:, i * 4 + kc, oc * 128:(oc + 1) * 128], rhs=br_[:, i * 4 + kc, 0:w], start=(kc == 0), stop=(kc == 3)),
                                    reads=[wB, brb], writes=[psb[p]])
                            if i == 0:
                                P.op("vector", lambda e, p=p, yacc=yacc, gl_=gl_, oc=oc, w=w: e.tensor_tensor(
                                    out=yacc[:, 0:w], in0=psum[p][:, 0:w], in1=gl_[:, oc, 0:w], op=ALU.mult), reads=[psb[p], glb], writes=[yab])
                            else:
                                y2, y2b = yb_.next()
                                P.op("vector", lambda e, p=p, y2=y2, gl_=gl_, oc=oc, i=i, w=w: e.tensor_tensor(
                                    out=y2[:, 0:w], in0=psum[p][:, 0:w], in1=gl_[:, i * 8 + oc, 0:w], op=ALU.mult), reads=[psb[p], glb], writes=[y2b])
                                if i == 1:
                                    P.op("vector", lambda e, yacc=yacc, y2=y2, w=w: e.tensor_tensor(out=yacc[:, 0:w], in0=yacc[:, 0:w], in1=y2[:, 0:w], op=ALU.add),
                                         reads=[y2b], writes=[yab])
                                else:
                                    P.op("vector", lambda e, yacc=yacc, y2=y2, yt=yt, oc=oc, w=w: e.tensor_tensor(
                                        out=yt[:, oc, 0:w], in0=yacc[:, 0:w], in1=y2[:, 0:w], op=ALU.add),
                                        reads=[y2b, yab], writes=[ytb] if oc == 0 else (), pwrites=[ytb] if oc else ())
                    for oc in range(8):
                        p = nps()
                        for kc in range(8):
                            P.op("tensor", lambda e, p=p, kc=kc, oc=oc, yt=yt, w=w: e.matmul(
                                psum[p][:, 0:w], lhsT=wo[:, kc, oc * 128:(oc + 1) * 128], rhs=yt[:, kc, 0:w], start=(kc == 0), stop=(kc == 7)),
                                reads=[wB, ytb], writes=[psb[p]])
                        P.op("vector", lambda e, p=p, oc=oc, xt=xt, col=col, w=w: e.scalar_tensor_tensor(
                            out=xt[:, oc, 0:w], in0=psum[p][:, 0:w], scalar=mv(l, col, 2)[:, oc:oc + 1], in1=xt[:, oc, 0:w], op0=ALU.mult, op1=ALU.add),
                            reads=[psb[p], cB], writes=[xtb])
                    P.op("pool", lambda e, xt=xt, t0=t0, w=w: e.dma_start(out=xT.rearrange("(kc k) t -> k kc t", k=128)[:, :, t0:t0 + w], in_=xt[:, :, 0:w]),
                         reads=[xtb], writes=[xb[ti]])

            is_moe = (l % 2 == 1)
            l2 = l // 2
            if upto >= 6:
              with ExitStack() as ph:
                P.barrier()
                tiles = {
                    "x": Rot([sb(ph, f"n2x{i}", [128, 8, 512], F32) for i in range(2)]),
                    "sq": Rot([sb(ph, "n2sq", [128, 8, 512], BF16)]),
                    "rs": Rot([sb(ph, f"n2rs{i}", [128, 512], F32) for i in range(2)]),
                    "tmp": Rot([sb(ph, f"n2tmp{i}", [128, 512], F32) for i in range(6)]),
                    "h": Rot([sb(ph, f"n2h{i}", [128, 8, 512], BF16) for i in range(2)]),
                    "hf": Rot([sb(ph, f"n2hf{i}", [128, 8, 512], F32) for i in range(2 if is_moe else 0)]),
                }
                if is_moe:
                    wr = sb(ph, "wr", [128, 8, NE], F32)
                    wrB = Buf()
                    P.op("sync", lambda e: e.dma_start(out=wr[:], in_=moe_router[l2].rearrange("(kc k) n -> k kc n", k=128)), writes=[wrB])
                    lg_ = Rot([sb(ph, f"lg{i}", [128, 8], F32) for i in range(2)])
                    mx_ = Rot([sb(ph, f"mx{i}", [128, 8], F32) for i in range(2)])
                    gt_ = Rot([sb(ph, f"gt{i}", [128, 8], F32) for i in range(2)])
                    g2_ = Rot([sb(ph, f"g2{i}", [128, 8], F32) for i in range(2)])
                    gT_ = Rot([sb(ph, f"gT{i}", [8, 512], F32) for i in range(2)])
                for ti, (t0, w) in enumerate(TT):
                    if ti == 8 and not ctx_out:
                        continue
                    xt, xtb, ht, htb, hf, hfb = norm_tile(tiles, l, ti, 3, want_f32=is_moe)
                    P.op("pool", lambda e, ht=ht, t0=t0, w=w: e.dma_start(out=H2.rearrange("(kc k) t -> k kc t", k=128)[:, :, t0:t0 + w], in_=ht[:, :, 0:w]),
                         reads=[htb], pwrites=[H2b])
                    if is_moe:
                        gT, gTb = gT_.next()
                        for s in range(w // 128):
                            for kc in range(8):
                                P.op("tensor", lambda e, s=s, kc=kc: e.matmul(psum[0][:, 0:8], lhsT=hf[:, kc, s * 128:(s + 1) * 128], rhs=wr[:, kc, :],
                                                                             start=(kc == 0), stop=(kc == 7)), reads=[hfb, wrB], writes=[psb[0]])
                            lg, lgb = lg_.next()
                            mx, mxb = mx_.next()
                            gt, gtb = gt_.next()
                            g2, g2b = g2_.next()
                            P.op("vector", lambda e, lg=lg: e.tensor_copy(out=lg[:], in_=psum[0][:, 0:8]), reads=[psb[0]], writes=[lgb])
                            P.op("vector", lambda e, lg=lg, mx=mx: e.max(out=mx[:], in_=lg[:]), reads=[lgb], writes=[mxb])
                            P.op("vector", lambda e, mx=mx: e.tensor_tensor(out=mx[:, 2:3], in0=mx[:, 0:1], in1=mx[:, 1:2], op=ALU.subtract), writes=[mxb])
                            P.op("scalar", lambda e, mx=mx: e.activation(out=mx[:, 3:4], in_=mx[:, 2:3], func=AF.Sigmoid), reads=[mxb], writes=[mxb])
                            P.op("scalar", lambda e, mx=mx: e.activation(out=mx[:, 4:5], in_=mx[:, 2:3], func=AF.Sigmoid, scale=-1.0), writes=[mxb])
                            P.op("vector", lambda e, lg=lg, mx=mx, gt=gt: e.tensor_scalar(
                                out=gt[:], in0=lg[:], scalar1=mx[:, 0:1], scalar2=mx[:, 3:4], op0=ALU.is_equal, op1=ALU.mult), reads=[lgb, mxb], writes=[gtb])
                            P.op("vector", lambda e, lg=lg, mx=mx, g2=g2: e.tensor_scalar(
                                out=g2[:], in0=lg[:], scalar1=mx[:, 1:2], scalar2=mx[:, 4:5], op0=ALU.is_equal, op1=ALU.mult), reads=[lgb, mxb], writes=[g2b])
                            P.op("vector", lambda e, gt=gt, g2=g2: e.tensor_tensor(out=gt[:], in0=gt[:], in1=g2[:], op=ALU.add), reads=[g2b], writes=[gtb])
                            P.op("tensor", lambda e, gt=gt: e.transpose(out=psum[1][0:8, 0:128], in_=gt[:], identity=identf[:]), reads=[gtb, cB], writes=[psb[1]])
                            P.op("vector", lambda e, gT=gT, s=s: e.tensor_copy(out=gT[:, s * 128:(s + 1) * 128], in_=psum[1][0:8, 0:128]),
                                 reads=[psb[1]], writes=[gTb] if s == 0 else (), pwrites=[gTb] if s else ())
                        P.op("pool", lambda e, gT=gT, t0=t0, w=w: e.dma_start(out=GT[:, t0:t0 + w], in_=gT[:, 0:w]), reads=[gTb], pwrites=[GTb])

            if upto >= 7:
              with ExitStack() as ph:
                P.barrier()
                FH = DFF // 2
                w1r = Rot([sb(ph, f"w1h{i}", [128, 8, FH], BF16) for i in range(2)])
                w3r = Rot([sb(ph, f"w3h{i}", [128, 8, FH], BF16) for i in range(2)])
                w2r = Rot([sb(ph, f"w2h{i}", [128, 11, D], BF16) for i in range(2)])
                stg = Rot([sb(ph, f"stgF{i}", [128, FH], F32) for i in range(3)])
                h2r = Rot([sb(ph, f"h2r{i}", [128, 8, 512], BF16) for i in range(2)])
                ur = Rot([sb(ph, f"ur{i}", [128, 11, 512], BF16) for i in range(2)])
                sr = Rot([sb(ph, f"sr{i}", [128, 512], F32) for i in range(2)])
                cr = Rot([sb(ph, f"cr{i}", [128, 512], F32) for i in range(4)])
                gr = Rot([sb(ph, f"gr{i}", [128, 512], F32) for i in range(2)])
                pi = [0]

                def nps():
                    pi[0] = (pi[0] + 1) % 7
                    return [0, 1, 2, 3, 4, 5, 7][pi[0]]
                experts = list(range(NE)) if is_moe else [None]
                pendF = [None]
                cf = 0
                def wsteps(ex, half):
                    if ex is None:
                        W1, W3, W2 = ffn_w1[l2], ffn_w3[l2], ffn_w2[l2]
                    else:
                        W1, W3, W2 = moe_w1[l2, ex], moe_w3[l2, ex], moe_w2[l2, ex]
                    w1h, w1b = w1r.next()
                    w3h, w3b = w3r.next()
                    w2h, w2b = w2r.next()
                    steps = []
                    for (Wsrc, wdst, wbuf) in [(W1, w1h, w1b), (W3, w3h, w3b)]:
                        for kc in range(8):
                            def f(Wsrc=Wsrc, wdst=wdst, wbuf=wbuf, kc=kc):
                                st_, stb = stg.next()
                                P.op("sync", lambda e: e.dma_start(out=st_[:], in_=Wsrc[kc * 128:(kc + 1) * 128, half * FH:(half + 1) * FH]), writes=[stb])
                                evac_copy(wdst[:, kc, :], st_[:], [stb], writes=[wbuf] if kc == 0 else (), pwrites=[wbuf] if kc else ())
                            steps.append(f)
                    for j in range(11):
                        def f(j=j):
                            st_, stb = stg.next()
                            P.op("sync", lambda e: e.dma_start(out=st_[:, 0:D], in_=W2[half * FH + j * 128:half * FH + (j + 1) * 128, :]), writes=[stb])
                            evac_copy(w2h[:, j, :], st_[:, 0:D], [stb], writes=[w2b] if j == 0 else (), pwrites=[w2b] if j else ())
                        steps.append(f)
                    return (w1h, w1b, w3h, w3b, w2h, w2b), steps

                halves = [(ex, half) for ex in experts for half in range(2)]
                cur_w, cur_steps = wsteps(*halves[0])
                for f in cur_steps:
                    f()
                for hi, (ex, half) in enumerate(halves):
                    if True:
                        w1h, w1b, w3h, w3b, w2h, w2b = cur_w
                        if hi + 1 < len(halves):
                            nxt_w, nxt_steps = wsteps(*halves[hi + 1])
                        else:
                            nxt_w, nxt_steps = None, []
                        def stF1(ti):
                            t0, w = TT[ti]
                            h2t, h2tb = h2r.next()
                            P.op("sync", lambda e, h2t=h2t, t0=t0, w=w: e.dma_start(out=h2t[:, :, 0:w], in_=H2.rearrange("(kc k) t -> k kc t", k=128)[:, :, t0:t0 + w]),
                                 reads=[H2b], writes=[h2tb])
                            gtile = gtb_ = None
                            if ex is not None:
                                gtile, gtb_ = gr.next()
                                P.op("sync", lambda e, gtile=gtile, t0=t0, w=w: e.dma_start(out=gtile[:, 0:w], in_=GT[ex:ex + 1, t0:t0 + w].broadcast_to([128, w])),
                                     reads=[GTb], writes=[gtb_])
                            ut, utb = ur.next()
                            for j in range(11):
                                p1, p3 = nps(), nps()
                                for kc in range(8):
                                    P.op("tensor", lambda e, p1=p1, kc=kc, j=j: e.matmul(
                                        psum[p1][:, 0:w], lhsT=w1h[:, kc, j * 128:(j + 1) * 128], rhs=h2t[:, kc, 0:w], start=(kc == 0), stop=(kc == 7)),
                                        reads=[w1b, h2tb], writes=[psb[p1]])
                                for kc in range(8):
                                    P.op("tensor", lambda e, p3=p3, kc=kc, j=j: e.matmul(
                                        psum[p3][:, 0:w], lhsT=w3h[:, kc, j * 128:(j + 1) * 128], rhs=h2t[:, kc, 0:w], start=(kc == 0), stop=(kc == 7)),
                                        reads=[w3b, h2tb], writes=[psb[p3]])
                                s_, s_b = sr.next()
                                P.op("scalar", lambda e, s_=s_, p1=p1: e.activation(out=s_[:, 0:w], in_=psum[p1][:, 0:w], func=AF.Silu), reads=[psb[p1]], writes=[s_b])
                                P.op("vector", lambda e, s_=s_, p3=p3, j=j: e.tensor_tensor(out=ut[:, j, 0:w], in0=psum[p3][:, 0:w], in1=s_[:, 0:w], op=ALU.mult),
                                     reads=[psb[p3], s_b], writes=[utb] if j == 0 else (), pwrites=[utb] if j else ())
                            return (ti, ut, utb, gtile, gtb_, w2h, w2b, ex)

                        def stF2(st):
                            ti, ut, utb, gtile, gtb_, w2h_, w2b_, ex_ = st
                            t0, w = TT[ti]
                            col = 1 if ti == 8 else 0
                            for oc in range(8):
                                p = nps()
                                for j in range(11):
                                    P.op("tensor", lambda e, p=p, j=j, oc=oc: e.matmul(
                                        psum[p][:, 0:w], lhsT=w2h_[:, j, oc * 128:(oc + 1) * 128], rhs=ut[:, j, 0:w], start=(j == 0), stop=(j == 10)),
                                        reads=[w2b_, utb], writes=[psb[p]])
                                ct, ctb = cr.next()
                                if ex_ is None:
                                    P.op("scalar", lambda e, ct=ct, p=p, oc=oc: e.activation(
                                        out=ct[:, 0:w], in_=psum[p][:, 0:w], func=AF.Identity, scale=mv(l, col, 5)[:, oc:oc + 1]), reads=[psb[p], cB], writes=[ctb])
                                else:
                                    P.op("vector", lambda e, ct=ct, p=p, oc=oc: e.scalar_tensor_tensor(
                                        out=ct[:, 0:w], in0=psum[p][:, 0:w], scalar=mv(l, col, 5)[:, oc:oc + 1], in1=gtile[:, 0:w], op0=ALU.mult, op1=ALU.mult),
                                        reads=[psb[p], cB, gtb_], writes=[ctb])
                                P.op("pool", lambda e, ct=ct, oc=oc: e.dma_start(
                                    out=xT[oc * 128:(oc + 1) * 128, t0:t0 + w], in_=ct[:, 0:w], accum_op=ALU.add), reads=[ctb], pwrites=[xb[ti]])

                        for ti in range(len(TT)):
                            if ti == 8 and not ctx_out:
                                continue
                            st_new = stF1(ti)
                            if pendF[0] is not None:
                                stF2(pendF[0])
                            pendF[0] = st_new
                            for _ in range(4):
                                if nxt_steps:
                                    nxt_steps.pop(0)()
                        while nxt_steps:
                            nxt_steps.pop(0)()
                        cur_w = nxt_w
                if pendF[0] is not None:
                    stF2(pendF[0])
                    pendF[0] = None

        if upto >= 8:
          with ExitStack() as ph:
            P.barrier()
            xr = Rot([sb(ph, f"fx{i}", [128, 8, 512], F32) for i in range(2)])
            sqr = Rot([sb(ph, "fsq", [128, 8, 512], BF16)])
            rsr = Rot([sb(ph, f"frs{i}", [128, 512], F32) for i in range(2)])
            for ti in range(8):
                t0, w = TT[ti]
                xt, xtb = xr.next()
                sq, sqb = sqr.next()
                rs, rsb = rsr.next()
                P.op("sync", lambda e, xt=xt, t0=t0: e.dma_start(out=xt[:], in_=xT.rearrange("(kc k) t -> k kc t", k=128)[:, :, t0:t0 + 512]),
                     reads=[xb[ti]], writes=[xtb])
                P.op("scalar", lambda e, xt=xt, sq=sq: e.activation(out=sq[:], in_=xt[:], func=AF.Square), reads=[xtb], writes=[sqb])
                for kc in range(8):
                    P.op("tensor", lambda e, kc=kc, sq=sq: e.matmul(psum[7][:], lhsT=onesb[:], rhs=sq[:, kc, :], start=(kc == 0), stop=(kc == 7)),
                         reads=[sqb, cB], writes=[psb[7]])
                P.op("scalar", lambda e, rs=rs: e.activation(out=rs[:], in_=psum[7][:], func=AF.Ln, scale=1.0 / D, bias=epsc[:, 0:1]), reads=[psb[7], cB], writes=[rsb])
                P.op("scalar", lambda e, rs=rs: e.activation(out=rs[:], in_=rs[:], func=AF.Exp, scale=-0.5), writes=[rsb])
                for kc in range(8):
                    P.op("vector", lambda e, kc=kc, xt=xt, rs=rs: e.scalar_tensor_tensor(
                        out=xt[:, kc, :], in0=xt[:, kc, :], scalar=nfin_sb[:, kc:kc + 1], in1=rs[:], op0=ALU.mult, op1=ALU.mult),
                        reads=[rsb, cB], writes=[xtb])
                P.op("pool", lambda e, xt=xt, t0=t0: e.dma_start(out=outT.rearrange("(kc k) t -> k kc t", k=128)[:, :, t0:t0 + 512], in_=xt[:]),
                     reads=[xtb], pwrites=[outb])
        P.finish([outb, FMb, TMb, BRb, H2b, GTb] + xb)
        print(f"[build] ops={P.n_ops} waits={P.n_waits}")
    return nc


_CONSTS = None


def _prep(inputs, ncores=8):
    global _CONSTS
    if _CONSTS is None:
        _CONSTS = _consts()
    f32 = lambda a: np.ascontiguousarray(np.asarray(a, dtype=np.float32))
    x, c, ctx, c_ctx = f32(inputs["x"]), f32(inputs["c"]), f32(inputs["ctx"]), f32(inputs["c_ctx"])

    def pk(v):
        v = f32(v)
        lead = v.shape[:-1]
        return np.ascontiguousarray(np.moveaxis(v.reshape(lead + (8, 128)), -1, 0))
    shared = {
        "w_mod": f32(inputs["w_mod"]),
        "b_mod": np.ascontiguousarray(np.moveaxis(f32(inputs["b_mod"]).reshape(DEPTH, 48, 128), -1, 0)),
        "nmix": pk(inputs["norm_mix"]), "nffn": pk(inputs["norm_ffn"]), "nfin": pk(inputs["norm_final"]),
        "w_in": f32(inputs["w_in"]),
        "sink": np.ascontiguousarray(np.broadcast_to(f32(inputs["attn_sink"]).reshape(1, -1), (128, DEPTH * 8))),
        "decs": np.ascontiguousarray(np.broadcast_to(
            np.stack([f32(inputs["ret_decay_fwd"]), f32(inputs["ret_decay_bwd"])], 1).reshape(1, -1), (128, DEPTH * 8))),
        "dlam": np.ascontiguousarray(np.broadcast_to(f32(inputs["diff_lambda"])[None], (128, DEPTH, 4, 64))),
        "dgain": np.ascontiguousarray(np.broadcast_to(f32(inputs["diff_norm"])[None], (128, DEPTH, 128))),
        "w_branch": f32(inputs["w_branch"]), "w_out": f32(inputs["w_out"]),
        "ffn_w1": f32(inputs["ffn_w1"]), "ffn_w3": f32(inputs["ffn_w3"]), "ffn_w2": f32(inputs["ffn_w2"]),
        "moe_router": f32(inputs["moe_router"]),
        "moe_w1": f32(inputs["moe_w1"]), "moe_w3": f32(inputs["moe_w3"]), "moe_w2": f32(inputs["moe_w2"]),
    }
    shared.update(_CONSTS)
    maps = []
    for b in range(ncores):
        m = dict(shared)
        m["xT_in"] = np.ascontiguousarray(np.concatenate([x[b], ctx[b]], 0).T)
        c2 = np.stack([c[b], c_ctx], -1)
        m["c2"] = np.ascontiguousarray(c2.reshape(8, 128, 2).transpose(1, 0, 2))
        maps.append(m)
    return maps


_NC = None


def kernel(**inputs):
    global _NC
    if _NC is None:
        _NC = build()
    maps = _prep(inputs)
    res = run_bass_kernel_spmd(_NC, maps, core_ids=list(range(8)))
    out = np.stack([np.ascontiguousarray(res.results[b]["outT"].T) for b in range(8)], 0)
    return out.astype(np.float32)
```

```python
import math
import os
from contextlib import ExitStack
import numpy as np
import ml_dtypes
import concourse.bass as bass
import concourse.mybir as mybir
from concourse.bass_utils import run_bass_kernel_spmd

F32 = mybir.dt.float32
BF16 = mybir.dt.bfloat16
AF = mybir.ActivationFunctionType
ALU = mybir.AluOpType

T = 4352
NLAT = 4096
NCTX = 256
D = 1024
DEPTH = 4
DIN = 6912
DFF = 2816
NE = 8
EPS = 1e-6
TT = [(i * 512, 512) for i in range(8)] + [(4096, 256)]
NFM = 41
NTM = 1664


class Buf:
    __slots__ = ("w", "r", "rp")

    def __init__(self):
        self.w = {}
        self.r = {}
        self.rp = {}


class EngState:
    def __init__(self, name, handle, sem, inc, stream):
        self.name, self.h, self.sem, self.inc, self.stream = name, handle, sem, inc, stream
        self.count = 0


class Prog:
    def __init__(self, nc, stack, nq=4):
        self.nc = nc
        self.engs = {}
        for name in ["tensor", "vector", "scalar", "gpsimd"]:
            sem = stack.enter_context(nc.semaphore("s_" + name))
            self.engs[name] = EngState(name, getattr(nc, name), sem, 1, name)
        self.dmaq = {}
        for qname, issuer, stream in [("sync", nc.sync, "sync"), ("pool", nc.gpsimd, "gpsimd"), ("actq", nc.scalar, "scalar")]:
            lst = []
            for i in range(nq):
                sem = stack.enter_context(nc.semaphore(f"d_{qname}{i}"))
                st = EngState(f"{qname}{i}", issuer, sem, 16, stream)
                lst.append(st)
                self.engs[st.name] = st
            self.dmaq[qname] = [lst, 0]
        self.known = {s: {} for s in ["tensor", "vector", "scalar", "gpsimd", "sync"]}
        self.n_ops = 0
        self.n_waits = 0

    def op(self, eng, fn, reads=(), writes=(), pwrites=()):
        if eng in self.dmaq:
            lst, k = self.dmaq[eng]
            st = lst[k % len(lst)]
            self.dmaq[eng][1] = k + 1
            is_dma = True
        else:
            st = self.engs[eng]
            is_dma = False
        deps = {}

        def add(d):
            for e, i in d.items():
                if deps.get(e, -1) < i:
                    deps[e] = i
        for b in reads:
            add(b.w)
        for b in writes:
            add(b.w)
            add(b.r)
            add(b.rp)
        for b in pwrites:
            if b.r:
                b.rp = dict(b.r)
                b.r = {}
                b.w = {}
            add(b.rp)
        if is_dma and st.count > 0:
            deps[st.name] = max(deps.get(st.name, -1), st.count - 1)
        known = self.known[st.stream]
        for e, i in deps.items():
            if (not is_dma) and e == st.name and e == "tensor":
                continue
            if known.get(e, -1) >= i:
                continue
            es = self.engs[e]
            st.h.wait_ge(es.sem, (i + 1) * es.inc)
            known[e] = i
            self.n_waits += 1
        ins = fn(st.h)
        ins.then_inc(st.sem, st.inc)
        idx = st.count
        st.count += 1
        self.n_ops += 1
        for b in reads:
            if b.r.get(st.name, -1) < idx:
                b.r[st.name] = idx
        for b in writes:
            b.w = {st.name: idx}
            b.r = {}
            b.rp = {}
        for b in pwrites:
            b.w[st.name] = idx
        return idx

    def barrier(self):
        hs = {"tensor": self.nc.tensor, "vector": self.nc.vector, "scalar": self.nc.scalar, "gpsimd": self.nc.gpsimd, "sync": self.nc.sync}
        for sname, h in hs.items():
            known = self.known[sname]
            for e, es in self.engs.items():
                if es.count == 0 or known.get(e, -1) >= es.count - 1:
                    continue
                if e == sname and e == "tensor":
                    continue
                h.wait_ge(es.sem, es.count * es.inc)
                known[e] = es.count - 1
                self.n_waits += 1

    def finish(self, bufs):
        for b in bufs:
            for e, i in b.w.items():
                es = self.engs[e]
                self.nc.sync.wait_ge(es.sem, (i + 1) * es.inc)


class Rot:
    def __init__(self, tiles):
        self.tiles = tiles
        self.bufs = [Buf() for _ in tiles]
        self.i = 0

    def next(self):
        k = self.i % len(self.tiles)
        self.i += 1
        return self.tiles[k], self.bufs[k]


def _consts():
    c = {}
    rows = NLAT // 64
    row = np.repeat(np.arange(rows, dtype=np.float32), 64)
    col = np.tile(np.arange(64, dtype=np.float32), rows)
    inv = (1.0 / (np.float32(10000.0) ** (np.arange(16, dtype=np.float32) / np.float32(16)))).astype(np.float32)
    ang = np.stack([row[:, None] * inv, col[:, None] * inv], axis=1).astype(np.float32)
    cos, sin = np.cos(ang).astype(np.float32), np.sin(ang).astype(np.float32)
    C = np.zeros((64, NLAT), np.float32)
    S = np.zeros((64, NLAT), np.float32)
    for d in range(64):
        ax, f = d // 32, d % 16
        C[d] = cos[:, ax, f]
        S[d] = sin[:, ax, f]
    c["ropeC"] = np.concatenate([C, C], 0)
    c["ropeS"] = np.concatenate([S, S], 0)
    R = np.zeros((64, 64), np.float32)
    for m in range(64):
        if (m % 32) < 16:
            R[m, m + 16] = -1.0
        else:
            R[m, m - 16] = 1.0
    R2 = np.zeros((128, 128), np.float32)
    R2[:64, :64] = R
    R2[64:, 64:] = R
    c["rmatT"] = R2.T.copy().astype(ml_dtypes.bfloat16)
    c["identb"] = np.eye(128, dtype=np.float32).astype(ml_dtypes.bfloat16)
    c["identf"] = np.eye(128, dtype=np.float32)
    c["onesb"] = np.ones((128, 128), np.float32).astype(ml_dtypes.bfloat16)
    k = np.arange(128)[:, None].astype(np.float32)
    q = np.arange(128)[None, :].astype(np.float32)
    ret = np.zeros((128, 8, 128), np.float32)
    ret[:, 0] = np.maximum(q - k, 0)
    ret[:, 1] = (q >= k)
    ret[:, 2] = np.maximum(k - q, 0)
    ret[:, 3] = (k >= q)
    ret[:, 4] = q + 1.0
    ret[:, 5] = 128.0 - q
    ret[:, 6, 0] = 127.0 - k[:, 0]
    ret[:, 6, 1] = k[:, 0]
    ret[:, 6, 2] = 128.0
    c["rettab"] = ret
    mprev = (k >= q).astype(np.float32)
    mnext = (k <= q).astype(np.float32)
    c["wmask"] = np.stack([np.tile(mprev, (1, 4)), np.tile(mnext, (1, 4))], 1).astype(ml_dtypes.bfloat16)
    return c


def build(dbg=False, upto=99, layers=DEPTH, sub=99):
    nc = bass.Bass("TRN2", target_bir_lowering=False)

    def din(name, shape, dt=F32):
        return nc.dram_tensor(name, list(shape), dt, kind="ExternalInput").ap()

    def dscr(name, shape, dt):
        return nc.dram_tensor(name, list(shape), dt, kind=("ExternalOutput" if dbg else "Internal")).ap()

    xT_in = din("xT_in", [D, T])
    c2 = din("c2", [128, 8, 2])
    w_mod = din("w_mod", [DEPTH, D, 6 * D])
    b_mod = din("b_mod", [128, DEPTH, 48])
    nmix = din("nmix", [128, DEPTH, 8])
    nffn = din("nffn", [128, DEPTH, 8])
    nfin = din("nfin", [128, 8])
    w_in = din("w_in", [DEPTH, D, DIN])
    sink = din("sink", [128, DEPTH * 8])
    decs = din("decs", [128, DEPTH * 8])
    dlam = din("dlam", [128, DEPTH, 4, 64])
    dgain = din("dgain", [128, DEPTH, 128])
    w_branch = din("w_branch", [DEPTH, 3, 512, D])
    w_out = din("w_out", [DEPTH, D, D])
    ffn_w1 = din("ffn_w1", [2, D, DFF])
    ffn_w3 = din("ffn_w3", [2, D, DFF])
    ffn_w2 = din("ffn_w2", [2, DFF, D])
    if layers >= 2:
        moe_router = din("moe_router", [2, D, NE])
        moe_w1 = din("moe_w1", [2, NE, D, DFF])
        moe_w3 = din("moe_w3", [2, NE, D, DFF])
        moe_w2 = din("moe_w2", [2, NE, DFF, D])
    ropeC_d = din("ropeC", [128, NLAT])
    ropeS_d = din("ropeS", [128, NLAT])
    rmatT_d = din("rmatT", [128, 128], BF16)
    identb_d = din("identb", [128, 128], BF16)
    identf_d = din("identf", [128, 128])
    onesb_d = din("onesb", [128, 128], BF16)
    rettab_d = din("rettab", [128, 8, 128])
    wmask_d = din("wmask", [128, 2, 512], BF16)

    outT = nc.dram_tensor("outT", [D, NLAT], F32, kind="ExternalOutput").ap()
    xT = dscr("xT", [D, T], F32)
    FM = dscr("FM", [NFM * 128, T], BF16)
    TM = dscr("TM", [T, NTM], BF16)
    BR = dscr("BR", [1536, T], BF16)
    H2 = dscr("H2", [D, T], BF16)
    GT = dscr("GT", [NE, T], F32)

    with ExitStack() as top:
        P = Prog(nc, top)
        ps01 = top.enter_context(nc.psum_tensor("ps01", [128, 1024], F32))
        ps23 = top.enter_context(nc.psum_tensor("ps23", [128, 1024], F32))
        psum = [ps01[:, 0:512], ps01[:, 512:1024], ps23[:, 0:512], ps23[:, 512:1024]]
        for i in range(4, 8):
            psum.append(top.enter_context(nc.psum_tensor(f"ps{i}", [128, 1024], BF16) if i == 6 else nc.psum_tensor(f"ps{i}", [128, 512], F32)))
        psb = [Buf() for _ in range(8)]

        uid = [0]

        def sb(stack, name, shape, dt):
            uid[0] += 1
            return stack.enter_context(nc.sbuf_tensor(f"{name}_s{uid[0]}", list(shape), dt))

        identb = sb(top, "identb", [128, 128], BF16)
        identf = sb(top, "identf", [128, 128], F32)
        onesb = sb(top, "onesb", [128, 128], BF16)
        rmatT = sb(top, "rmatT", [128, 128], BF16)
        modv = sb(top, "modv", [128, DEPTH, 2, 6, 8], F32)
        lgt = sb(top, "lgt", [128, DEPTH * 8], F32)
        esink = sb(top, "esink", [128, DEPTH * 8], F32)
        neglam = sb(top, "neglam", [128, DEPTH], F32)
        gainb = sb(top, "gainb", [128, DEPTH, 128], F32)
        nfin_sb = sb(top, "nfin_sb", [128, 8], F32)
        epsc = sb(top, "epsc", [128, 1], F32)
        cB = Buf()
        xb = [Buf() for _ in TT]
        FMb, TMb, BRb, H2b, GTb, outb = Buf(), Buf(), Buf(), Buf(), Buf(), Buf()

        for dst, src in [(identb, identb_d), (identf, identf_d), (onesb, onesb_d), (rmatT, rmatT_d), (nfin_sb, nfin)]:
            P.op("sync", lambda e, dst=dst, src=src: e.dma_start(out=dst[:], in_=src), pwrites=[cB])
        P.op("vector", lambda e: e.memset(epsc[:], EPS), pwrites=[cB])

        with ExitStack() as ph:
            P.barrier()
            xcp = Rot([sb(ph, f"xcp{i}", [128, 8, 512], F32) for i in range(2)])
            for ti, (t0, w) in enumerate(TT):
                tl, tb = xcp.next()
                P.op("sync", lambda e, tl=tl, t0=t0, w=w: e.dma_start(
                    out=tl[:, :, 0:w], in_=xT_in.rearrange("(kc k) t -> k kc t", k=128)[:, :, t0:t0 + w]), writes=[tb])
                P.op("pool", lambda e, tl=tl, t0=t0, w=w: e.dma_start(
                    out=xT.rearrange("(kc k) t -> k kc t", k=128)[:, :, t0:t0 + w], in_=tl[:, :, 0:w]), reads=[tb], writes=[xb[ti]])

            c_sb = sb(ph, "c_sb", [128, 8, 2], F32)
            cs_sb = sb(ph, "cs_sb", [128, 8, 2], F32)
            bmod_sb = sb(ph, "bmod_sb", [128, DEPTH, 48], F32)
            nmix_sb = sb(ph, "nmix_sb", [128, DEPTH, 8], F32)
            nffn_sb = sb(ph, "nffn_sb", [128, DEPTH, 8], F32)
            modT = sb(ph, "modT", [128, 48, 2], F32)
            sB, mB = Buf(), Buf()
            mtB = Buf()
            for dst, src in [(c_sb, c2), (bmod_sb, b_mod), (nmix_sb, nmix), (nffn_sb, nffn)]:
                P.op("sync", lambda e, dst=dst, src=src: e.dma_start(out=dst[:], in_=src), pwrites=[sB])
            P.op("scalar", lambda e: e.activation(out=cs_sb[:], in_=c_sb[:], func=AF.Silu), reads=[sB], writes=[mB])
            wm = Rot([sb(ph, f"wm{i}", [128, 8, 512], F32) for i in range(2)])
            for l in range(layers):
                for n in range(12):
                    wt, wb = wm.next()
                    P.op("sync", lambda e, wt=wt, l=l, n=n: e.dma_start(
                        out=wt[:], in_=w_mod[l].rearrange("(kc k) n -> k kc n", k=128)[:, :, n * 512:(n + 1) * 512]), writes=[wb])
                    for jj in range(4):
                        j = n * 4 + jj
                        for kc in range(8):
                            P.op("tensor", lambda e, wt=wt, jj=jj, kc=kc, j=j: e.matmul(
                                psum[0][:, 2 * j:2 * j + 2], lhsT=wt[:, kc, jj * 128:(jj + 1) * 128], rhs=cs_sb[:, kc, :],
                                start=(kc == 0), stop=(kc == 7)), reads=[wb, mB], writes=[psb[0]])
                for col in range(2):
                    P.op("vector", lambda e, col=col, l=l: e.tensor_tensor(
                        out=modT[:, :, col], in0=psum[0][:, 0:96].rearrange("p (j c) -> p j c", c=2)[:, :, col],
                        in1=bmod_sb[:, l, :], op=ALU.add), reads=[psb[0], sB], pwrites=[mtB])
                for col in range(2):
                    for kind, (src0, nrm) in enumerate([(8, nmix_sb), (0, None), (16, None), (32, nffn_sb), (24, None), (40, None)]):
                        if nrm is not None:
                            P.op("vector", lambda e, col=col, l=l, kind=kind, src0=src0, nrm=nrm: e.scalar_tensor_tensor(
                                out=modv[:, l, col, kind, :], in0=modT[:, src0:src0 + 8, col], scalar=1.0, in1=nrm[:, l, :],
                                op0=ALU.add, op1=ALU.mult), reads=[mtB, sB], pwrites=[cB])
                        else:
                            P.op("vector", lambda e, col=col, l=l, kind=kind, src0=src0: e.tensor_copy(
                                out=modv[:, l, col, kind, :], in_=modT[:, src0:src0 + 8, col]), reads=[mtB], pwrites=[cB])
            dtmp = sb(ph, "dtmp", [128, DEPTH * 8], F32)
            dtmp2 = sb(ph, "dtmp2", [128, DEPTH * 8], F32)
            dB, dB2 = Buf(), Buf()
            P.op("sync", lambda e: e.dma_start(out=dtmp[:], in_=decs), writes=[dB])
            P.op("scalar", lambda e: e.activation(out=dtmp2[:], in_=dtmp[:], func=AF.Exp, scale=-1.0), reads=[dB], writes=[dB2])
            P.op("scalar", lambda e: e.activation(out=dtmp2[:], in_=dtmp2[:], func=AF.Ln, bias=1.0), reads=[dB2], writes=[dB2])
            P.op("vector", lambda e: e.tensor_scalar(out=lgt[:], in0=dtmp2[:], scalar1=-1.0, scalar2=None, op0=ALU.mult),
                 reads=[dB2], pwrites=[cB])
            stmp = sb(ph, "stmp", [128, DEPTH * 8], F32)
            sB2 = Buf()
            P.op("sync", lambda e: e.dma_start(out=stmp[:], in_=sink), writes=[sB2])
            P.op("scalar", lambda e: e.activation(out=esink[:], in_=stmp[:], func=AF.Exp), reads=[sB2], pwrites=[cB])
            lam_sb = sb(ph, "lam_sb", [128, DEPTH, 4, 64], F32)
            lprod = sb(ph, "lprod", [128, DEPTH, 2, 64], F32)
            lsum = sb(ph, "lsum", [128, DEPTH, 2], F32)
            lB, lB2, lB3 = Buf(), Buf(), Buf()
            P.op("sync", lambda e: e.dma_start(out=lam_sb[:], in_=dlam), writes=[lB])
            P.op("vector", lambda e: e.tensor_tensor(
                out=lprod[:], in0=lam_sb[:].rearrange("p l (a b) d -> p l a b d", b=2)[:, :, :, 0, :],
                in1=lam_sb[:].rearrange("p l (a b) d -> p l a b d", b=2)[:, :, :, 1, :], op=ALU.mult), reads=[lB], writes=[lB2])
            P.op("vector", lambda e: e.tensor_reduce(out=lsum[:], in_=lprod[:], axis=mybir.AxisListType.X, op=ALU.add),
                 reads=[lB2], writes=[lB3])
            P.op("scalar", lambda e: e.activation(out=lsum[:], in_=lsum[:], func=AF.Exp), reads=[lB3], writes=[lB3])
            gn_sb = sb(ph, "gn_sb", [128, DEPTH, 128], F32)
            gB = Buf()
            P.op("sync", lambda e: e.dma_start(out=gn_sb[:], in_=dgain), writes=[gB])
            for l in range(DEPTH):
                lam_init = 0.8 - 0.6 * math.exp(-0.3 * l)
                P.op("vector", lambda e, l=l, lam_init=lam_init: e.scalar_tensor_tensor(
                    out=neglam[:, l:l + 1], in0=lsum[:, l, 1:2], scalar=-lam_init, in1=lsum[:, l, 0:1],
                    op0=ALU.add, op1=ALU.subtract), reads=[lB3], pwrites=[cB])
                P.op("vector", lambda e, l=l, lam_init=lam_init: e.tensor_scalar(
                    out=gainb[:, l, :], in0=gn_sb[:, l, :], scalar1=(1.0 - lam_init), scalar2=None, op0=ALU.mult),
                    reads=[gB], pwrites=[cB])

        def mv(l, col, kind):
            return modv[:, l, col, kind, :]

        def norm_A(ph_tiles, l, ti):
            t0, w = TT[ti]
            xt, xtb = ph_tiles["x"].next()
            sq, sqb = ph_tiles["sq"].next()
            rs, rsb = ph_tiles["rs"].next()
            P.op("sync", lambda e: e.dma_start(out=xt[:, :, 0:w], in_=xT.rearrange("(kc k) t -> k kc t", k=128)[:, :, t0:t0 + w]),
                 reads=[xb[ti]], writes=[xtb])
            P.op("scalar", lambda e: e.activation(out=sq[:, :, 0:w], in_=xt[:, :, 0:w], func=AF.Square), reads=[xtb], writes=[sqb])
            for kc in range(8):
                P.op("tensor", lambda e, kc=kc: e.matmul(psum[7][:, 0:w], lhsT=onesb[:], rhs=sq[:, kc, 0:w], start=(kc == 0), stop=(kc == 7)),
                     reads=[sqb, cB], writes=[psb[7]])
            P.op("scalar", lambda e: e.activation(out=rs[:, 0:w], in_=psum[7][:, 0:w], func=AF.Ln, scale=1.0 / D, bias=epsc[:, 0:1]),
                 reads=[psb[7], cB], writes=[rsb])
            P.op("scalar", lambda e: e.activation(out=rs[:, 0:w], in_=rs[:, 0:w], func=AF.Exp, scale=-0.5), reads=[rsb], writes=[rsb])
            return (ti, xt, xtb, rs, rsb)

        def norm_B(ph_tiles, l, st, kind0, want_f32=False):
            ti, xt, xtb, rs, rsb = st
            t0, w = TT[ti]
            col = 1 if ti == 8 else 0
            ht, htb = ph_tiles["h"].next(ti) if ph_tiles.get("h_by_tile") else ph_tiles["h"].next()
            hf = hfb = None
            if want_f32:
                hf, hfb = ph_tiles["hf"].next()
            for kc in range(8):
                tmp, tmpb = ph_tiles["tmp"].next()
                P.op("vector", lambda e, kc=kc, tmp=tmp: e.scalar_tensor_tensor(
                    out=tmp[:, 0:w], in0=xt[:, kc, 0:w], scalar=mv(l, col, kind0)[:, kc:kc + 1], in1=rs[:, 0:w],
                    op0=ALU.mult, op1=ALU.mult), reads=[xtb, rsb, cB], writes=[tmpb])
                if want_f32:
                    P.op("scalar", lambda e, kc=kc, tmp=tmp: e.activation(
                        out=hf[:, kc, 0:w], in_=tmp[:, 0:w], func=AF.Identity, bias=mv(l, col, kind0 + 1)[:, kc:kc + 1]),
                        reads=[tmpb, cB], pwrites=[hfb])
                    P.op("vector", lambda e, kc=kc: e.tensor_copy(out=ht[:, kc, 0:w], in_=hf[:, kc, 0:w]), reads=[hfb], pwrites=[htb])
                else:
                    P.op("scalar", lambda e, kc=kc, tmp=tmp: e.activation(
                        out=ht[:, kc, 0:w], in_=tmp[:, 0:w], func=AF.Identity, bias=mv(l, col, kind0 + 1)[:, kc:kc + 1]),
                        reads=[tmpb, cB], pwrites=[htb])
            return xt, xtb, ht, htb, hf, hfb

        evac_flip = [0]

        def evac_copy(out_ap, in_ap, reads, writes=(), pwrites=()):
            evac_flip[0] ^= 1
            if evac_flip[0] or os.environ.get("CASTV"):
                P.op("vector", lambda e: e.tensor_copy(out=out_ap, in_=in_ap), reads=reads, writes=writes, pwrites=pwrites)
            else:
                P.op("scalar", lambda e: e.copy(out=out_ap, in_=in_ap), reads=reads, writes=writes, pwrites=pwrites)

        for l in range(layers):
            ctx_out = l < DEPTH - 1
            lam_init = 0.8 - 0.6 * math.exp(-0.3 * l)
            ntt = 9 if True else 8

            if upto >= 1:
              with ExitStack() as ph:
                P.barrier()
                t1r = Rot([sb(ph, f"t1r{i}", [128, 512], F32) for i in range(2)])
                t2r = Rot([sb(ph, f"t2r{i}", [128, 512], F32) for i in range(2)])
                hall = sb(ph, "hall", [128, 8, T], BF16)
                hallb = Buf()
                ropeC = sb(ph, "ropeC", [128, NLAT], F32)
                ropeS = sb(ph, "ropeS", [128, NLAT], F32)
                rB = Buf()
                P.op("sync", lambda e: e.dma_start(out=ropeC[:], in_=ropeC_d), pwrites=[rB])
                P.op("sync", lambda e: e.dma_start(out=ropeS[:], in_=ropeS_d), pwrites=[rB])
                with ExitStack() as ph2:
                    P.barrier()
                    tiles = {
                        "x": Rot([sb(ph2, f"p1x{i}", [128, 8, 512], F32) for i in range(2)]),
                        "sq": Rot([sb(ph2, f"p1sq{i}", [128, 8, 512], BF16) for i in range(2)]),
                        "rs": Rot([sb(ph2, f"p1rs{i}", [128, 512], F32) for i in range(3)]),
                        "tmp": Rot([sb(ph2, f"p1tmp{i}", [128, 512], F32) for i in range(8)]),
                    }
                    class _H:
                        @staticmethod
                        def next(ti):
                            t0 = TT[ti][0]
                            return hall[:, :, t0:t0 + 512 if t0 < 4096 else T], hallb
                    tiles["h"] = _H
                    tiles["h_by_tile"] = True
                    pend = None
                    for ti in range(len(TT)):
                        stA = norm_A(tiles, l, ti)
                        if pend is not None:
                            norm_B(tiles, l, pend, 0)
                        pend = stA
                    norm_B(tiles, l, pend, 0)
                P.barrier()
                wst = Rot([sb(ph, f"wst{i}", [128, 8, 512], F32) for i in range(2)])
                wbf = Rot([sb(ph, f"wbf{i}", [128, 8, 512], BF16) for i in range(2)])
                ev = Rot([sb(ph, f"ev{i}", [128, 512], BF16) for i in range(4)])
                xbt = Rot([sb(ph, f"xbt{i}", [128, 512], BF16) for i in range(2)])
                segs = [("fm", 0, 512, 0, "rope"), ("fm", 512, 128, 4, "rope"), ("tm", 640, 128, 0, "copy"),
                        ("fm", 768, 256, 5, "rope"), ("fm", 1024, 256, 7, "rope"), ("tm", 1280, 512, 128, "copy"),
                        ("tm", 1792, 512, 640, "silu"), ("fm", 2304, 512, 9, "rope"), ("fm", 2816, 512, 13, "rope"),
                        ("tm", 3328, 512, 1152, "copy")] + [("fm", 3840 + 512 * i, 512, 17 + 4 * i, "sigmoid") for i in range(6)]
                pi = [0]

                def nps():
                    pi[0] = (pi[0] + 1) % 6
                    return pi[0]
                castflip = 0
                for si, (kind, c0, ncol, d0, mode) in enumerate(segs):
                    if si >= sub:
                        break
                    ws, wsb = wst.next()
                    wb_, wbb = wbf.next()
                    P.op("sync", lambda e, ws=ws, c0=c0, ncol=ncol: e.dma_start(
                        out=ws[:, :, 0:ncol], in_=w_in[l].rearrange("(kc k) n -> k kc n", k=128)[:, :, c0:c0 + ncol]), writes=[wsb])
                    for kc in range(8):
                        castflip ^= 1
                        evac_copy(wb_[:, kc, 0:ncol], ws[:, kc, 0:ncol], [wsb], pwrites=[wbb])
                    if kind == "fm":
                        for cj in range(ncol // 128):
                            for ti, (t0, w) in enumerate(TT):
                                p = nps()
                                for kc in range(8):
                                    P.op("tensor", lambda e, p=p, wb_=wb_, kc=kc, cj=cj, t0=t0, w=w: e.matmul(
                                        psum[p][:, 0:w], lhsT=wb_[:, kc, cj * 128:(cj + 1) * 128], rhs=hall[:, kc, t0:t0 + w],
                                        start=(kc == 0), stop=(kc == 7)), reads=[wbb, hallb], writes=[psb[p]])
                                et, etb = ev.next()
                                dst = FM[(d0 + cj) * 128:(d0 + cj + 1) * 128, t0:t0 + w]
                                if mode == "sigmoid":
                                    P.op("scalar", lambda e, p=p, et=et, w=w: e.activation(out=et[:, 0:w], in_=psum[p][:, 0:w], func=AF.Sigmoid),
                                         reads=[psb[p]], writes=[etb])
                                elif mode == "rope" and ti < 8:
                                    xq, xqb = xbt.next()
                                    t1, t1b = t1r.next()
                                    t2, t2b = t2r.next()
                                    p2 = nps()
                                    RS = int(os.environ.get("ROPE_STAGE", "5"))
                                    P.op("scalar", lambda e, p=p, xq=xq: e.copy(out=xq[:], in_=psum[p][:]), reads=[psb[p]], writes=[xqb])
                                    if RS >= 2:
                                        P.op("vector", lambda e, p=p, t1=t1, t0=t0: e.tensor_tensor(
                                            out=t1[:], in0=psum[p][:], in1=ropeC[:, t0:t0 + 512], op=ALU.mult), reads=[psb[p], rB, xqb], writes=[t1b])
                                    if RS >= 3:
                                        P.op("tensor", lambda e, p2=p2, xq=xq: e.matmul(psum[p2][:], lhsT=rmatT[:], rhs=xq[:], start=True, stop=True),
                                             reads=[xqb, cB], writes=[psb[p2]])
                                    if RS >= 4:
                                        P.op("vector", lambda e, p2=p2, t2=t2, t0=t0: e.tensor_tensor(
                                            out=t2[:], in0=psum[p2][:], in1=ropeS[:, t0:t0 + 512], op=ALU.mult), reads=[psb[p2], rB], writes=[t2b])
                                    if RS >= 5:
                                        P.op("vector", lambda e, et=et, t1=t1, t2=t2: e.tensor_tensor(out=et[:], in0=t1[:], in1=t2[:], op=ALU.add),
                                             reads=[t1b, t2b], writes=[etb])
                                    else:
                                        P.op("vector", lambda e, et=et, xq=xq: e.tensor_copy(out=et[:], in_=xq[:]), reads=[xqb], writes=[etb])
                                else:
                                    evac_copy(et[:, 0:w], psum[p][:, 0:w], [psb[p]], writes=[etb])
                                P.op("pool", lambda e, et=et, dst=dst, w=w: e.dma_start(out=dst, in_=et[:, 0:w]), reads=[etb], pwrites=[FMb])
                    else:
                        for s in range(T // 128):
                            p = nps()
                            for kc in range(8):
                                P.op("tensor", lambda e, p=p, wb_=wb_, kc=kc, s=s, ncol=ncol: e.matmul(
                                    psum[p][:, 0:ncol], lhsT=hall[:, kc, s * 128:(s + 1) * 128], rhs=wb_[:, kc, 0:ncol],
                                    start=(kc == 0), stop=(kc == 7)), reads=[wbb, hallb], writes=[psb[p]])
                            et, etb = ev.next()
                            if mode == "silu":
                                P.op("scalar", lambda e, p=p, et=et, ncol=ncol: e.activation(out=et[:, 0:ncol], in_=psum[p][:, 0:ncol], func=AF.Silu),
                                     reads=[psb[p]], writes=[etb])
                            else:
                                evac_copy(et[:, 0:ncol], psum[p][:, 0:ncol], [psb[p]], writes=[etb])
                            P.op("pool", lambda e, et=et, s=s, d0=d0, ncol=ncol: e.dma_start(
                                out=TM[s * 128:(s + 1) * 128, d0:d0 + ncol], in_=et[:, 0:ncol]), reads=[etb], pwrites=[TMb])

            qblocks = list(range(32)) + ([32, 33] if ctx_out else [])

            def transpose_store(ph_t, src_tile, src_buf, nchunks, row0, s, acc):
                for cc in range(nchunks):
                    P.op("tensor", lambda e, cc=cc: e.transpose(
                        out=ph_t["pst"][:, cc, :], in_=src_tile[:, cc * 128:(cc + 1) * 128], identity=identb[:]),
                        reads=[src_buf, cB], writes=[ph_t["pstb"]] if cc == 0 else (), pwrites=[ph_t["pstb"]] if cc > 0 else ())
                if acc["cnt"] == 0:
                    acc["tile"], acc["buf"] = ph_t["brt"].next()
                    acc["s0"] = s
                k = acc["cnt"]
                tile_, buf_ = acc["tile"], acc["buf"]
                evac_copy(tile_[:, 0:nchunks, k * 128:(k + 1) * 128], ph_t["pst"][:, 0:nchunks, :], [ph_t["pstb"]],
                          writes=[buf_] if k == 0 else (), pwrites=[buf_] if k > 0 else ())
                acc["cnt"] += 1
                last = (s == 31) or (s == 33)
                if acc["cnt"] == 4 or last:
                    n = acc["cnt"]
                    s0 = acc["s0"]
                    for cc in range(nchunks):
                        P.op("pool", lambda e, cc=cc, n=n, s0=s0, tile_=tile_: e.dma_start(
                            out=BR[row0 + cc * 128:row0 + (cc + 1) * 128, s0 * 128:(s0 + n) * 128], in_=tile_[:, cc, 0:n * 128]),
                            reads=[buf_], pwrites=[BRb])
                    acc["cnt"] = 0

            if upto >= 2:
              with ExitStack() as ph:
                P.barrier()
                kA = sb(ph, "kA", [64, T], BF16)
                vA = sb(ph, "vA", [128, 34, 65], BF16)
                qA = sb(ph, "qA", [64, 4, T], BF16)
                wmask = sb(ph, "wmask", [128, 2, 512], BF16)
                wmB = Buf()
                P.op("sync", lambda e: e.dma_start(out=wmask[:], in_=wmask_d), writes=[wmB])
                Er = Rot([sb(ph, f"EA{i}", [128, 512], BF16) for i in range(10)])
                oat = Rot([sb(ph, f"oat{i}", [128, 256], BF16) for i in range(3)])
                den = Rot([sb(ph, f"denA{i}", [128, 4], F32) for i in range(3)])
                ph_t = {"pst": psum[6][:].rearrange("p (c t) -> p c t", t=128)[:, 0:2, :], "pstb": psb[6],
                        "brt": Rot([sb(ph, f"brtA{i}", [128, 2, 512], BF16) for i in range(2)])}
                kvB = Buf()
                for g in range(2):
                    P.op("sync", lambda e, g=g: e.dma_start(out=kA[:], in_=FM[4 * 128 + g * 64:4 * 128 + (g + 1) * 64, :]), reads=[FMb], writes=[kvB])
                    P.op("sync", lambda e, g=g: e.dma_start(out=vA[:, :, 0:64], in_=TM[:, g * 64:(g + 1) * 64].rearrange("(j p) c -> p j c", p=128)),
                         reads=[TMb], pwrites=[kvB])
                    P.op("vector", lambda e: e.memset(vA[:, :, 64:65], 1.0), pwrites=[kvB])
                    for r in range(4):
                        hh = g * 4 + r
                        P.op("sync", lambda e, r=r, hh=hh: e.dma_start(out=qA[:, r, :], in_=FM[hh * 64:(hh + 1) * 64, :]), reads=[FMb], pwrites=[kvB])
                    acc = {"cnt": 0}
                    def stA1(i):
                        if i < 32:
                            keys = ([(i - 1, 0)] if i > 0 else []) + [(i, None)] + ([(i + 1, 1)] if i < 31 else []) + [(32, None), (33, None)]
                        else:
                            keys = [(32, None), (33, None)]
                        Es = []
                        for (j, mk) in keys:
                            p = 0 + (Er.i % 4)
                            P.op("tensor", lambda e, p=p, j=j, i=i: e.matmul(
                                psum[p][:], lhsT=kA[:, j * 128:(j + 1) * 128], rhs=qA[:, :, i * 128:(i + 1) * 128], start=True, stop=True),
                                reads=[kvB], writes=[psb[p]])
                            Et, Eb = Er.next()
                            P.op("scalar", lambda e, p=p, Et=Et: e.activation(out=Et[:], in_=psum[p][:], func=AF.Exp, scale=0.125),
                                 reads=[psb[p]], writes=[Eb])
                            if mk is not None:
                                P.op("vector", lambda e, Et=Et, mk=mk: e.tensor_tensor(out=Et[:], in0=Et[:], in1=wmask[:, mk, :], op=ALU.mult),
                                     reads=[wmB], writes=[Eb])
                            Es.append((j, Et, Eb))
                        return Es

                    def stA2(i, Es):
                        po = 4 + (i % 2)
                        pov = psum[po][:, 0:260].rearrange("p (r c) -> p r c", c=65)
                        for r in range(4):
                            for n_, (j, Et, Eb) in enumerate(Es):
                                P.op("tensor", lambda e, r=r, j=j, Et=Et, n_=n_, pov=pov: e.matmul(
                                    pov[:, r, :], lhsT=Et[:, r * 128:(r + 1) * 128], rhs=vA[:, j, :], start=(n_ == 0), stop=(n_ == len(Es) - 1)),
                                    reads=[Eb, kvB], writes=[psb[po]])
                        dn, dnb = den.next()
                        P.op("vector", lambda e, dn=dn, pov=pov, g=g: e.tensor_tensor(
                            out=dn[:], in0=pov[:, :, 64], in1=esink[:, l * 8 + g * 4:l * 8 + g * 4 + 4], op=ALU.add),
                            reads=[psb[po], cB], writes=[dnb])
                        P.op("vector", lambda e, dn=dn: e.reciprocal(out=dn[:], in_=dn[:]), writes=[dnb])
                        ot, otb = oat.next()
                        for r in range(4):
                            P.op("vector", lambda e, r=r, ot=ot, pov=pov, dn=dn: e.tensor_scalar(
                                out=ot[:, r * 64:(r + 1) * 64], in0=pov[:, r, 0:64], scalar1=dn[:, r:r + 1], scalar2=None, op0=ALU.mult),
                                reads=[psb[po], dnb], writes=[otb] if r == 0 else (), pwrites=[otb] if r > 0 else ())
                        return ot, otb

                    nb = len(qblocks)
                    stash1, stash2 = {}, {}
                    for k in range(nb + 2):
                        if k < nb:
                            stash1[k] = stA1(qblocks[k])
                        if 0 <= k - 1 < nb:
                            stash2[k - 1] = stA2(qblocks[k - 1], stash1.pop(k - 1))
                        if 0 <= k - 2 < nb:
                            ot, otb = stash2.pop(k - 2)
                            transpose_store(ph_t, ot, otb, 2, g * 256, qblocks[k - 2], acc)

            if upto >= 3:
              with ExitStack() as ph:
                P.barrier()
                rt = sb(ph, "rt", [128, 8, 128], F32)
                rtB = Buf()
                P.op("sync", lambda e: e.dma_start(out=rt[:], in_=rettab_d), writes=[rtB])
                qB_ = sb(ph, "qB", [64, T], BF16)
                kB_ = sb(ph, "kB", [64, T], BF16)
                vB_ = sb(ph, "vB", [128, 34, 128], BF16)
                gB_ = sb(ph, "gB", [128, 34, 128], BF16)
                MT = sb(ph, "MT", [128, 128], F32)
                MT2 = sb(ph, "MT2", [128, 128], F32)
                kd = sb(ph, "kd", [128, 4], F32)
                qdf = sb(ph, "qdf", [64, 128], BF16)
                qdb = sb(ph, "qdb", [64, 128], BF16)
                kvF = sb(ph, "kvF", [64, 34, 128], F32)
                kvBk = sb(ph, "kvBk", [64, 34, 128], F32)
                Sin = sb(ph, "Sin", [64, 34, 128], F32)
                Tin = sb(ph, "Tin", [64, 34, 128], F32)
                SinB = sb(ph, "SinB", [64, 34, 128], BF16)
                TinB = sb(ph, "TinB", [64, 34, 128], BF16)
                Kf = Rot([sb(ph, f"Kf{i}", [128, 64], BF16) for i in range(2)])
                Kb = Rot([sb(ph, f"Kb{i}", [128, 64], BF16) for i in range(2)])
                Sm = Rot([sb(ph, f"Sm{i}", [128, 128], BF16) for i in range(3)])
                Qf = Rot([sb(ph, f"Qf{i}", [64, 128], BF16) for i in range(3)])
                Qb = Rot([sb(ph, f"Qb{i}", [64, 128], BF16) for i in range(3)])
                obt = Rot([sb(ph, f"obt{i}", [128, 128], BF16) for i in range(3)])
                ssr = Rot([sb(ph, f"ssr{i}", [128, 2], F32) for i in range(3)])
                junk = sb(ph, "junkB", [128, 128], F32)
                ph_t = {"pst": psum[6][:].rearrange("p (c t) -> p c t", t=128)[:, 0:1, :], "pstb": psb[6],
                        "brt": Rot([sb(ph, f"brtB{i}", [128, 1, 512], BF16) for i in range(2)])}
                ldB, tbB, kvb_, scB = Buf(), Buf(), Buf(), Buf()
                ptbufs = [Buf(), Buf()]
                for h in range(4):
                    ch, half = h // 2, h % 2
                    P.op("sync", lambda e, ch=ch, half=half: e.dma_start(out=qB_[:], in_=FM[(5 + ch) * 128 + half * 64:(5 + ch) * 128 + half * 64 + 64, :]),
                         reads=[FMb], writes=[ldB])
                    P.op("sync", lambda e, ch=ch, half=half: e.dma_start(out=kB_[:], in_=FM[(7 + ch) * 128 + half * 64:(7 + ch) * 128 + half * 64 + 64, :]),
                         reads=[FMb], pwrites=[ldB])
                    P.op("sync", lambda e, h=h: e.dma_start(out=vB_[:], in_=TM[:, 128 + h * 128:128 + (h + 1) * 128].rearrange("(j p) c -> p j c", p=128)),
                         reads=[TMb], pwrites=[ldB])
                    P.op("sync", lambda e, h=h: e.dma_start(out=gB_[:], in_=TM[:, 640 + h * 128:640 + (h + 1) * 128].rearrange("(j p) c -> p j c", p=128)),
                         reads=[TMb], pwrites=[ldB])
                    lf = lgt[:, l * 8 + h:l * 8 + h + 1]
                    lb = lgt[:, l * 8 + 4 + h:l * 8 + 4 + h + 1]
                    P.op("scalar", lambda e, lf=lf: e.activation(out=MT[:], in_=rt[:, 0, :], func=AF.Exp, scale=lf), reads=[rtB, cB], writes=[tbB])
                    P.op("scalar", lambda e, lb=lb: e.activation(out=MT2[:], in_=rt[:, 2, :], func=AF.Exp, scale=lb), reads=[rtB, cB], pwrites=[tbB])
                    P.op("vector", lambda e: e.tensor_tensor(out=MT[:], in0=MT[:], in1=rt[:, 1, :], op=ALU.mult), reads=[tbB, rtB], writes=[tbB])
                    P.op("vector", lambda e: e.tensor_tensor(out=MT2[:], in0=MT2[:], in1=rt[:, 3, :], op=ALU.mult), reads=[tbB], writes=[tbB])
                    P.op("vector", lambda e: e.scalar_tensor_tensor(out=MT[:], in0=MT[:], scalar=0.125, in1=MT2[:], op0=ALU.mult, op1=ALU.add),
                         reads=[tbB], writes=[tbB])
                    P.op("vector", lambda e: e.scalar_tensor_tensor(out=MT[:], in0=MT2[:], scalar=-0.875, in1=MT[:], op0=ALU.mult, op1=ALU.add),
                         reads=[tbB], writes=[tbB])
                    P.op("scalar", lambda e, lf=lf: e.activation(out=kd[:, 0:1], in_=rt[:, 6, 0:1], func=AF.Exp, scale=lf), reads=[tbB], writes=[tbB])
                    P.op("scalar", lambda e, lb=lb: e.activation(out=kd[:, 1:2], in_=rt[:, 6, 1:2], func=AF.Exp, scale=lb), reads=[tbB], writes=[tbB])
                    P.op("scalar", lambda e, lf=lf: e.activation(out=kd[:, 2:3], in_=rt[:, 6, 2:3], func=AF.Exp, scale=lf), reads=[tbB], writes=[tbB])
                    P.op("scalar", lambda e, lb=lb: e.activation(out=kd[:, 3:4], in_=rt[:, 6, 2:3], func=AF.Exp, scale=lb), reads=[tbB], writes=[tbB])
                    P.op("vector", lambda e: e.tensor_scalar(out=kd[:, 0:2], in0=kd[:, 0:2], scalar1=0.125, scalar2=None, op0=ALU.mult), writes=[tbB])
                    P.op("scalar", lambda e, lf=lf: e.activation(out=qdf[:], in_=rt[0:64, 4, :], func=AF.Exp, scale=lf[0:64]), reads=[tbB], writes=[tbB])
                    P.op("scalar", lambda e, lb=lb: e.activation(out=qdb[:], in_=rt[0:64, 5, :], func=AF.Exp, scale=lb[0:64]), reads=[tbB], writes=[tbB])
                    def stP1(n):
                        pt = psum[6][:, 0:64] if n % 2 == 0 else psum[7][:, 0:32].bitcast(BF16)
                        ptb = psb[6] if n % 2 == 0 else psb[7]
                        P.op("tensor", lambda e, n=n, pt=pt: e.transpose(out=pt, in_=kB_[:, n * 128:(n + 1) * 128], identity=identb[0:64, 0:64]),
                             reads=[ldB, cB], writes=[ptb])
                        kf, kfb = Kf.next()
                        kb, kbb = Kb.next()
                        P.op("vector", lambda e, kf=kf, pt=pt: e.tensor_scalar(out=kf[:], in0=pt, scalar1=kd[:, 0:1], scalar2=None, op0=ALU.mult),
                             reads=[ptb, tbB], writes=[kfb])
                        P.op("vector", lambda e, kb=kb, pt=pt: e.tensor_scalar(out=kb[:], in0=pt, scalar1=kd[:, 1:2], scalar2=None, op0=ALU.mult),
                             reads=[ptb, tbB], writes=[kbb])
                        return kf, kfb, kb, kbb

                    def stP2(n, st):
                        kf, kfb, kb, kbb = st
                        pa, pb = n % 2, 2 + n % 2
                        P.op("tensor", lambda e, n=n, kf=kf, pa=pa: e.matmul(psum[pa][0:64, 0:128], lhsT=kf[:], rhs=vB_[:, n, :], start=True, stop=True),
                             reads=[kfb, ldB], writes=[psb[pa]])
                        P.op("tensor", lambda e, n=n, kb=kb, pb=pb: e.matmul(psum[pb][0:64, 0:128], lhsT=kb[:], rhs=vB_[:, n, :], start=True, stop=True),
                             reads=[kbb, ldB], writes=[psb[pb]])
                        P.op("scalar", lambda e, n=n, pa=pa: e.copy(out=kvF[:, n, :], in_=psum[pa][0:64, 0:128]), reads=[psb[pa]], pwrites=[kvb_])
                        P.op("scalar", lambda e, n=n, pb=pb: e.copy(out=kvBk[:, n, :], in_=psum[pb][0:64, 0:128]), reads=[psb[pb]], pwrites=[kvb_])

                    shp = {}
                    for k in range(35):
                        if k < 34:
                            shp[k] = stP1(k)
                        if k >= 1:
                            stP2(k - 1, shp.pop(k - 1))
                    G, Gb = kd[0:64, 2:3], kd[0:64, 3:4]
                    P.op("vector", lambda e: e.memset(Sin[:, 32, :], 0.0), reads=[kvb_], writes=[scB])
                    P.op("vector", lambda e: e.memset(Tin[:, 33, :], 0.0), writes=[scB])
                    P.op("vector", lambda e: e.tensor_copy(out=Sin[:, 33, :], in_=kvF[:, 32, :]), reads=[kvb_], writes=[scB])
                    P.op("vector", lambda e: e.tensor_copy(out=Tin[:, 32, :], in_=kvBk[:, 33, :]), reads=[kvb_], writes=[scB])
                    P.op("vector", lambda e: e.scalar_tensor_tensor(out=Sin[:, 0, :], in0=Sin[:, 33, :], scalar=G, in1=kvF[:, 33, :], op0=ALU.mult, op1=ALU.add),
                         reads=[tbB, kvb_], writes=[scB])
                    P.op("vector", lambda e: e.scalar_tensor_tensor(out=Tin[:, 31, :], in0=Tin[:, 32, :], scalar=Gb, in1=kvBk[:, 32, :], op0=ALU.mult, op1=ALU.add),
                         reads=[kvb_], writes=[scB])
                    for n in range(31):
                        P.op("vector", lambda e, n=n: e.scalar_tensor_tensor(
                            out=Sin[:, n + 1, :], in0=Sin[:, n, :], scalar=G, in1=kvF[:, n, :], op0=ALU.mult, op1=ALU.add), reads=[kvb_], writes=[scB])
                        m = 31 - n
                        P.op("vector", lambda e, m=m: e.scalar_tensor_tensor(
                            out=Tin[:, m - 1, :], in0=Tin[:, m, :], scalar=Gb, in1=kvBk[:, m, :], op0=ALU.mult, op1=ALU.add), reads=[kvb_], writes=[scB])
                    P.op("vector", lambda e: e.tensor_copy(out=SinB[:], in_=Sin[:]), writes=[scB])
                    P.op("vector", lambda e: e.tensor_copy(out=TinB[:], in_=Tin[:]), reads=[scB], pwrites=[scB])
                    scB2 = scB
                    acc = {"cnt": 0}

                    def stB1(n):
                        ps_ = 0 + n % 2
                        P.op("tensor", lambda e, n=n, ps_=ps_: e.matmul(psum[ps_][:, 0:128], lhsT=kB_[:, n * 128:(n + 1) * 128], rhs=qB_[:, n * 128:(n + 1) * 128],
                                                                     start=True, stop=True), reads=[ldB], writes=[psb[ps_]])
                        sm, smb = Sm.next()
                        P.op("vector", lambda e, sm=sm, ps_=ps_: e.tensor_tensor(out=sm[:], in0=psum[ps_][:, 0:128], in1=MT[:], op=ALU.mult),
                             reads=[psb[ps_], tbB], writes=[smb])
                        qf, qfb = Qf.next()
                        qb, qbb = Qb.next()
                        P.op("vector", lambda e, n=n, qf=qf: e.tensor_tensor(out=qf[:], in0=qB_[:, n * 128:(n + 1) * 128], in1=qdf[:], op=ALU.mult),
                             reads=[ldB, tbB], writes=[qfb])
                        P.op("vector", lambda e, n=n, qb=qb: e.tensor_tensor(out=qb[:], in0=qB_[:, n * 128:(n + 1) * 128], in1=qdb[:], op=ALU.mult),
                             reads=[ldB, tbB], writes=[qbb])
                        return (sm, smb, qf, qfb, qb, qbb)

                    def stB2(n, st):
                        sm, smb, qf, qfb, qb, qbb = st
                        po_ = 2 + n % 2
                        P.op("tensor", lambda e, n=n, sm=sm, po_=po_: e.matmul(psum[po_][:, 0:128], lhsT=sm[:], rhs=vB_[:, n, :], start=True, stop=False),
                             reads=[smb, ldB], writes=[psb[po_]])
                        P.op("tensor", lambda e, n=n, qf=qf, po_=po_: e.matmul(psum[po_][:, 0:128], lhsT=qf[:], rhs=SinB[:, n, :], start=False, stop=False),
                             reads=[qfb, scB2], writes=[psb[po_]])
                        P.op("tensor", lambda e, n=n, qb=qb, po_=po_: e.matmul(psum[po_][:, 0:128], lhsT=qb[:], rhs=TinB[:, n, :], start=False, stop=True),
                             reads=[qbb, scB2], writes=[psb[po_]])
                        ss, ssb = ssr.next()
                        P.op("scalar", lambda e, ss=ss, po_=po_: e.activation(out=junk[:], in_=psum[po_][:, 0:128], func=AF.Square, accum_out=ss[:, 0:1]),
                             reads=[psb[po_]], writes=[ssb])
                        P.op("scalar", lambda e, ss=ss: e.activation(out=ss[:, 1:2], in_=ss[:, 0:1], func=AF.Ln, scale=1.0 / 128, bias=epsc[:, 0:1]), writes=[ssb])
                        P.op("scalar", lambda e, ss=ss: e.activation(out=ss[:, 1:2], in_=ss[:, 1:2], func=AF.Exp, scale=-0.5), writes=[ssb])
                        ob, obb = obt.next()
                        P.op("vector", lambda e, n=n, ob=ob, ss=ss, po_=po_: e.scalar_tensor_tensor(
                            out=ob[:], in0=psum[po_][:, 0:128], scalar=ss[:, 1:2], in1=gB_[:, n, :], op0=ALU.mult, op1=ALU.mult),
                            reads=[psb[po_], ssb, ldB], writes=[obb])
                        return ob, obb

                    nb = len(qblocks)
                    sh1, sh2 = {}, {}
                    for k in range(nb + 2):
                        if k < nb:
                            sh1[k] = stB1(qblocks[k])
                        if 0 <= k - 1 < nb:
                            sh2[k - 1] = stB2(qblocks[k - 1], sh1.pop(k - 1))
                        if 0 <= k - 2 < nb:
                            ob, obb = sh2.pop(k - 2)
                            transpose_store(ph_t, ob, obb, 1, 512 + h * 128, qblocks[k - 2], acc)

            if upto >= 4:
              with ExitStack() as ph:
                P.barrier()
                Osb = [sb(ph, f"Osb{c}", [128, 4, 129], F32) for c in range(2)]
                Osbb = [Buf(), Buf()]
                t1c = Rot([sb(ph, f"t1c{i}", [128, 128], F32) for i in range(2)])
                occ = Rot([sb(ph, f"occ{i}", [128, 128], F32) for i in range(2)])
                junk = sb(ph, "junkC", [128, 128], F32)
                kD2 = sb(ph, "kD2", [128, T], BF16)
                qD = [sb(ph, f"qD{c}", [128, T], BF16) for c in range(2)]
                vD = sb(ph, "vD", [128, 34, 129], BF16)
                E = [sb(ph, f"ED{c}", [128, 34, 512], BF16) for c in range(2)]
                Ebuf = [Buf(), Buf()]
                oct_ = Rot([sb(ph, f"oct{i}", [128, 128], BF16) for i in range(2)])
                rcp = Rot([sb(ph, f"rcp{i}", [128, 4], F32) for i in range(2)])
                ph_t = {"pst": psum[6][:].rearrange("p (c t) -> p c t", t=128)[:, 0:1, :], "pstb": psb[6],
                        "brt": Rot([sb(ph, f"brtC{i}", [128, 1, 512], BF16) for i in range(2)])}
                ldB, zB = Buf(), Buf()
                P.op("vector", lambda e: e.memset(qD[0][64:128, :], 0.0), pwrites=[zB])
                P.op("vector", lambda e: e.memset(qD[1][0:64, :], 0.0), pwrites=[zB])
                P.op("vector", lambda e: e.memset(vD[:, :, 128:129], 1.0), pwrites=[zB])
                for h in range(4):
                    P.op("sync", lambda e, h=h: e.dma_start(out=kD2[:], in_=FM[(13 + h) * 128:(14 + h) * 128, :]), reads=[FMb], writes=[ldB])
                    P.op("sync", lambda e, h=h: e.dma_start(out=qD[0][0:64, :], in_=FM[(9 + h) * 128:(9 + h) * 128 + 64, :]), reads=[FMb], pwrites=[ldB])
                    P.op("sync", lambda e, h=h: e.dma_start(out=qD[1][64:128, :], in_=FM[(9 + h) * 128 + 64:(10 + h) * 128, :]), reads=[FMb], pwrites=[ldB])
                    P.op("sync", lambda e, h=h: e.dma_start(out=vD[:, :, 0:128], in_=TM[:, 1152 + h * 128:1152 + (h + 1) * 128].rearrange("(j p) c -> p j c", p=128)),
                         reads=[TMb], pwrites=[ldB])
                    acc = {"cnt": 0}
                    PVB = [4, 4, 5, 5]
                    pairs = [(ps01, 0, 1), (ps23, 2, 3)]
                    def finish_unit(ti, c):
                        t0, w = TT[ti]
                        nr = w // 128
                        for r in range(nr):
                            P.op("vector", lambda e, r=r, c=c: e.tensor_copy(out=Osb[c][:, r, :], in_=psum[PVB[r]][:, (r % 2) * 129:(r % 2) * 129 + 129]),
                                 reads=[psb[PVB[r]]], writes=[Osbb[c]] if r == 0 else (), pwrites=[Osbb[c]] if r else ())
                        if c == 0:
                            return
                        for r in range(nr):
                            o1 = Osb[0][:, r, :]
                            o2 = Osb[1][:, r, :]
                            rc, rcb = rcp.next()
                            P.op("vector", lambda e, rc=rc, o1=o1: e.reciprocal(out=rc[:, 0:1], in_=o1[:, 128:129]), reads=[Osbb[0]], writes=[rcb])
                            P.op("vector", lambda e, rc=rc, o2=o2: e.reciprocal(out=rc[:, 1:2], in_=o2[:, 128:129]), reads=[Osbb[1]], writes=[rcb])
                            P.op("vector", lambda e, rc=rc: e.tensor_tensor(out=rc[:, 1:2], in0=rc[:, 1:2], in1=neglam[:, l:l + 1], op=ALU.mult),
                                 reads=[cB], writes=[rcb])
                            t1, t1b = t1c.next()
                            oc_, ocb = occ.next()
                            P.op("vector", lambda e, t1=t1, rc=rc, o1=o1: e.tensor_scalar(out=t1[:], in0=o1[:, 0:128], scalar1=rc[:, 0:1], scalar2=None, op0=ALU.mult),
                                 reads=[Osbb[0], rcb], writes=[t1b])
                            P.op("vector", lambda e, t1=t1, rc=rc, o2=o2, oc_=oc_: e.scalar_tensor_tensor(
                                out=oc_[:], in0=o2[:, 0:128], scalar=rc[:, 1:2], in1=t1[:], op0=ALU.mult, op1=ALU.add),
                                reads=[Osbb[1], rcb, t1b], writes=[ocb])
                            P.op("scalar", lambda e, rc=rc, oc_=oc_: e.activation(out=junk[:], in_=oc_[:], func=AF.Square, accum_out=rc[:, 2:3]),
                                 reads=[ocb], writes=[rcb])
                            P.op("scalar", lambda e, rc=rc: e.activation(out=rc[:, 3:4], in_=rc[:, 2:3], func=AF.Ln, scale=1.0 / 128, bias=epsc[:, 0:1]), writes=[rcb])
                            P.op("scalar", lambda e, rc=rc: e.activation(out=rc[:, 3:4], in_=rc[:, 3:4], func=AF.Exp, scale=-0.5), writes=[rcb])
                            ot, otb = oct_.next()
                            P.op("vector", lambda e, ot=ot, oc_=oc_, rc=rc: e.scalar_tensor_tensor(
                                out=ot[:], in0=oc_[:], scalar=rc[:, 3:4], in1=gainb[:, l, :], op0=ALU.mult, op1=ALU.mult),
                                reads=[ocb, rcb, cB], writes=[otb])
                            transpose_store(ph_t, ot, otb, 1, 1024 + h * 128, t0 // 128 + r, acc)


                    units = []
                    for ti, (t0, w) in enumerate(TT):
                        if ti == 8 and not ctx_out:
                            continue
                        for c in range(2):
                            units.append((ti, c))

                    def pv_ops(u_idx):
                        ti, c = units[u_idx]
                        t0, w = TT[ti]
                        keys = list(range(34)) if ti < 8 else [32, 33]
                        ops = []
                        for r in range(w // 128):
                            for n_, j in enumerate(keys):
                                ops.append((r, j, n_ == 0, n_ == len(keys) - 1, c))
                        return ops

                    def emit_pv(op):
                        r, j, st_, sp_, eb = op
                        P.op("tensor", lambda e: e.matmul(
                            psum[PVB[r]][:, (r % 2) * 129:(r % 2) * 129 + 129], lhsT=E[eb][:, j, r * 128:(r + 1) * 128], rhs=vD[:, j, :], start=st_, stop=sp_),
                            reads=[Ebuf[eb], ldB, zB], writes=[psb[PVB[r]]])

                    pcount = 0
                    for u_idx in range(len(units) + 1):
                        pend = pv_ops(u_idx - 1) if u_idx >= 1 else []
                        if u_idx < len(units):
                            ti, c = units[u_idx]
                            t0, w = TT[ti]
                            keys = list(range(34)) if ti < 8 else [32, 33]
                            npairs = len(keys) // 2
                            per = -(-len(pend) // npairs) if pend else 0
                            for jp in range(0, len(keys), 2):
                                pt_, pa_, pb_ = pairs[pcount % 2]
                                pcount += 1
                                for hh, pbk in ((0, pa_), (1, pb_)):
                                    j = keys[jp + hh]
                                    P.op("tensor", lambda e, c=c, j=j, pbk=pbk: e.matmul(
                                        psum[pbk][:, 0:w], lhsT=kD2[:, j * 128:(j + 1) * 128], rhs=qD[c][:, t0:t0 + w], start=True, stop=True),
                                        reads=[ldB, zB], writes=[psb[pbk]])
                                j0 = keys[jp]
                                P.op("scalar", lambda e, c=c, j0=j0, pt_=pt_: e.activation(
                                    out=E[c][:, j0:j0 + 2, 0:w], in_=pt_[:].rearrange("p (a b) -> p a b", b=512)[:, :, 0:w], func=AF.Exp, scale=0.125),
                                    reads=[psb[pa_], psb[pb_]], writes=[Ebuf[c]] if jp == 0 else (), pwrites=[Ebuf[c]] if jp else ())
                                for _ in range(per):
                                    if pend:
                                        emit_pv(pend.pop(0))
                        while pend:
                            emit_pv(pend.pop(0))
                        if u_idx >= 1:
                            finish_unit(*units[u_idx - 1])

            if upto >= 5:
              with ExitStack() as ph:
                P.barrier()
                wbr = sb(ph, "wbr", [128, 12, D], BF16)
                wo = sb(ph, "wo", [128, 8, D], BF16)
                wB = Buf()
                stg = Rot([sb(ph, f"stgM{i}", [128, D], F32) for i in range(3)])
                cf = 0
                for k in range(20):
                    st_, stb = stg.next()
                    src = w_branch[l].rearrange("i (kc k) n -> k (i kc) n", k=128)[:, k, :] if k < 12 else \
                        w_out[l].rearrange("(kc k) n -> k kc n", k=128)[:, k - 12, :]
                    dstw = wbr[:, k, :] if k < 12 else wo[:, k - 12, :]
                    P.op("sync", lambda e, st_=st_, src=src: e.dma_start(out=st_[:], in_=src), writes=[stb])
                    cf ^= 1
                    evac_copy(dstw, st_[:], [stb], pwrites=[wB])
                brr = Rot([sb(ph, f"brr{i}", [128, 12, 512], BF16) for i in range(2)])
                glr = Rot([sb(ph, f"glr{i}", [128, 24, 512], BF16) for i in range(2)])
                xr = Rot([sb(ph, f"xrM{i}", [128, 8, 512], F32) for i in range(2)])
                yr = Rot([sb(ph, f"yrM{i}", [128, 8, 512], BF16) for i in range(2)])
                ya = Rot([sb(ph, f"yaM{i}", [128, 512], F32) for i in range(2)])
                yb_ = Rot([sb(ph, f"ybM{i}", [128, 512], F32) for i in range(2)])
                pi = [0]

                def nps():
                    pi[0] = (pi[0] + 1) % 6
                    return pi[0]
                for ti, (t0, w) in enumerate(TT):
                    if ti == 8 and not ctx_out:
                        continue
                    col = 1 if ti == 8 else 0
                    br_, brb = brr.next()
                    gl_, glb = glr.next()
                    xt, xtb = xr.next()
                    yt, ytb = yr.next()
                    P.op("sync", lambda e, br_=br_, t0=t0, w=w: e.dma_start(out=br_[:, :, 0:w], in_=BR.rearrange("(c k) t -> k c t", k=128)[:, :, t0:t0 + w]),
                         reads=[BRb], writes=[brb])
                    P.op("sync", lambda e, gl_=gl_, t0=t0, w=w: e.dma_start(out=gl_[:, :, 0:w], in_=FM[17 * 128:41 * 128, :].rearrange("(c k) t -> k c t", k=128)[:, :, t0:t0 + w]),
                         reads=[FMb], writes=[glb])
                    P.op("sync", lambda e, xt=xt, t0=t0, w=w: e.dma_start(out=xt[:, :, 0:w], in_=xT.rearrange("(kc k) t -> k kc t", k=128)[:, :, t0:t0 + w]),
                         reads=[xb[ti]], writes=[xtb])
                    for oc in range(8):
                        yacc, yab = ya.next()
                        for i in range(3):
                            p = nps()
                            for kc in range(4):
                                P.op("tensor", lambda e, p=p, i=i, kc=kc, oc=oc, br_=br_, w=w: e.matmul(
                                    psum[p][:, 0:w], lhsT=wbr[:, i * 4 + kc, oc * 128:(oc + 1) * 128], rhs=br_[:, i * 4 + kc, 0:w], start=(kc == 0), stop=(kc == 3)),
                                    reads=[wB, brb], writes=[psb[p]])
                            if i == 0:
                                P.op("vector", lambda e, p=p, yacc=yacc, gl_=gl_, oc=oc, w=w: e.tensor_tensor(
                                    out=yacc[:, 0:w], in0=psum[p][:, 0:w], in1=gl_[:, oc, 0:w], op=ALU.mult), reads=[psb[p], glb], writes=[yab])
                            else:
                                y2, y2b = yb_.next()
                                P.op("vector", lambda e, p=p, y2=y2, gl_=gl_, oc=oc, i=i, w=w: e.tensor_tensor(
                                    out=y2[:, 0:w], in0=psum[p][:, 0:w], in1=gl_[:, i * 8 + oc, 0:w], op=ALU.mult), reads=[psb[p], glb], writes=[y2b])
                                if i == 1:
                                    P.op("vector", lambda e, yacc=yacc, y2=y2, w=w: e.tensor_tensor(out=yacc[:, 0:w], in0=yacc[:, 0:w], in1=y2[:, 0:w], op=ALU.add),
                                         reads=[y2b], writes=[yab])
                                else:
                                    P.op("vector", lambda e, yacc=yacc, y2=y2, yt=yt, oc=oc, w=w: e.tensor_tensor(
                                        out=yt[:, oc, 0:w], in0=yacc[:, 0:w], in1=y2[:, 0:w], op=ALU.add),
                                        reads=[y2b, yab], writes=[ytb] if oc == 0 else (), pwrites=[ytb] if oc else ())
                    for oc in range(8):
                        p = nps()
                        for kc in range(8):
                            P.op("tensor", lambda e, p=p, kc=kc, oc=oc, yt=yt, w=w: e.matmul(
                                psum[p][:, 0:w], lhsT=wo[:, kc, oc * 128:(oc + 1) * 128], rhs=yt[:, kc, 0:w], start=(kc == 0), stop=(kc == 7)),
                                reads=[wB, ytb], writes=[psb[p]])
                        P.op("vector", lambda e, p=p, oc=oc, xt=xt, col=col, w=w: e.scalar_tensor_tensor(
                            out=xt[:, oc, 0:w], in0=psum[p][:, 0:w], scalar=mv(l, col, 2)[:, oc:oc + 1], in1=xt[:, oc, 0:w], op0=ALU.mult, op1=ALU.add),
                            reads=[psb[p], cB], writes=[xtb])
                    P.op("pool", lambda e, xt=xt, t0=t0, w=w: e.dma_start(out=xT.rearrange("(kc k) t -> k kc t", k=128)[:, :, t0:t0 + w], in_=xt[:, :, 0:w]),
                         reads=[xtb], writes=[xb[ti]])

            is_moe = (l % 2 == 1)
            l2 = l // 2
            if upto >= 6:
              with ExitStack() as ph:
                P.barrier()
                tiles = {
                    "x": Rot([sb(ph, f"n2x{i}", [128, 8, 512], F32) for i in range(2)]),
                    "sq": Rot([sb(ph, f"n2sq{i}", [128, 8, 512], BF16) for i in range(2)]),
                    "rs": Rot([sb(ph, f"n2rs{i}", [128, 512], F32) for i in range(3)]),
                    "tmp": Rot([sb(ph, f"n2tmp{i}", [128, 512], F32) for i in range(6)]),
                    "h": Rot([sb(ph, f"n2h{i}", [128, 8, 512], BF16) for i in range(2)]),
                    "hf": Rot([sb(ph, f"n2hf{i}", [128, 8, 512], F32) for i in range(2 if is_moe else 0)]),
                }
                if is_moe:
                    wr = sb(ph, "wr", [128, 8, NE], F32)
                    wrB = Buf()
                    P.op("sync", lambda e: e.dma_start(out=wr[:], in_=moe_router[l2].rearrange("(kc k) n -> k kc n", k=128)), writes=[wrB])
                    lg_ = Rot([sb(ph, f"lg{i}", [128, 8], F32) for i in range(2)])
                    mx_ = Rot([sb(ph, f"mx{i}", [128, 8], F32) for i in range(2)])
                    gt_ = Rot([sb(ph, f"gt{i}", [128, 8], F32) for i in range(2)])
                    g2_ = Rot([sb(ph, f"g2{i}", [128, 8], F32) for i in range(2)])
                    gT_ = Rot([sb(ph, f"gT{i}", [8, 512], F32) for i in range(2)])
                def n2_post(stA):
                    ti = stA[0]
                    t0, w = TT[ti]
                    xt, xtb, ht, htb, hf, hfb = norm_B(tiles, l, stA, 3, want_f32=is_moe)
                    P.op("pool", lambda e, ht=ht, t0=t0, w=w: e.dma_start(out=H2.rearrange("(kc k) t -> k kc t", k=128)[:, :, t0:t0 + w], in_=ht[:, :, 0:w]),
                         reads=[htb], pwrites=[H2b])
                    if is_moe:
                        gT, gTb = gT_.next()
                        for s in range(w // 128):
                            for kc in range(8):
                                P.op("tensor", lambda e, s=s, kc=kc: e.matmul(psum[0][:, 0:8], lhsT=hf[:, kc, s * 128:(s + 1) * 128], rhs=wr[:, kc, :],
                                                                             start=(kc == 0), stop=(kc == 7)), reads=[hfb, wrB], writes=[psb[0]])
                            lg, lgb = lg_.next()
                            mx, mxb = mx_.next()
                            gt, gtb = gt_.next()
                            g2, g2b = g2_.next()
                            P.op("vector", lambda e, lg=lg: e.tensor_copy(out=lg[:], in_=psum[0][:, 0:8]), reads=[psb[0]], writes=[lgb])
                            P.op("vector", lambda e, lg=lg, mx=mx: e.max(out=mx[:], in_=lg[:]), reads=[lgb], writes=[mxb])
                            P.op("vector", lambda e, mx=mx: e.tensor_tensor(out=mx[:, 2:3], in0=mx[:, 0:1], in1=mx[:, 1:2], op=ALU.subtract), writes=[mxb])
                            P.op("scalar", lambda e, mx=mx: e.activation(out=mx[:, 3:4], in_=mx[:, 2:3], func=AF.Sigmoid), reads=[mxb], writes=[mxb])
                            P.op("scalar", lambda e, mx=mx: e.activation(out=mx[:, 4:5], in_=mx[:, 2:3], func=AF.Sigmoid, scale=-1.0), writes=[mxb])
                            P.op("vector", lambda e, lg=lg, mx=mx, gt=gt: e.tensor_scalar(
                                out=gt[:], in0=lg[:], scalar1=mx[:, 0:1], scalar2=mx[:, 3:4], op0=ALU.is_equal, op1=ALU.mult), reads=[lgb, mxb], writes=[gtb])
                            P.op("vector", lambda e, lg=lg, mx=mx, g2=g2: e.tensor_scalar(
                                out=g2[:], in0=lg[:], scalar1=mx[:, 1:2], scalar2=mx[:, 4:5], op0=ALU.is_equal, op1=ALU.mult), reads=[lgb, mxb], writes=[g2b])
                            P.op("vector", lambda e, gt=gt, g2=g2: e.tensor_tensor(out=gt[:], in0=gt[:], in1=g2[:], op=ALU.add), reads=[g2b], writes=[gtb])
                            P.op("tensor", lambda e, gt=gt: e.transpose(out=psum[1][0:8, 0:128], in_=gt[:], identity=identf[:]), reads=[gtb, cB], writes=[psb[1]])
                            P.op("vector", lambda e, gT=gT, s=s: e.tensor_copy(out=gT[:, s * 128:(s + 1) * 128], in_=psum[1][0:8, 0:128]),
                                 reads=[psb[1]], writes=[gTb] if s == 0 else (), pwrites=[gTb] if s else ())
                        P.op("pool", lambda e, gT=gT, t0=t0, w=w: e.dma_start(out=GT[:, t0:t0 + w], in_=gT[:, 0:w]), reads=[gTb], pwrites=[GTb])


                n2tiles = [ti for ti in range(len(TT)) if not (ti == 8 and not ctx_out)]
                pend = None
                for ti in n2tiles:
                    stA = norm_A(tiles, l, ti)
                    if pend is not None:
                        n2_post(pend)
                    pend = stA
                n2_post(pend)

            if upto >= 7:
              with ExitStack() as ph:
                P.barrier()
                FH = DFF // 2
                w1r = Rot([sb(ph, f"w1h{i}", [128, 8, FH], BF16) for i in range(2)])
                w3r = Rot([sb(ph, f"w3h{i}", [128, 8, FH], BF16) for i in range(2)])
                w2r = Rot([sb(ph, f"w2h{i}", [128, 11, D], BF16) for i in range(2)])
                stg = Rot([sb(ph, f"stgF{i}", [128, FH], F32) for i in range(3)])
                h2r = Rot([sb(ph, f"h2r{i}", [128, 8, 512], BF16) for i in range(2)])
                ur = Rot([sb(ph, f"ur{i}", [128, 11, 512], BF16) for i in range(2)])
                sr = Rot([sb(ph, f"sr{i}", [128, 512], F32) for i in range(2)])
                cr = Rot([sb(ph, f"cr{i}", [128, 512], F32) for i in range(4)])
                gr = Rot([sb(ph, f"gr{i}", [128, 512], F32) for i in range(2)])
                pi = [0]

                def nps():
                    pi[0] = (pi[0] + 1) % 7
                    return [0, 1, 2, 3, 4, 5, 7][pi[0]]
                experts = list(range(NE)) if is_moe else [None]
                pendF = [None]
                cf = 0
                def wsteps(ex, half):
                    if ex is None:
                        W1, W3, W2 = ffn_w1[l2], ffn_w3[l2], ffn_w2[l2]
                    else:
                        W1, W3, W2 = moe_w1[l2, ex], moe_w3[l2, ex], moe_w2[l2, ex]
                    w1h, w1b = w1r.next()
                    w3h, w3b = w3r.next()
                    w2h, w2b = w2r.next()
                    steps = []
                    for (Wsrc, wdst, wbuf) in [(W1, w1h, w1b), (W3, w3h, w3b)]:
                        for kc in range(8):
                            def f(Wsrc=Wsrc, wdst=wdst, wbuf=wbuf, kc=kc):
                                st_, stb = stg.next()
                                P.op("sync", lambda e: e.dma_start(out=st_[:], in_=Wsrc[kc * 128:(kc + 1) * 128, half * FH:(half + 1) * FH]), writes=[stb])
                                evac_copy(wdst[:, kc, :], st_[:], [stb], writes=[wbuf] if kc == 0 else (), pwrites=[wbuf] if kc else ())
                            steps.append(f)
                    for j in range(11):
                        def f(j=j):
                            st_, stb = stg.next()
                            P.op("sync", lambda e: e.dma_start(out=st_[:, 0:D], in_=W2[half * FH + j * 128:half * FH + (j + 1) * 128, :]), writes=[stb])
                            evac_copy(w2h[:, j, :], st_[:, 0:D], [stb], writes=[w2b] if j == 0 else (), pwrites=[w2b] if j else ())
                        steps.append(f)
                    return (w1h, w1b, w3h, w3b, w2h, w2b), steps

                halves = [(ex, half) for ex in experts for half in range(2)]
                cur_w, cur_steps = wsteps(*halves[0])
                for f in cur_steps:
                    f()
                for hi, (ex, half) in enumerate(halves):
                    if True:
                        w1h, w1b, w3h, w3b, w2h, w2b = cur_w
                        if hi + 1 < len(halves):
                            nxt_w, nxt_steps = wsteps(*halves[hi + 1])
                        else:
                            nxt_w, nxt_steps = None, []
                        def stF1(ti):
                            t0, w = TT[ti]
                            h2t, h2tb = h2r.next()
                            P.op("sync", lambda e, h2t=h2t, t0=t0, w=w: e.dma_start(out=h2t[:, :, 0:w], in_=H2.rearrange("(kc k) t -> k kc t", k=128)[:, :, t0:t0 + w]),
                                 reads=[H2b], writes=[h2tb])
                            gtile = gtb_ = None
                            if ex is not None:
                                gtile, gtb_ = gr.next()
                                P.op("sync", lambda e, gtile=gtile, t0=t0, w=w: e.dma_start(out=gtile[:, 0:w], in_=GT[ex:ex + 1, t0:t0 + w].broadcast_to([128, w])),
                                     reads=[GTb], writes=[gtb_])
                            ut, utb = ur.next()
                            for j in range(11):
                                p1, p3 = nps(), nps()
                                for kc in range(8):
                                    P.op("tensor", lambda e, p1=p1, kc=kc, j=j: e.matmul(
                                        psum[p1][:, 0:w], lhsT=w1h[:, kc, j * 128:(j + 1) * 128], rhs=h2t[:, kc, 0:w], start=(kc == 0), stop=(kc == 7)),
                                        reads=[w1b, h2tb], writes=[psb[p1]])
                                for kc in range(8):
                                    P.op("tensor", lambda e, p3=p3, kc=kc, j=j: e.matmul(
                                        psum[p3][:, 0:w], lhsT=w3h[:, kc, j * 128:(j + 1) * 128], rhs=h2t[:, kc, 0:w], start=(kc == 0), stop=(kc == 7)),
                                        reads=[w3b, h2tb], writes=[psb[p3]])
                                s_, s_b = sr.next()
                                P.op("scalar", lambda e, s_=s_, p1=p1: e.activation(out=s_[:, 0:w], in_=psum[p1][:, 0:w], func=AF.Silu), reads=[psb[p1]], writes=[s_b])
                                P.op("vector", lambda e, s_=s_, p3=p3, j=j: e.tensor_tensor(out=ut[:, j, 0:w], in0=psum[p3][:, 0:w], in1=s_[:, 0:w], op=ALU.mult),
                                     reads=[psb[p3], s_b], writes=[utb] if j == 0 else (), pwrites=[utb] if j else ())
                            return (ti, ut, utb, gtile, gtb_, w2h, w2b, ex)

                        def stF2(st):
                            ti, ut, utb, gtile, gtb_, w2h_, w2b_, ex_ = st
                            t0, w = TT[ti]
                            col = 1 if ti == 8 else 0
                            for oc in range(8):
                                p = nps()
                                for j in range(11):
                                    P.op("tensor", lambda e, p=p, j=j, oc=oc: e.matmul(
                                        psum[p][:, 0:w], lhsT=w2h_[:, j, oc * 128:(oc + 1) * 128], rhs=ut[:, j, 0:w], start=(j == 0), stop=(j == 10)),
                                        reads=[w2b_, utb], writes=[psb[p]])
                                ct, ctb = cr.next()
                                if ex_ is None:
                                    P.op("scalar", lambda e, ct=ct, p=p, oc=oc: e.activation(
                                        out=ct[:, 0:w], in_=psum[p][:, 0:w], func=AF.Identity, scale=mv(l, col, 5)[:, oc:oc + 1]), reads=[psb[p], cB], writes=[ctb])
                                else:
                                    P.op("vector", lambda e, ct=ct, p=p, oc=oc: e.scalar_tensor_tensor(
                                        out=ct[:, 0:w], in0=psum[p][:, 0:w], scalar=mv(l, col, 5)[:, oc:oc + 1], in1=gtile[:, 0:w], op0=ALU.mult, op1=ALU.mult),
                                        reads=[psb[p], cB, gtb_], writes=[ctb])
                                P.op("pool", lambda e, ct=ct, oc=oc: e.dma_start(
                                    out=xT[oc * 128:(oc + 1) * 128, t0:t0 + w], in_=ct[:, 0:w], accum_op=ALU.add), reads=[ctb], pwrites=[xb[ti]])

                        for ti in range(len(TT)):
                            if ti == 8 and not ctx_out:
                                continue
                            st_new = stF1(ti)
                            if pendF[0] is not None:
                                stF2(pendF[0])
                            pendF[0] = st_new
                            for _ in range(4):
                                if nxt_steps:
                                    nxt_steps.pop(0)()
                        while nxt_steps:
                            nxt_steps.pop(0)()
                        cur_w = nxt_w
                if pendF[0] is not None:
                    stF2(pendF[0])
                    pendF[0] = None

        if upto >= 8:
          with ExitStack() as ph:
            P.barrier()
            xr = Rot([sb(ph, f"fx{i}", [128, 8, 512], F32) for i in range(2)])
            sqr = Rot([sb(ph, "fsq", [128, 8, 512], BF16)])
            rsr = Rot([sb(ph, f"frs{i}", [128, 512], F32) for i in range(2)])
            for ti in range(8):
                t0, w = TT[ti]
                xt, xtb = xr.next()
                sq, sqb = sqr.next()
                rs, rsb = rsr.next()
                P.op("sync", lambda e, xt=xt, t0=t0: e.dma_start(out=xt[:], in_=xT.rearrange("(kc k) t -> k kc t", k=128)[:, :, t0:t0 + 512]),
                     reads=[xb[ti]], writes=[xtb])
                P.op("scalar", lambda e, xt=xt, sq=sq: e.activation(out=sq[:], in_=xt[:], func=AF.Square), reads=[xtb], writes=[sqb])
                for kc in range(8):
                    P.op("tensor", lambda e, kc=kc, sq=sq: e.matmul(psum[7][:], lhsT=onesb[:], rhs=sq[:, kc, :], start=(kc == 0), stop=(kc == 7)),
                         reads=[sqb, cB], writes=[psb[7]])
                P.op("scalar", lambda e, rs=rs: e.activation(out=rs[:], in_=psum[7][:], func=AF.Ln, scale=1.0 / D, bias=epsc[:, 0:1]), reads=[psb[7], cB], writes=[rsb])
                P.op("scalar", lambda e, rs=rs: e.activation(out=rs[:], in_=rs[:], func=AF.Exp, scale=-0.5), writes=[rsb])
                for kc in range(8):
                    P.op("vector", lambda e, kc=kc, xt=xt, rs=rs: e.scalar_tensor_tensor(
                        out=xt[:, kc, :], in0=xt[:, kc, :], scalar=nfin_sb[:, kc:kc + 1], in1=rs[:], op0=ALU.mult, op1=ALU.mult),
                        reads=[rsb, cB], writes=[xtb])
                P.op("pool", lambda e, xt=xt, t0=t0: e.dma_start(out=outT.rearrange("(kc k) t -> k kc t", k=128)[:, :, t0:t0 + 512], in_=xt[:]),
                     reads=[xtb], pwrites=[outb])
        P.finish([outb, FMb, TMb, BRb, H2b, GTb] + xb)
        print(f"[build] ops={P.n_ops} waits={P.n_waits}")
    return nc


_CONSTS = None


def _prep(inputs, ncores=8):
    global _CONSTS
    if _CONSTS is None:
        _CONSTS = _consts()
    f32 = lambda a: np.ascontiguousarray(np.asarray(a, dtype=np.float32))
    x, c, ctx, c_ctx = f32(inputs["x"]), f32(inputs["c"]), f32(inputs["ctx"]), f32(inputs["c_ctx"])

    def pk(v):
        v = f32(v)
        lead = v.shape[:-1]
        return np.ascontiguousarray(np.moveaxis(v.reshape(lead + (8, 128)), -1, 0))
    shared = {
        "w_mod": f32(inputs["w_mod"]),
        "b_mod": np.ascontiguousarray(np.moveaxis(f32(inputs["b_mod"]).reshape(DEPTH, 48, 128), -1, 0)),
        "nmix": pk(inputs["norm_mix"]), "nffn": pk(inputs["norm_ffn"]), "nfin": pk(inputs["norm_final"]),
        "w_in": f32(inputs["w_in"]),
        "sink": np.ascontiguousarray(np.broadcast_to(f32(inputs["attn_sink"]).reshape(1, -1), (128, DEPTH * 8))),
        "decs": np.ascontiguousarray(np.broadcast_to(
            np.stack([f32(inputs["ret_decay_fwd"]), f32(inputs["ret_decay_bwd"])], 1).reshape(1, -1), (128, DEPTH * 8))),
        "dlam": np.ascontiguousarray(np.broadcast_to(f32(inputs["diff_lambda"])[None], (128, DEPTH, 4, 64))),
        "dgain": np.ascontiguousarray(np.broadcast_to(f32(inputs["diff_norm"])[None], (128, DEPTH, 128))),
        "w_branch": f32(inputs["w_branch"]), "w_out": f32(inputs["w_out"]),
        "ffn_w1": f32(inputs["ffn_w1"]), "ffn_w3": f32(inputs["ffn_w3"]), "ffn_w2": f32(inputs["ffn_w2"]),
        "moe_router": f32(inputs["moe_router"]),
        "moe_w1": f32(inputs["moe_w1"]), "moe_w3": f32(inputs["moe_w3"]), "moe_w2": f32(inputs["moe_w2"]),
    }
    shared.update(_CONSTS)
    maps = []
    for b in range(ncores):
        m = dict(shared)
        m["xT_in"] = np.ascontiguousarray(np.concatenate([x[b], ctx[b]], 0).T)
        c2 = np.stack([c[b], c_ctx], -1)
        m["c2"] = np.ascontiguousarray(c2.reshape(8, 128, 2).transpose(1, 0, 2))
        maps.append(m)
    return maps


_NC = None


def kernel(**inputs):
    global _NC
    if _NC is None:
        _NC = build()
    maps = _prep(inputs)
    res = run_bass_kernel_spmd(_NC, maps, core_ids=list(range(8)))
    out = np.stack([np.ascontiguousarray(res.results[b]["outT"].T) for b in range(8)], 0)
    return out.astype(np.float32)
```
